# Optimizing a Trainium2 kernel written in Bass

```python
import jax, jax.numpy as jnp
from jax import lax
import numpy as np

D_MODEL = 1024
BATCH = 16
SEQ = 4096
DEPTH = 1

D_MIX = D_MODEL
ROPE_THETA = 500000.0
EPS = 1e-6
NEG = -1e30
FORCE = 1e4
MLA_HEADS = 8
MLA_Q_LORA = 256
MLA_KV_LORA = 128
MLA_NOPE = 64
MLA_ROPE = 32
MLA_V = 64
MLA_QK = MLA_NOPE + MLA_ROPE
MLA_WIDTH = MLA_HEADS * MLA_V
MLA_QBLOCK = 128
NSA_HEADS = 8
NSA_GROUPS = 2
NSA_REP = NSA_HEADS // NSA_GROUPS
NSA_DK = 64
NSA_DV = 64
NSA_WIDTH = NSA_HEADS * NSA_DV
NSA_ROT = NSA_DK // 4
CMP_LEN = 32
CMP_STRIDE = 16
CMP_HIDDEN = 128
SEL_LEN = 64
N_SEL = 16
WIN = 512
NSA_QBLOCK = 32
MEM_TOKENS = 256
XA_HEADS = 4
XA_DH = 128
PEER_HEADS = 8
PEER_KEYS = 128
PEER_EXPERTS = PEER_KEYS * PEER_KEYS
PEER_QDIM = 128
PEER_HALF = PEER_QDIM // 2
PEER_TOPK = 16
PEER_CHUNK = 128

IN_SIZES = (MLA_Q_LORA, MLA_KV_LORA, MLA_ROPE,
            NSA_HEADS * NSA_DK,
            NSA_GROUPS * NSA_DK, NSA_GROUPS * NSA_DV,
            NSA_GROUPS * NSA_DK, NSA_GROUPS * NSA_DV,
            NSA_GROUPS * NSA_DK, NSA_GROUPS * NSA_DV,
            NSA_HEADS * 3)
IN_COLS = sum(IN_SIZES)

kernel_name = "hybrid_mla_nsa_memxattn_peer"


def rms_norm(x, g):
    xf = x.astype(jnp.float32)
    y = xf * lax.rsqrt(jnp.mean(xf * xf, axis=-1, keepdims=True) + EPS)
    return (y * g.astype(jnp.float32)).astype(x.dtype)


def rope_tables(pos, rot_dim):
    inv = 1.0 / (ROPE_THETA ** (jnp.arange(0, rot_dim, 2, dtype=jnp.float32) / rot_dim))
    ang = pos.astype(jnp.float32)[:, None] * inv[None, :]
    return jnp.cos(ang), jnp.sin(ang)


def rotate(x, cos, sin):
    x1, x2 = jnp.split(x.astype(jnp.float32), 2, axis=-1)
    c = cos[None, :, None, :]
    s = sin[None, :, None, :]
    return jnp.concatenate([x1 * c - x2 * s, x2 * c + x1 * s], axis=-1).astype(x.dtype)


def partial_rope(x, cos, sin):
    r = 2 * cos.shape[-1]
    return jnp.concatenate([rotate(x[..., :r], cos, sin), x[..., r:]], axis=-1)


def masked_softmax(s, mask):
    p = jax.nn.softmax(jnp.where(mask, s.astype(jnp.float32), NEG), axis=-1)
    return p * mask


def mla_mixer(c_q, c_kv, k_pe, q_norm, kv_norm, w_uq, w_ukv, qk_norm):
    B, S, _ = c_q.shape
    cos, sin = rope_tables(jnp.arange(S), MLA_ROPE)
    q = (rms_norm(c_q, q_norm) @ w_uq).reshape(B, S, MLA_HEADS, MLA_QK)
    kv = (rms_norm(c_kv, kv_norm) @ w_ukv).reshape(B, S, MLA_HEADS, MLA_NOPE + MLA_V)
    k_nope, v = kv[..., :MLA_NOPE], kv[..., MLA_NOPE:]
    k = jnp.concatenate([k_nope, jnp.broadcast_to(k_pe[:, :, None, :], (B, S, MLA_HEADS, MLA_ROPE))], axis=-1)
    q = rms_norm(q, qk_norm[0])
    k = rms_norm(k, qk_norm[1])
    q = jnp.concatenate([q[..., :MLA_NOPE], rotate(q[..., MLA_NOPE:], cos, sin)], axis=-1)
    k = jnp.concatenate([k[..., :MLA_NOPE], rotate(k[..., MLA_NOPE:], cos, sin)], axis=-1)
    scale = MLA_QK ** -0.5
    kpos = jnp.arange(S)

    def block(i):
        s0 = i * MLA_QBLOCK
        qb = lax.dynamic_slice_in_dim(q, s0, MLA_QBLOCK, axis=1)
        sc = jnp.einsum('bqhd,bkhd->bhqk', qb, k).astype(jnp.float32) * scale
        mask = kpos[None, :] <= (s0 + jnp.arange(MLA_QBLOCK))[:, None]
        p = masked_softmax(sc, mask)
        return jnp.einsum('bhqk,bkhd->bqhd', p.astype(v.dtype), v)

    o = lax.map(block, jnp.arange(S // MLA_QBLOCK))
    return o.transpose(1, 0, 2, 3, 4).reshape(B, S, MLA_WIDTH)


def compress(t, pe, w1, w2):
    B, S, G, d = t.shape
    n_cmp = (S - CMP_LEN) // CMP_STRIDE + 1
    idx = jnp.arange(n_cmp)[:, None] * CMP_STRIDE + jnp.arange(CMP_LEN)[None, :]
    blk = t[:, idx] + pe[None, None, :, None, :]
    blk = blk.transpose(0, 1, 3, 2, 4).reshape(B, n_cmp, G, CMP_LEN * d)
    return jax.nn.gelu(blk @ w1) @ w2


def nsa_mixer(q_in, kc_in, vc_in, ks_in, vs_in, kw_in, vw_in, gate_in, q_norm, k_norm, cmp_pe, cmp_w1, cmp_w2):
    B, S, _ = q_in.shape
    G, R = NSA_GROUPS, NSA_REP
    n_cmp = (S - CMP_LEN) // CMP_STRIDE + 1
    n_blk = S // SEL_LEN
    n_sel = min(N_SEL, n_blk)
    cos, sin = rope_tables(jnp.arange(S), NSA_ROT)
    cmp_end = jnp.arange(n_cmp) * CMP_STRIDE + CMP_LEN - 1
    cos_c, sin_c = rope_tables(cmp_end, NSA_ROT)

    def grp(t, d):
        return t.reshape(B, S, G, d)

    q = partial_rope(rms_norm(q_in.reshape(B, S, NSA_HEADS, NSA_DK), q_norm), cos, sin).reshape(B, S, G, R, NSA_DK)
    kc = compress(grp(kc_in, NSA_DK), cmp_pe[0], cmp_w1[0], cmp_w2[0])
    vc = compress(grp(vc_in, NSA_DV), cmp_pe[1], cmp_w1[1], cmp_w2[1])
    kc = partial_rope(rms_norm(kc, k_norm[0]), cos_c, sin_c)
    ks = partial_rope(rms_norm(grp(ks_in, NSA_DK), k_norm[1]), cos, sin)
    vs = grp(vs_in, NSA_DV)
    kw = partial_rope(rms_norm(grp(kw_in, NSA_DK), k_norm[2]), cos, sin)
    vw = grp(vw_in, NSA_DV)
    gates = jax.nn.sigmoid(gate_in.astype(jnp.float32)).reshape(B, S, G, R, 3).astype(q_in.dtype)

    ks_blk = ks.reshape(B, n_blk, SEL_LEN, G, NSA_DK).transpose(0, 3, 1, 2, 4).reshape(B, G, n_blk, SEL_LEN * NSA_DK)
    vs_blk = vs.reshape(B, n_blk, SEL_LEN, G, NSA_DV).transpose(0, 3, 1, 2, 4).reshape(B, G, n_blk, SEL_LEN * NSA_DV)
    cmp_start = jnp.arange(n_cmp) * CMP_STRIDE
    blk_ids = jnp.arange(n_blk)
    blk_start = blk_ids * SEL_LEN
    overlap = ((cmp_start[:, None] < blk_start[None, :] + SEL_LEN)
               & (cmp_start[:, None] + CMP_LEN > blk_start[None, :])).astype(jnp.float32)
    kw_pad = jnp.pad(kw, ((0, 0), (WIN, 0), (0, 0), (0, 0)))
    vw_pad = jnp.pad(vw, ((0, 0), (WIN, 0), (0, 0), (0, 0)))
    scale = NSA_DK ** -0.5
    QB = NSA_QBLOCK

    def block(i):
        s0 = i * QB
        qpos = s0 + jnp.arange(QB)
        qb = lax.dynamic_slice_in_dim(q, s0, QB, axis=1)
        sc = jnp.einsum('bqgrd,bngd->bgrqn', qb, kc).astype(jnp.float32) * scale
        p_c = masked_softmax(sc, cmp_end[None, :] <= qpos[:, None])
        o_c = jnp.einsum('bgrqn,bngd->bqgrd', p_c.astype(vc.dtype), vc)
        imp = jnp.einsum('bgrqn,nm->bgqm', p_c, overlap)
        cur = qpos // SEL_LEN
        forced = (blk_ids[None, :] == 0) | (blk_ids[None, :] == cur[:, None]) | (blk_ids[None, :] == cur[:, None] - 1)
        score = jnp.where(blk_ids[None, :] <= cur[:, None], jnp.where(forced, FORCE, imp), NEG)
        top_val, top_idx = lax.top_k(score, n_sel)
        flat = top_idx.reshape(B, G, QB * n_sel, 1)
        ks_g = jnp.take_along_axis(ks_blk, flat, axis=2).reshape(B, G, QB, n_sel * SEL_LEN, NSA_DK)
        vs_g = jnp.take_along_axis(vs_blk, flat, axis=2).reshape(B, G, QB, n_sel * SEL_LEN, NSA_DV)
        tok = (top_idx[..., None] * SEL_LEN + jnp.arange(SEL_LEN)).reshape(B, G, QB, n_sel * SEL_LEN)
        ok = jnp.repeat(top_val > 0.5 * NEG, SEL_LEN, axis=-1)
        smask = ok & (tok <= qpos[None, None, :, None])
        ss = jnp.einsum('bqgrd,bgqkd->bgrqk', qb, ks_g).astype(jnp.float32) * scale
        p_s = masked_softmax(ss, smask[:, :, None])
        o_s = jnp.einsum('bgrqk,bgqkd->bqgrd', p_s.astype(vs_g.dtype), vs_g)
        kwb = lax.dynamic_slice_in_dim(kw_pad, s0, WIN + QB, axis=1)
        vwb = lax.dynamic_slice_in_dim(vw_pad, s0, WIN + QB, axis=1)
        kpos = s0 - WIN + jnp.arange(WIN + QB)
        dist = qpos[:, None] - kpos[None, :]
        wmask = (kpos[None, :] >= 0) & (dist >= 0) & (dist < WIN)
        sw = jnp.einsum('bqgrd,bkgd->bgrqk', qb, kwb).astype(jnp.float32) * scale
        p_w = masked_softmax(sw, wmask)
        o_w = jnp.einsum('bgrqk,bkgd->bqgrd', p_w.astype(vwb.dtype), vwb)
        gb = lax.dynamic_slice_in_dim(gates, s0, QB, axis=1)
        return gb[..., 0:1] * o_c + gb[..., 1:2] * o_s + gb[..., 2:3] * o_w

    o = lax.map(block, jnp.arange(S // QB))
    return o.transpose(1, 0, 2, 3, 4, 5).reshape(B, S, NSA_WIDTH)


def memory_cross_attention(hn, mn, wq, wkv, qk_norm, wo):
    B, S, _ = hn.shape
    M = mn.shape[1]
    q = rms_norm((hn @ wq).reshape(B, S, XA_HEADS, XA_DH), qk_norm[0])
    kv = (mn @ wkv).reshape(B, M, 2, XA_HEADS, XA_DH)
    k = rms_norm(kv[:, :, 0], qk_norm[1])
    v = kv[:, :, 1]
    s = jnp.einsum('bqhd,bkhd->bhqk', q, k).astype(jnp.float32) * (XA_DH ** -0.5)
    p = jax.nn.softmax(s, axis=-1)
    o = jnp.einsum('bhqk,bkhd->bqhd', p.astype(v.dtype), v).reshape(B, S, XA_HEADS * XA_DH)
    return o @ wo


def peer_ffn(hn, wq, sub_keys, u, v):
    B, S, D = hn.shape
    xt = hn.reshape(-1, PEER_CHUNK, D)

    def chunk(xc):
        q = (xc @ wq).reshape(PEER_CHUNK, PEER_HEADS, 2, PEER_HALF)
        s = jnp.einsum('thpd,hpkd->thpk', q, sub_keys).astype(jnp.float32)
        s1, i1 = lax.top_k(s[:, :, 0], PEER_TOPK)
        s2, i2 = lax.top_k(s[:, :, 1], PEER_TOPK)
        cand = (s1[..., :, None] + s2[..., None, :]).reshape(PEER_CHUNK, PEER_HEADS, PEER_TOPK * PEER_TOPK)
        cidx = (i1[..., :, None] * PEER_KEYS + i2[..., None, :]).reshape(PEER_CHUNK, PEER_HEADS, PEER_TOPK * PEER_TOPK)
        top, pos = lax.top_k(cand, PEER_TOPK)
        eidx = jnp.take_along_axis(cidx, pos, axis=-1)
        g = jax.nn.softmax(top, axis=-1)
        u_e = jnp.take(u, eidx, axis=0)
        act = jax.nn.gelu(jnp.einsum('thkd,td->thk', u_e, xc).astype(jnp.float32))
        v_e = jnp.take(v, eidx, axis=0)
        return jnp.einsum('thk,thkd->td', (g * act).astype(v.dtype), v_e)

    return lax.map(chunk, xt).reshape(B, S, D)


def setup_inputs(seed: int = 0) -> dict:
    key = jax.random.key(seed)
    ks = jax.random.split(key, 32)
    f32 = jnp.float32
    L = DEPTH

    def nrm(k, shape, scale):
        return jax.random.normal(k, shape, f32) * scale

    def gain(k, shape):
        return 1.0 + 0.02 * jax.random.normal(k, shape, f32)

    return {
        "x": nrm(ks[0], (BATCH, SEQ, D_MODEL), 1.0),
        "mem": nrm(ks[1], (BATCH, MEM_TOKENS, D_MODEL), 1.0),
        "mix_norm": gain(ks[2], (L, D_MODEL)),
        "w_in": nrm(ks[3], (L, D_MODEL, IN_COLS), D_MODEL ** -0.5),
        "mla_q_norm": gain(ks[4], (L, MLA_Q_LORA)),
        "mla_kv_norm": gain(ks[5], (L, MLA_KV_LORA)),
        "mla_w_uq": nrm(ks[6], (L, MLA_Q_LORA, MLA_HEADS * MLA_QK), MLA_Q_LORA ** -0.5),
        "mla_w_ukv": nrm(ks[7], (L, MLA_KV_LORA, MLA_HEADS * (MLA_NOPE + MLA_V)), MLA_KV_LORA ** -0.5),
        "mla_qk_norm": gain(ks[8], (L, 2, MLA_QK)),
        "nsa_q_norm": gain(ks[9], (L, NSA_DK)),
        "nsa_k_norm": gain(ks[10], (L, 3, NSA_DK)),
        "nsa_cmp_pe": nrm(ks[11], (L, 2, CMP_LEN, NSA_DK), 0.1),
        "nsa_cmp_w1": nrm(ks[12], (L, 2, CMP_LEN * NSA_DK, CMP_HIDDEN), (CMP_LEN * NSA_DK) ** -0.5),
        "nsa_cmp_w2": nrm(ks[13], (L, 2, CMP_HIDDEN, NSA_DK), CMP_HIDDEN ** -0.5),
        "mix_out_norm": gain(ks[14], (L, D_MIX)),
        "w_out": nrm(ks[15], (L, D_MIX, D_MODEL), D_MIX ** -0.5),
        "xa_norm": gain(ks[16], (L, D_MODEL)),
        "mem_norm": gain(ks[17], (L, D_MODEL)),
        "xa_wq": nrm(ks[18], (L, D_MODEL, XA_HEADS * XA_DH), D_MODEL ** -0.5),
        "xa_wkv": nrm(ks[19], (L, D_MODEL, 2 * XA_HEADS * XA_DH), D_MODEL ** -0.5),
        "xa_qk_norm": gain(ks[20], (L, 2, XA_DH)),
        "xa_wo": nrm(ks[21], (L, XA_HEADS * XA_DH, D_MODEL), (XA_HEADS * XA_DH) ** -0.5),
        "ffn_norm": gain(ks[22], (L, D_MODEL)),
        "peer_wq": nrm(ks[23], (L, D_MODEL, PEER_HEADS * PEER_QDIM), D_MODEL ** -0.5),
        "peer_keys": nrm(ks[24], (L, PEER_HEADS, 2, PEER_KEYS, PEER_HALF), PEER_HALF ** -0.5),
        "peer_u": nrm(ks[25], (L, PEER_EXPERTS, D_MODEL), D_MODEL ** -0.5),
        "peer_v": nrm(ks[26], (L, PEER_EXPERTS, D_MODEL), PEER_HEADS ** -0.5),
    }


def reference(x, mem, mix_norm, w_in, mla_q_norm, mla_kv_norm, mla_w_uq, mla_w_ukv, mla_qk_norm,
              nsa_q_norm, nsa_k_norm, nsa_cmp_pe, nsa_cmp_w1, nsa_cmp_w2, mix_out_norm, w_out,
              xa_norm, mem_norm, xa_wq, xa_wkv, xa_qk_norm, xa_wo,
              ffn_norm, peer_wq, peer_keys, peer_u, peer_v):
    offsets = [int(o) for o in np.cumsum(IN_SIZES)[:-1]]
    h = x
    for l in range(DEPTH):
        n = rms_norm(h, mix_norm[l])
        (c_q, c_kv, k_pe, q_nsa, kc_in, vc_in, ks_in, vs_in,
         kw_in, vw_in, gate_in) = jnp.split(n @ w_in[l], offsets, axis=-1)
        o_mla = mla_mixer(c_q, c_kv, k_pe, mla_q_norm[l], mla_kv_norm[l], mla_w_uq[l], mla_w_ukv[l], mla_qk_norm[l])
        o_nsa = nsa_mixer(q_nsa, kc_in, vc_in, ks_in, vs_in, kw_in, vw_in, gate_in,
                          nsa_q_norm[l], nsa_k_norm[l], nsa_cmp_pe[l], nsa_cmp_w1[l], nsa_cmp_w2[l])
        g_out = mix_out_norm[l]
        mixed = jnp.concatenate([rms_norm(o_mla, g_out[:MLA_WIDTH]), rms_norm(o_nsa, g_out[MLA_WIDTH:])], axis=-1)
        h = h + mixed @ w_out[l]
        h = h + memory_cross_attention(rms_norm(h, xa_norm[l]), rms_norm(mem, mem_norm[l]),
                                       xa_wq[l], xa_wkv[l], xa_qk_norm[l], xa_wo[l])
        h = h + peer_ffn(rms_norm(h, ffn_norm[l]), peer_wq[l], peer_keys[l], peer_u[l], peer_v[l])
    return h
```

```python
import numpy as np
import concourse.bass as bass
import concourse.mybir as mybir
from concourse.bass_utils import run_bass_kernel_spmd
from contextlib import ExitStack

F32 = mybir.dt.float32
BF16 = mybir.dt.bfloat16
U32 = mybir.dt.uint32
I32 = mybir.dt.int32
AF = mybir.ActivationFunctionType
ALU = mybir.AluOpType
AX = mybir.AxisListType

SEM_LIMIT = 1 << 30


class Buf:
    __slots__ = ("w", "r")

    def __init__(self):
        self.w = None
        self.r = {}


class Slot:
    __slots__ = ("ctr",)

    def __init__(self, ctr):
        self.ctr = ctr


class Eng:
    def __init__(self, fw, name, handle, is_pe=False):
        self.fw = fw
        self.name = name
        self.h = handle
        self.sem = None
        self.cnt = 0
        self.known = {}
        self.q = []
        self.is_pe = is_pe

    def new_event(self):
        if self.sem is None or self.cnt >= SEM_LIMIT:
            self.sem = self.fw.new_sem()
            self.cnt = 0
        self.cnt += 1
        return (self.sem, self.cnt)


class FW:
    def __init__(self, nc, stack):
        self.nc = nc
        self.stack = stack
        self.nsem = 0
        self.pe = Eng(self, "pe", nc.tensor, True)
        self.act = Eng(self, "act", nc.scalar)
        self.dve = Eng(self, "dve", nc.vector)
        self.pool = Eng(self, "pool", nc.gpsimd)
        self.sp = Eng(self, "sp", nc.sync)
        self.engs = [self.pe, self.act, self.dve, self.pool, self.sp]
        self.ctrs = []
        self.free_ctrs = []

    def new_sem(self):
        self.nsem += 1
        return self.stack.enter_context(self.nc.semaphore(f"fs{self.nsem}"))

    def slot(self):
        if self.free_ctrs:
            ctr = self.free_ctrs.pop()
        else:
            ctr = [None, 0]
            self.ctrs.append(ctr)
        return Slot(ctr)

    def _waits(self, eng, reads, writes):
        evs = {}

        def add(ev):
            if ev is None:
                return
            s, v = ev
            if evs.get(s, 0) < v:
                evs[s] = v

        for b in reads:
            add(b.w)
        for b in writes:
            add(b.w)
            for s, v in b.r.items():
                add((s, v))
        out = []
        for s, v in evs.items():
            if eng.known.get(s, 0) >= v:
                continue
            if eng.is_pe and s is eng.sem:
                continue
            out.append((s, v))
            eng.known[s] = v
        return out

    def _commit(self, ev, reads, writes):
        s, v = ev
        for b in reads:
            if b.r.get(s, 0) < v:
                b.r[s] = v
        for b in writes:
            b.w = ev
            b.r = {}

    def op(self, eng, fn, reads=(), writes=()):
        waits = self._waits(eng, reads, writes)
        ev = eng.new_event()
        eng.q.append((waits, fn, ev, 1))
        self._commit(ev, reads, writes)
        return ev

    def dma(self, eng, out, in_, reads, writes, slot, fn=None):
        waits = self._waits(eng, reads, writes)
        ctr = slot.ctr
        if ctr[0] is None:
            ctr[0] = self.new_sem()
            ctr[1] = 0
        if ctr[1] > 0 and eng.known.get(ctr[0], 0) < ctr[1]:
            if not any(s_ is ctr[0] for s_, _ in waits):
                waits.append((ctr[0], ctr[1]))
            else:
                waits = [((s_, max(v_, ctr[1])) if s_ is ctr[0] else (s_, v_)) for s_, v_ in waits]
            eng.known[ctr[0]] = ctr[1]
        ctr[1] += 16
        ev = (ctr[0], ctr[1])
        if fn is None:
            fn = lambda h: h.dma_start(out=out, in_=in_)
        eng.q.append((waits, fn, ev, 16))
        self._commit(ev, reads, writes)
        return ev

    def barrier(self):
        evs = []
        for e in self.engs:
            if e.sem is not None and e.cnt > 0:
                evs.append((e.sem, e.cnt))
        for c in self.ctrs:
            if c[0] is not None and c[1] > 0:
                evs.append((c[0], c[1]))
        for e in self.engs:
            waits = []
            for s, v in evs:
                if e.known.get(s, 0) >= v:
                    continue
                if s is e.sem:
                    continue
                waits.append((s, v))
                e.known[s] = v
            if waits:
                e.q.append((waits, None, None, 0))

    def finish(self):
        self.barrier()
        with self.nc.Block() as block:
            for eng, deco in [(self.pe, block.tensor), (self.act, block.scalar),
                              (self.dve, block.vector), (self.pool, block.gpsimd),
                              (self.sp, block.sync)]:
                def body(h, eng=eng):
                    for waits, fn, ev, inc in eng.q:
                        for s, v in waits:
                            h.wait_ge(s, v)
                        if fn is None:
                            continue
                        ins = fn(h)
                        ins.then_inc(ev[0], inc)
                deco(body)


D = 1024
IN_COLS = 1720
EPS = 1e-6
ROPE_THETA = 500000.0

PARAM_NAMES = ["mix_norm", "w_in", "mla_q_norm", "mla_kv_norm", "mla_w_uq", "mla_w_ukv",
               "mla_qk_norm", "nsa_q_norm", "nsa_k_norm", "nsa_cmp_pe", "nsa_cmp_w1",
               "nsa_cmp_w2", "mix_out_norm", "w_out", "xa_norm", "mem_norm", "xa_wq",
               "xa_wkv", "xa_qk_norm", "xa_wo", "ffn_norm", "peer_wq", "peer_keys",
               "peer_u", "peer_v"]
PARAM_SHAPES = {
    "mix_norm": [D], "w_in": [D, IN_COLS], "mla_q_norm": [256], "mla_kv_norm": [128],
    "mla_w_uq": [256, 768], "mla_w_ukv": [128, 1024], "mla_qk_norm": [2, 96],
    "nsa_q_norm": [64], "nsa_k_norm": [3, 64], "nsa_cmp_pe": [2, 32, 64],
    "nsa_cmp_w1": [2, 2048, 128], "nsa_cmp_w2": [2, 128, 64], "mix_out_norm": [D],
    "w_out": [D, D], "xa_norm": [D], "mem_norm": [D], "xa_wq": [D, 512],
    "xa_wkv": [D, 1024], "xa_qk_norm": [2, 128], "xa_wo": [512, D], "ffn_norm": [D],
    "peer_wq": [D, D], "peer_keys": [8, 2, 128, 64], "peer_u": [16384, D],
    "peer_v": [16384, D],
}


def host_consts(S):
    c = {}
    pos = np.arange(S, dtype=np.float32)
    inv_m = (1.0 / (ROPE_THETA ** (np.arange(0, 32, 2, dtype=np.float32) / 32))).astype(np.float32)
    ang = pos[:, None] * inv_m[None, :]
    c["rope_m"] = np.concatenate([np.cos(ang), np.sin(ang)], axis=1).astype(np.float32)
    inv_n = (1.0 / (ROPE_THETA ** (np.arange(0, 16, 2, dtype=np.float32) / 16))).astype(np.float32)
    ang = pos[:, None] * inv_n[None, :]
    c["rope_n"] = np.concatenate([np.cos(ang), np.sin(ang)], axis=1).astype(np.float32)
    ncmp = (S - 32) // 16 + 1
    ncp = ((ncmp + 127) // 128) * 128
    cend = (np.arange(ncp) * 16 + 31).astype(np.float32)
    ang = cend[:, None] * inv_n[None, :]
    c["rope_c"] = np.concatenate([np.cos(ang), np.sin(ang)], axis=1).astype(np.float32)
    nblk = S // 64
    cs = np.arange(ncp) * 16
    bs = np.arange(nblk) * 64
    ov = ((cs[:, None] < bs[None, :] + 64) & (cs[:, None] + 32 > bs[None, :])).astype(np.float32)
    ov[ncmp:] = 0.0
    c["overlap"] = ov
    q = np.arange(S)
    cur = q // 64
    b = np.arange(nblk)
    forced = (b[None, :] == 0) | (b[None, :] == cur[:, None]) | (b[None, :] == cur[:, None] - 1)
    valid = b[None, :] <= cur[:, None]
    c["selmul"] = (valid & ~forced).astype(np.float32)
    c["seladd"] = np.where(valid, np.where(forced, 1e4, 0.0), -1e30).astype(np.float32)
    nt = S // 128
    E = np.zeros((nblk, nt, 128), np.float32)
    for kt in range(nt):
        for k in range(128):
            E[2 * kt + k // 64, kt, k] = 1.0
    c["emat"] = E
    c["iota128"] = np.arange(128, dtype=np.float32).reshape(1, 128)
    c["iota"] = np.concatenate([np.arange(16), np.arange(1, 16) * 16, [0]]).astype(np.float32).reshape(1, 32)
    return c


class KB:
    pass


def build(S, NSEQ, dbg=False, phases=(1, 2, 3, 4)):
    nc = bass.Bass("TRN2", target_bir_lowering=False)
    NT = S // 128
    NBLK = S // 64
    NCMP = (S - 32) // 16 + 1
    NCP = ((NCMP + 127) // 128) * 128
    TOK = NSEQ * S
    okind = "ExternalOutput" if dbg else "Internal"
    dr = {}
    dr["x"] = nc.dram_tensor("x", [NSEQ, S, D], F32, kind="ExternalInput").ap()
    dr["mem"] = nc.dram_tensor("mem", [NSEQ, 256, D], F32, kind="ExternalInput").ap()
    for n in PARAM_NAMES:
        dr[n] = nc.dram_tensor(n, PARAM_SHAPES[n], F32, kind="ExternalInput").ap()
    cshape = {"rope_m": [S, 32], "rope_n": [S, 16], "rope_c": [NCP, 16], "overlap": [NCP, NBLK],
              "selmul": [S, NBLK], "seladd": [S, NBLK], "emat": [NBLK, NT, 128], "iota": [1, 32], "iota128": [1, 128]}
    for n, sh in cshape.items():
        dr[n] = nc.dram_tensor(n, sh, F32, kind="ExternalInput").ap()
    out = nc.dram_tensor("out", [NSEQ, S, D], F32, kind="ExternalOutput").ap()
    dbgt = nc.dram_tensor("dbg", [128, 1024], F32, kind="ExternalOutput").ap() if dbg else None
    sc = {}

    def scr(name, shape, dt):
        sc[name] = nc.dram_tensor("sc_" + name, shape, dt, kind=okind).ap()

    scr("QTm", [NSEQ, 96, 8, S], BF16)
    scr("KTm", [NSEQ, 96, 8, S], BF16)
    scr("Vm", [NSEQ, S, 8, 65], BF16)
    scr("QTn", [NSEQ, 64, 8, S], BF16)
    scr("KsT", [NSEQ, 64, 2, S], BF16)
    scr("KwT", [NSEQ, 64, 2, S], BF16)
    scr("Vs", [NSEQ, S, 2, 65], BF16)
    scr("Vw", [NSEQ, S, 2, 65], BF16)
    scr("gate", [NSEQ, S, 24], F32)
    scr("kcT", [NSEQ, 128, S], BF16)
    scr("vcT", [NSEQ, 128, S], BF16)
    scr("mixed", [NSEQ, S, D], BF16)
    sc["uT"] = nc.dram_tensor("sc_uT", [128, 128, 8, 128], BF16, kind="Internal").ap()
    sc["vbf"] = nc.dram_tensor("sc_vbf", [16384, D], BF16, kind="Internal").ap()
    sc["h2s"] = nc.dram_tensor("sc_h2s", [NSEQ, S, D], F32, kind="Internal").ap()
    scB = {k: Buf() for k in sc}

    with ExitStack() as gst:
        fw = FW(nc, gst)
        gst.enter_context(nc.allow_non_contiguous_dma(reason="strided param / scratch layouts"))
        gst.enter_context(nc.allow_low_precision(reason="bf16 matmul operands, fp32 accumulation"))
        k = KB()
        k.nc, k.fw, k.dr, k.sc, k.scB, k.out = nc, fw, dr, sc, scB, out
        k.S, k.NSEQ, k.NT, k.NBLK, k.NCMP, k.NCP = S, NSEQ, NT, NBLK, NCMP, NCP
        k.dbgt = dbgt
        k.banks = []
        for i in range(8):
            t = gst.enter_context(nc.psum_tensor(f"bank{i}", [128, 512], F32))
            k.banks.append((t, Buf()))
        k.bank_i = 0
        k.ident = gst.enter_context(nc.sbuf_tensor("ident", [128, 128], BF16))
        k.identf = gst.enter_context(nc.sbuf_tensor("identf", [128, 128], F32))
        k.eps = gst.enter_context(nc.sbuf_tensor("eps", [128, 1], F32))
        k.cB = Buf()
        fw.op(fw.pool, lambda h: h.memset(k.eps[:], EPS), [], [k.cB])
        fw.op(fw.pool, lambda h: h.memset(k.identf[:], 0.0), [], [k.cB])
        fw.op(fw.pool, lambda h: h.affine_select(out=k.identf[:], in_=k.identf[:], pattern=[[-1, 128]],
                                                 compare_op=ALU.not_equal, fill=1.0, base=0,
                                                 channel_multiplier=1), [], [k.cB])
        fw.op(fw.dve, lambda h: h.tensor_copy(out=k.ident[:], in_=k.identf[:]), [k.cB], [k.cB])
        if 1 in phases:
            phase1(k)
        if 2 in phases:
            phase2(k)
        if 3 in phases:
            phase3(k)
        if 4 in phases:
            (phase4d if PEER_DENSE else phase4)(k)
        fw.finish()
    return nc


def nextbank(k):
    t, b = k.banks[k.bank_i % 8]
    k.bank_i += 1
    return t, b


class Ph:
    def __init__(self, k, name):
        self.k = k
        self.fw = k.fw
        self.nc = k.nc
        self.name = name
        self.st = ExitStack()
        self.n = 0
        self.slots = []

    def slot(self):
        s = self.fw.slot()
        self.slots.append(s)
        return s

    def sb(self, shape, dt, name=None):
        self.n += 1
        t = self.st.enter_context(self.nc.sbuf_tensor(f"{self.name}_{name or 't'}{self.n}", shape, dt))
        return t

    def sbn(self, n, shape, dt, name=None):
        return [(self.sb(shape, dt, name), Buf()) for _ in range(n)]

    def close(self):
        self.fw.barrier()
        for s in self.slots:
            self.fw.free_ctrs.append(s.ctr)
        self.slots = []
        self.st.close()

    def tt(self, eng, out, a, b, op, r, w):
        return self.fw.op(eng, lambda h: h.tensor_tensor(out=out, in0=a, in1=b, op=op), r, w)

    def ts(self, eng, out, a, s1, op0, r, w, s2=None, op1=None):
        if op1 is None:
            return self.fw.op(eng, lambda h: h.tensor_scalar(out=out, in0=a, scalar1=s1, scalar2=None, op0=op0), r, w)
        return self.fw.op(eng, lambda h: h.tensor_scalar(out=out, in0=a, scalar1=s1, scalar2=s2, op0=op0, op1=op1), r, w)

    def act(self, out, in_, func, r, w, scale=1.0, bias=None, accum=None):
        kw = {}
        if bias is not None:
            kw["bias"] = bias
        if accum is not None:
            kw["accum_out"] = accum
        return self.fw.op(self.fw.act, lambda h: h.activation(out=out, in_=in_, func=func, scale=scale, **kw), r, w)

    def cp(self, eng, out, in_, r, w):
        if eng is self.fw.act:
            return self.fw.op(eng, lambda h: h.copy(out=out, in_=in_), r, w)
        return self.fw.op(eng, lambda h: h.tensor_copy(out=out, in_=in_), r, w)

    def red(self, out, in_, r, w, op=ALU.add):
        return self.fw.op(self.fw.dve, lambda h: h.tensor_reduce(out=out, in_=in_, axis=AX.X, op=op), r, w)

    def mm(self, out, lhsT, rhs, start, stop, r, w):
        return self.fw.op(self.fw.pe, lambda h: h.matmul(out=out, lhsT=lhsT, rhs=rhs, start=start, stop=stop), r, w)

    def tr(self, out, in_, r, w):
        ident = self.k.ident
        p = in_.partition_size()
        return self.fw.op(self.fw.pe, lambda h: h.transpose(out=out, in_=in_, identity=ident[0:p, 0:p]), list(r) + [self.k.cB], w)

    def rstd(self, out, ss, d, r, w):
        p = ss.partition_size()
        self.act(out, ss, AF.Sqrt, list(r) + [self.k.cB], w, scale=1.0 / d, bias=self.k.eps[0:p, :])
        self.fw.op(self.fw.dve, lambda h: h.reciprocal(out=out, in_=out), w, w)

    def load_w(self, dst, dstB, src, gain=None, rows=128, stage=None):
        C = dst.shape[1]
        N = dst.shape[2]
        srcv = src.rearrange("(c p) n -> p c n", p=rows)
        for c in range(C):
            st_t, st_b, slot = stage
            self.fw.dma(self.fw.sp, st_t[0:rows, 0:N], srcv[:, c, :], [], [st_b], slot)
            if gain is not None:
                self.ts(self.fw.dve, dst[:, c, :], st_t[0:rows, 0:N], gain[:, c:c + 1], ALU.mult, [st_b], [dstB])
            else:
                self.cp(self.fw.dve, dst[:, c, :], st_t[0:rows, 0:N], [st_b], [dstB])


def bcast_row(ap_row, n):
    return ap_row.to_broadcast([128, n])


def phase1(k):
    fw, dr, sc, scB = k.fw, k.dr, k.sc, k.scB
    S, NSEQ, NT = k.S, k.NSEQ, k.NT
    P = Ph(k, "p1")
    dve, act, pool, sp, pe = fw.dve, fw.act, fw.pool, fw.sp, fw.pe
    parB = Buf()
    parS = P.slot()
    stage = (P.sb([128, 1720], F32, "stage"), Buf(), P.slot())
    g_mix = P.sb([128, 8], F32)
    g_q = P.sb([128, 2], F32)
    g_kv = P.sb([128, 1], F32)
    gq_m = P.sb([128, 96], F32)
    gk_m = P.sb([128, 96], F32)
    gq_n = P.sb([128, 64], F32)
    gk_n = P.sb([128, 2, 64], F32)
    ropem = P.sb([128, NT, 32], F32)
    ropen = P.sb([128, NT, 16], F32)
    fw.dma(sp, g_mix[:], dr["mix_norm"].rearrange("(c p) -> p c", p=128), [], [parB], parS)
    fw.dma(sp, g_q[:], dr["mla_q_norm"].rearrange("(c p) -> p c", p=128), [], [parB], parS)
    fw.dma(sp, g_kv[:], dr["mla_kv_norm"].rearrange("(c p) -> p c", p=128), [], [parB], parS)
    fw.dma(sp, gq_m[:], bcast_row(dr["mla_qk_norm"][0:1, :], 96), [], [parB], parS)
    fw.dma(sp, gk_m[:], bcast_row(dr["mla_qk_norm"][1:2, :], 96), [], [parB], parS)
    fw.dma(sp, gq_n[:], bcast_row(dr["nsa_q_norm"].rearrange("(o n) -> o n", o=1), 64), [], [parB], parS)
    fw.dma(sp, gk_n[:, 0, :], bcast_row(dr["nsa_k_norm"][1:2, :], 64), [], [parB], parS)
    fw.dma(sp, gk_n[:, 1, :], bcast_row(dr["nsa_k_norm"][2:3, :], 64), [], [parB], parS)
    fw.dma(sp, ropem[:], dr["rope_m"].rearrange("(t p) n -> p t n", p=128), [], [parB], parS)
    fw.dma(sp, ropen[:], dr["rope_n"].rearrange("(t p) n -> p t n", p=128), [], [parB], parS)
    P.ts(dve, gq_m[:], gq_m[:], 96 ** -0.5, ALU.mult, [parB], [parB])
    P.ts(dve, gq_n[:], gq_n[:], 64 ** -0.5, ALU.mult, [parB], [parB])
    w_in = P.sb([128, 8, IN_COLS], BF16, "w_in")
    w_uq = P.sb([128, 2, 768], BF16, "w_uq")
    w_ukv = P.sb([128, 1, 1024], BF16, "w_ukv")
    wB = Buf()
    P.load_w(w_in, wB, dr["w_in"], g_mix, stage=stage)
    P.load_w(w_uq, wB, dr["mla_w_uq"], g_q, stage=stage)
    P.load_w(w_ukv, wB, dr["mla_w_ukv"], g_kv, stage=stage)

    def wset(shape, dt, name):
        return [(P.sb(shape, dt, name), Buf(), P.slot()) for _ in range(2)]

    xts = wset([128, D], F32, "xt")
    junks = wset([128, D], BF16, "junk")
    stts = wset([128, 80], F32, "stt")
    xns = wset([128, D], BF16, "xn")
    nTs = wset([128, 8, 128], BF16, "nT")
    cqns = wset([128, 384], BF16, "cqn")
    kpes = wset([128, 32], F32, "kpe")
    cTs = wset([128, 3, 128], BF16, "cT")
    qms = wset([128, 8, 96], F32, "qm")
    kvms = wset([128, 8, 128], F32, "kvm")
    sqs = wset([128, D], F32, "sq")
    tmps = wset([128, 4, 8, 16], F32, "tmp")
    qbs = wset([128, 8, 96], BF16, "qb")
    kbs = wset([128, 8, 96], BF16, "kb")
    kps = wset([128, 8, 32], F32, "kp")
    vbs = wset([128, 8, 65], BF16, "vb")
    qTs = wset([96, 8, 128], BF16, "qT")
    kTs = wset([96, 8, 128], BF16, "kT")
    qns = wset([128, 8, 64], F32, "qn")
    qnbs = wset([128, 8, 64], BF16, "qnb")
    qnTs = wset([64, 8, 128], BF16, "qnT")
    c1s = wset([128, 512], F32, "c1")
    kkns = wset([128, 4, 64], F32, "kkn")
    kkbs = wset([128, 4, 64], BF16, "kkb")
    kkTs = wset([64, 4, 128], BF16, "kkT")
    vsws = wset([128, 2, 2, 65], BF16, "vsw")
    gts = wset([128, 24], F32, "gt")
    kvTs = wset([128, 256], BF16, "kvT")
    xS = [[P.slot() for _ in range(3)] for _ in range(2)]
    for s in range(2):
        fw.op(pool, lambda h, s=s: h.memset(vbs[s][0][:], 1.0), [], [vbs[s][1]])
        fw.op(pool, lambda h, s=s: h.memset(vsws[s][0][:], 1.0), [], [vsws[s][1]])

    def rope(src3, dst3, o1, o2, hw, cos, sin, tmp, tmpB, srcB, dstB, nh):
        x1 = src3[:, :, o1:o1 + hw]
        x2 = src3[:, :, o2:o2 + hw]
        c = cos.unsqueeze(1).to_broadcast([128, nh, hw])
        sn = sin.unsqueeze(1).to_broadcast([128, nh, hw])
        t = [tmp[:, j, 0:nh, 0:hw] for j in range(4)]
        P.tt(dve, t[0], x1, c, ALU.mult, [srcB, parB], [tmpB])
        P.tt(dve, t[1], x2, sn, ALU.mult, [srcB, parB], [tmpB])
        P.tt(dve, t[2], x2, c, ALU.mult, [srcB, parB], [tmpB])
        P.tt(dve, t[3], x1, sn, ALU.mult, [srcB, parB], [tmpB])
        P.tt(dve, dst3[:, :, o1:o1 + hw], t[0], t[1], ALU.subtract, [tmpB], [dstB])
        P.tt(dve, dst3[:, :, o2:o2 + hw], t[2], t[3], ALU.add, [tmpB], [dstB])

    cnt = 0
    for b in range(NSEQ):
        for i in range(NT):
            s = cnt % 2
            cnt += 1
            rows = slice(i * 128, (i + 1) * 128)
            xt, xtB, xtS = xts[s]
            junk, junkB, _ = junks[s]
            stt, sttB, _ = stts[s]
            xn, xnB, _ = xns[s]
            nT, nTB, _ = nTs[s]
            sq, sqB, _ = sqs[s]
            tmp, tmpB, _ = tmps[s]
            fw.dma(sp, xt[:], dr["x"][b, rows, :], [], [xtB], xtS)
            P.act(junk[:], xt[:], AF.Square, [xtB], [junkB, sttB], accum=stt[:, 0:1])
            P.rstd(stt[:, 1:2], stt[:, 0:1], D, [sttB], [sttB])
            P.act(xn[:], xt[:], AF.Copy, [xtB, sttB], [xnB], scale=stt[:, 1:2])
            bt, btB = nextbank(k)
            btv = bt[:].bitcast(BF16)
            for c in range(8):
                P.tr(btv[:, c * 128:(c + 1) * 128], xn[:, c * 128:(c + 1) * 128], [xnB], [btB])
            P.cp(dve, nT[:].rearrange("p c t -> p (c t)"), btv[:, 0:1024], [btB], [nTB])
            bA, bAB = nextbank(k)
            for c in range(8):
                P.mm(bA[:, 0:416], nT[:, c, :], w_in[:, c, 0:416], c == 0, c == 7, [nTB, wB], [bAB])
            bB, bBB = nextbank(k)
            for c in range(8):
                P.mm(bB[:, 0:512], nT[:, c, :], w_in[:, c, 416:928], c == 0, c == 7, [nTB, wB], [bBB])
            bC, bCB = nextbank(k)
            for c in range(8):
                P.mm(bC[:, 0:512], nT[:, c, :], w_in[:, c, 1184:1696], c == 0, c == 7, [nTB, wB], [bCB])
            bG, bGB = nextbank(k)
            for c in range(8):
                P.mm(bG[:, 0:24], nT[:, c, :], w_in[:, c, 1696:1720], c == 0, c == 7, [nTB, wB], [bGB])
            for j in range(2):
                for c in range(8):
                    P.mm(bG[:, 32 + j * 128:160 + j * 128], w_in[:, c, 928 + j * 128:1056 + j * 128], nT[:, c, :],
                         c == 0, c == 7, [nTB, wB], [bGB])
            qn, qnB, _ = qns[s]
            qnb, qnbB, _ = qnbs[s]
            qnT, qnTB, qnTS = qnTs[s]
            bB3 = bB[:, 0:512].rearrange("p (h d) -> p h d", h=8)
            P.act(sq[:, 0:512], bB[:, 0:512], AF.Square, [bBB], [sqB])
            P.red(stt[:, 48:56], sq[:, 0:512].rearrange("p (h d) -> p h d", h=8), [sqB], [sttB])
            P.rstd(stt[:, 56:64], stt[:, 48:56], 64, [sttB], [sttB])
            P.tt(dve, qn[:], bB3, stt[:, 56:64].unsqueeze(2).to_broadcast([128, 8, 64]), ALU.mult, [bBB, sttB], [qnB])
            P.tt(dve, qn[:], qn[:], gq_n[:, :].unsqueeze(1).to_broadcast([128, 8, 64]), ALU.mult, [qnB, parB], [qnB])
            rope(qn, qnb, 0, 8, 8, ropen[:, i, 0:8], ropen[:, i, 8:16], tmp, tmpB, qnB, qnbB, 8)
            P.cp(act, qnb[:, :, 16:64], qn[:, :, 16:64], [qnB], [qnbB])
            bq, bqB = nextbank(k)
            bqv = bq[:].bitcast(BF16)
            for h in range(8):
                P.tr(bqv[0:64, h * 128:(h + 1) * 128], qnb[:, h, :], [qnbB], [bqB])
            P.cp(act, qnT[:].rearrange("p h t -> p (h t)"), bqv[0:64, 0:1024], [bqB], [qnTB])
            fw.dma(sp, sc["QTn"][b, :, :, rows], qnT[:], [qnTB], [], qnTS)
            c1, c1B, _ = c1s[s]
            kkn, kknB, _ = kkns[s]
            kkb, kkbB, _ = kkbs[s]
            kkT, kkTB, kkTS = kkTs[s]
            vsw, vswB, vswS = vsws[s]
            P.cp(act, c1[:], bC[:, 0:512], [bCB], [c1B])
            kk4 = c1[:].rearrange("p (a r) -> p a r", a=2)[:, :, 0:128].rearrange("p a (g d) -> p a g d", g=2)
            sq4 = sq[:, 0:256].rearrange("p (a g d) -> p a g d", a=2, g=2)
            P.tt(dve, sq4, kk4, kk4, ALU.mult, [c1B], [sqB])
            P.red(stt[:, 64:68], sq[:, 0:256].rearrange("p (j d) -> p j d", j=4), [sqB], [sttB])
            P.rstd(stt[:, 68:72], stt[:, 64:68], 64, [sttB], [sttB])
            kkn4 = kkn[:].rearrange("p (a g) d -> p a g d", a=2)
            P.tt(dve, kkn4, kk4, stt[:, 68:72].rearrange("p (a g) -> p a g", a=2).unsqueeze(3).to_broadcast([128, 2, 2, 64]),
                 ALU.mult, [c1B, sttB], [kknB])
            P.tt(dve, kkn4, kkn4, gk_n[:, :, :].unsqueeze(2).to_broadcast([128, 2, 2, 64]), ALU.mult, [kknB, parB], [kknB])
            rope(kkn, kkb, 0, 8, 8, ropen[:, i, 0:8], ropen[:, i, 8:16], tmp, tmpB, kknB, kkbB, 4)
            P.cp(act, kkb[:, :, 16:64], kkn[:, :, 16:64], [kknB], [kkbB])
            bq, bqB = nextbank(k)
            bqv = bq[:].bitcast(BF16)
            for j in range(4):
                P.tr(bqv[0:64, j * 128:(j + 1) * 128], kkb[:, j, :], [kkbB], [bqB])
            P.cp(act, kkT[:].rearrange("p h t -> p (h t)"), bqv[0:64, 0:512], [bqB], [kkTB])
            fw.dma(sp, sc["KsT"][b, :, :, rows], kkT[:, 0:2, :], [kkTB], [], kkTS)
            fw.dma(sp, sc["KwT"][b, :, :, rows], kkT[:, 2:4, :], [kkTB], [], xS[s][0])
            P.cp(act, vsw[:, 0, :, 0:64], c1[:, 128:256].rearrange("p (g d) -> p g d", g=2), [c1B], [vswB])
            P.cp(act, vsw[:, 1, :, 0:64], c1[:, 384:512].rearrange("p (g d) -> p g d", g=2), [c1B], [vswB])
            fw.dma(sp, sc["Vs"][b, rows], vsw[:, 0], [vswB], [], vswS)
            fw.dma(sp, sc["Vw"][b, rows], vsw[:, 1], [vswB], [], xS[s][1])
            gt, gtB, gtS = gts[s]
            kvT, kvTB, kvTS = kvTs[s]
            P.act(gt[:], bG[:, 0:24], AF.Sigmoid, [bGB], [gtB])
            fw.dma(sp, sc["gate"][b, rows], gt[:], [gtB], [], gtS)
            P.cp(dve, kvT[:], bG[:, 32:288], [bGB], [kvTB])
            fw.dma(sp, sc["kcT"][b, :, rows], kvT[:, 0:128], [kvTB], [], kvTS)
            fw.dma(sp, sc["vcT"][b, :, rows], kvT[:, 128:256], [kvTB], [], xS[s][2])
            cqn, cqnB, _ = cqns[s]
            kpe, kpeB, _ = kpes[s]
            cT, cTB, _ = cTs[s]
            P.act(junk[:, 0:256], bA[:, 0:256], AF.Square, [bAB], [junkB, sttB], accum=stt[:, 2:3])
            P.act(junk[:, 0:128], bA[:, 256:384], AF.Square, [bAB], [junkB, sttB], accum=stt[:, 3:4])
            P.rstd(stt[:, 4:5], stt[:, 2:3], 256, [sttB], [sttB])
            P.rstd(stt[:, 5:6], stt[:, 3:4], 128, [sttB], [sttB])
            P.act(cqn[:, 0:256], bA[:, 0:256], AF.Copy, [bAB, sttB], [cqnB], scale=stt[:, 4:5])
            P.act(cqn[:, 256:384], bA[:, 256:384], AF.Copy, [bAB, sttB], [cqnB], scale=stt[:, 5:6])
            P.cp(dve, kpe[:], bA[:, 384:416], [bAB], [kpeB])
            bt2, bt2B = nextbank(k)
            bt2v = bt2[:].bitcast(BF16)
            for c in range(3):
                P.tr(bt2v[:, c * 128:(c + 1) * 128], cqn[:, c * 128:(c + 1) * 128], [cqnB], [bt2B])
            P.cp(dve, cT[:].rearrange("p c t -> p (c t)"), bt2v[:, 0:384], [bt2B], [cTB])
            qm, qmB, _ = qms[s]
            kvm, kvmB, _ = kvms[s]
            qmf = qm[:].rearrange("p h d -> p (h d)")
            kvf = kvm[:].rearrange("p h d -> p (h d)")
            bQ1, bQ1B = nextbank(k)
            for c in range(2):
                P.mm(bQ1[:, 0:512], cT[:, c, :], w_uq[:, c, 0:512], c == 0, c == 1, [cTB, wB], [bQ1B])
            bQ2, bQ2B = nextbank(k)
            for c in range(2):
                P.mm(bQ2[:, 0:256], cT[:, c, :], w_uq[:, c, 512:768], c == 0, c == 1, [cTB, wB], [bQ2B])
            P.cp(act, qmf[:, 0:512], bQ1[:, 0:512], [bQ1B], [qmB])
            P.cp(act, qmf[:, 512:768], bQ2[:, 0:256], [bQ2B], [qmB])
            bK1, bK1B = nextbank(k)
            P.mm(bK1[:, 0:512], cT[:, 2, :], w_ukv[:, 0, 0:512], True, True, [cTB, wB], [bK1B])
            bK2, bK2B = nextbank(k)
            P.mm(bK2[:, 0:512], cT[:, 2, :], w_ukv[:, 0, 512:1024], True, True, [cTB, wB], [bK2B])
            P.cp(act, kvf[:, 0:512], bK1[:, 0:512], [bK1B], [kvmB])
            P.cp(dve, kvf[:, 512:1024], bK2[:, 0:512], [bK2B], [kvmB])
            qb, qbB, _ = qbs[s]
            sq768 = sq[:, 0:768]
            P.tt(dve, sq768, qmf, qmf, ALU.mult, [qmB], [sqB])
            P.red(stt[:, 8:16], sq768.rearrange("p (h d) -> p h d", h=8), [sqB], [sttB])
            P.rstd(stt[:, 16:24], stt[:, 8:16], 96, [sttB], [sttB])
            P.tt(dve, qm[:], qm[:], stt[:, 16:24].unsqueeze(2).to_broadcast([128, 8, 96]), ALU.mult, [qmB, sttB], [qmB])
            P.tt(dve, qm[:], qm[:], gq_m[:, :].unsqueeze(1).to_broadcast([128, 8, 96]), ALU.mult, [qmB, parB], [qmB])
            rope(qm, qb, 64, 80, 16, ropem[:, i, 0:16], ropem[:, i, 16:32], tmp, tmpB, qmB, qbB, 8)
            P.cp(act, qb[:, :, 0:64], qm[:, :, 0:64], [qmB], [qbB])
            kb, kbB, _ = kbs[s]
            kp, kpB, _ = kps[s]
            vb, vbB, vbS = vbs[s]
            sq512 = sq[:, 0:512].rearrange("p (h d) -> p h d", h=8)
            P.tt(dve, sq512, kvm[:, :, 0:64], kvm[:, :, 0:64], ALU.mult, [kvmB], [sqB])
            P.red(stt[:, 24:32], sq512, [sqB], [sttB])
            P.act(junk[:, 0:32], kpe[:], AF.Square, [kpeB], [junkB, sttB], accum=stt[:, 32:33])
            P.ts(dve, stt[:, 24:32], stt[:, 24:32], stt[:, 32:33], ALU.add, [sttB], [sttB])
            P.rstd(stt[:, 40:48], stt[:, 24:32], 96, [sttB], [sttB])
            P.tt(dve, kvm[:, :, 0:64], kvm[:, :, 0:64], stt[:, 40:48].unsqueeze(2).to_broadcast([128, 8, 64]),
                 ALU.mult, [kvmB, sttB], [kvmB])
            P.tt(dve, kb[:, :, 0:64], kvm[:, :, 0:64], gk_m[:, 0:64].unsqueeze(1).to_broadcast([128, 8, 64]),
                 ALU.mult, [kvmB, parB], [kbB])
            P.tt(dve, kp[:], kpe[:, :].unsqueeze(1).to_broadcast([128, 8, 32]),
                 stt[:, 40:48].unsqueeze(2).to_broadcast([128, 8, 32]), ALU.mult, [kpeB, sttB], [kpB])
            P.tt(dve, kp[:], kp[:], gk_m[:, 64:96].unsqueeze(1).to_broadcast([128, 8, 32]), ALU.mult, [kpB, parB], [kpB])
            x1 = kp[:, :, 0:16]
            x2 = kp[:, :, 16:32]
            cc = ropem[:, i, 0:16].unsqueeze(1).to_broadcast([128, 8, 16])
            sn = ropem[:, i, 16:32].unsqueeze(1).to_broadcast([128, 8, 16])
            t = [tmp[:, j, 0:8, 0:16] for j in range(4)]
            P.tt(dve, t[0], x1, cc, ALU.mult, [kpB, parB], [tmpB])
            P.tt(dve, t[1], x2, sn, ALU.mult, [kpB, parB], [tmpB])
            P.tt(dve, t[2], x2, cc, ALU.mult, [kpB, parB], [tmpB])
            P.tt(dve, t[3], x1, sn, ALU.mult, [kpB, parB], [tmpB])
            P.tt(dve, kb[:, :, 64:80], t[0], t[1], ALU.subtract, [tmpB], [kbB])
            P.tt(dve, kb[:, :, 80:96], t[2], t[3], ALU.add, [tmpB], [kbB])
            P.cp(act, vb[:, :, 0:64], kvm[:, :, 64:128], [kvmB], [vbB])
            fw.dma(sp, sc["Vm"][b, rows], vb[:], [vbB], [], vbS)
            qT, qTB, qTS = qTs[s]
            kT, kTB, kTS = kTs[s]
            for (src, srcB, dst, dstB, dstS, name) in ((qb, qbB, qT, qTB, qTS, "QTm"), (kb, kbB, kT, kTB, kTS, "KTm")):
                bq, bqB = nextbank(k)
                bqv = bq[:].bitcast(BF16)
                for h in range(8):
                    P.tr(bqv[0:96, h * 128:(h + 1) * 128], src[:, h, :], [srcB], [bqB])
                P.cp(act, dst[:].rearrange("p h t -> p (h t)"), bqv[0:96, 0:1024], [bqB], [dstB])
                fw.dma(sp, sc[name][b, :, :, rows], dst[:], [dstB], [], dstS)
    P.close()


def phase2(k):
    fw, dr, sc, scB = k.fw, k.dr, k.sc, k.scB
    S, NSEQ, NT = k.S, k.NSEQ, k.NT
    P = Ph(k, "p2")
    dve, act, pool, sp, pe = fw.dve, fw.act, fw.pool, fw.sp, fw.pe
    GQ = min(512, S)
    TG = GQ // 128
    NG = S // GQ
    KT = P.sb([96, 8, S], BF16, "KT")
    V = P.sb([128, NT, 8, 65], BF16, "V")
    KTB, VB = Buf(), Buf()
    kvS = P.slot()
    QTs = [(P.sb([96, 8, GQ], BF16, "QT"), Buf(), P.slot()) for _ in range(2)]
    PTs = [(P.sb([128, GQ], BF16, "PT"), Buf()) for _ in range(3)]
    os_ = [(P.sb([128, TG, 512], F32, "o"), Buf()) for _ in range(2)]
    obs = [(P.sb([128, 512], BF16, "ob"), Buf(), P.slot()) for _ in range(2)]
    junk = P.sb([128, 512], BF16, "junk")
    junkB = Buf()
    stt = P.sb([128, 16], F32, "stt")
    sttB = Buf()
    rc = P.sb([128, 32], F32, "rc")
    rcB = [Buf() for _ in range(32)]
    pv = k.banks[0:4]
    scb = k.banks[4:8]
    nsc = 0
    npt = 0
    nob = 0
    gi = 0
    for b in range(NSEQ):
        for h in range(8):
            fw.dma(sp, KT[:, h, :], sc["KTm"][b, :, h, :], [scB["KTm"]], [KTB], kvS)
        vsrc = sc["Vm"][b].rearrange("(t p) h e -> p t h e", p=128)
        for t0 in range(0, NT, 8):
            t1 = min(NT, t0 + 8)
            fw.dma(sp, V[:, t0:t1], vsrc[:, t0:t1], [scB["Vm"]], [VB], kvS)
        for g in range(NG):
            QT, QTB, QTS = QTs[gi % 2]
            o, oB = os_[gi % 2]
            gi += 1
            fw.dma(sp, QT[:], sc["QTm"][b, :, :, g * GQ:(g + 1) * GQ], [scB["QTm"]], [QTB], QTS)
            for h in range(8):
                nk = (g + 1) * TG
                for kt in range(nk):
                    j0 = max(0, kt - g * TG)
                    bs, bsB = scb[nsc % 4]
                    nsc += 1
                    PT, PTB = PTs[npt % 3]
                    npt += 1
                    P.mm(bs[:, j0 * 128:GQ], KT[:, h, kt * 128:(kt + 1) * 128], QT[:, h, j0 * 128:GQ], True, True,
                         [KTB, QTB], [bsB])
                    P.act(PT[:, j0 * 128:GQ], bs[:, j0 * 128:GQ], AF.Exp, [bsB], [PTB])
                    if kt >= g * TG:
                        dsl = PT[:, j0 * 128:(j0 + 1) * 128]
                        fw.op(pool, lambda hh, dsl=dsl: hh.affine_select(out=dsl, in_=dsl, pattern=[[1, 128]],
                                                                         compare_op=ALU.is_ge, fill=0.0, base=0,
                                                                         channel_multiplier=-1), [PTB], [PTB])
                    for j in range(j0, TG):
                        bo, boB = pv[j]
                        P.mm(bo[:, 0:65], PT[:, j * 128:(j + 1) * 128], V[:, kt, h, :], kt == 0, kt == g * TG + j,
                             [PTB, VB], [boB])
                for j in range(TG):
                    bo, boB = pv[j]
                    c = (h * TG + j) % 32
                    fw.op(dve, lambda hh, bo=bo, c=c: hh.reciprocal(out=rc[:, c:c + 1], in_=bo[:, 64:65]), [boB], [rcB[c]])
                    P.act(o[:, j, h * 64:(h + 1) * 64], bo[:, 0:64], AF.Copy, [boB, rcB[c]], [oB], scale=rc[:, c:c + 1])
            for j in range(TG):
                ob, obB, obS = obs[nob % 2]
                nob += 1
                rows = slice((g * TG + j) * 128, (g * TG + j + 1) * 128)
                P.act(junk[:], o[:, j, :], AF.Square, [oB], [junkB, sttB], accum=stt[:, 0:1])
                P.rstd(stt[:, 1:2], stt[:, 0:1], 512, [sttB], [sttB])
                P.act(ob[:], o[:, j, :], AF.Copy, [oB, sttB], [obB], scale=stt[:, 1:2])
                fw.dma(sp, sc["mixed"][b, rows, 0:512], ob[:], [obB], [], obS)
    P.close()


NSA_BR = "csw"
DBG_QI = 2


def phase3(k):
    fw, dr, sc, scB = k.fw, k.dr, k.sc, k.scB
    S, NSEQ, NT, NBLK, NCMP, NCP = k.S, k.NSEQ, k.NT, k.NBLK, k.NCMP, k.NCP
    P = Ph(k, "p3")
    dve, act, pool, sp, pe = fw.dve, fw.act, fw.pool, fw.sp, fw.pe
    NNT = NCP // 128
    W = 65 + NBLK
    GC = 0.7978845608028654
    cB = Buf()
    cS = P.slot()
    stB = Buf()
    stS = P.slot()
    stage = P.sb([128, 4096], F32, "stage")
    w1 = P.sb([128, 2, 32, 128], BF16, "w1")
    for kv in range(2):
        src = dr["nsa_cmp_w1"][kv].rearrange("(l d) n -> d l n", d=64)
        for half in range(2):
            fw.dma(sp, stage[half * 64:(half + 1) * 64, :].rearrange("p (l n) -> p l n", l=32), src, [], [stB], stS)
        P.cp(dve, w1[:, kv].rearrange("p l n -> p (l n)"), stage[:, 0:4096], [stB], [cB])
    pst = P.sb([64, 2, 32], F32, "pst")
    peT = P.sb([64, 2, 32], BF16, "peT")
    fw.dma(sp, pst[:], dr["nsa_cmp_pe"].rearrange("k l d -> d k l"), [], [cB], cS)
    P.cp(dve, peT[:], pst[:], [cB], [cB])
    w2s = P.sb([128, 2, 64], F32, "w2s")
    w2 = P.sb([128, 2, 64], BF16, "w2")
    fw.dma(sp, w2s[:], dr["nsa_cmp_w2"].rearrange("k h d -> h k d"), [], [cB], cS)
    P.cp(dve, w2[:], w2s[:], [cB], [cB])
    gk_c = P.sb([128, 64], F32, "gk_c")
    fw.dma(sp, gk_c[:], bcast_row(dr["nsa_k_norm"][0:1, :], 64), [], [cB], cS)
    ropec = P.sb([128, NNT, 16], F32, "ropec")
    fw.dma(sp, ropec[:], dr["rope_c"].rearrange("(t p) n -> p t n", p=128), [], [cB], cS)
    ovs = P.sb([128, NNT, NBLK], F32, "ovs")
    fw.dma(sp, ovs[:], dr["overlap"].rearrange("(t p) n -> p t n", p=128), [], [cB], cS)
    selmul = P.sb([128, NT, NBLK], F32, "selmul")
    seladd = P.sb([128, NT, NBLK], F32, "seladd")
    fw.dma(sp, selmul[:], dr["selmul"].rearrange("(t p) n -> p t n", p=128), [], [cB], cS)
    fw.dma(sp, seladd[:], dr["seladd"].rearrange("(t p) n -> p t n", p=128), [], [cB], cS)
    ems = P.sb([NBLK, NT * 128], F32, "ems")
    emat = P.sb([NBLK, NT, 128], BF16, "emat")
    fw.dma(sp, ems[:], dr["emat"].rearrange("b t k -> b (t k)"), [], [cB], cS)
    P.cp(dve, emat[:].rearrange("b t k -> b (t k)"), ems[:], [cB], [cB])
    cbias = P.sb([128, 2], F32, "cbias")
    for kv in range(2):
        bb, bbB = nextbank(k)
        for l in range(32):
            P.mm(bb[:, 0:1], w1[0:64, kv, l, :], peT[:, kv, l:l + 1], l == 0, l == 31, [cB], [bbB])
        P.cp(dve, cbias[:, kv:kv + 1], bb[:, 0:1], [bbB], [cB])
    xT = [P.sb([128, S], BF16, "kcT_in"), P.sb([128, S], BF16, "vcT_in")]
    xTB = Buf()
    KcT = P.sb([64, 2, NCP], BF16, "KcT")
    VcOv = P.sb([128, NNT, 2, W], BF16, "VcOv")
    hT = P.sb([128, NCP], BF16, "hT")
    cmpB = Buf()
    KsT = P.sb([64, 2, S], BF16, "KsT")
    KwT = P.sb([64, 2, S], BF16, "KwT")
    Vs = P.sb([128, NT, 2, 65], BF16, "Vs")
    Vw = P.sb([128, NT, 2, 65], BF16, "Vw")
    gates = P.sb([128, NT, 24], F32, "gates")
    seqB = Buf()
    seqS = P.slot()
    xs = P.sb([128, NCP], F32, "xs")
    x2 = P.sb([128, NCP], F32, "x2")
    wkB = Buf()
    kc_f = P.sb([128, 64], F32, "kc_f")
    kc_b = P.sb([128, 64], BF16, "kc_b")
    ctmp = P.sb([128, 4, 1, 8], F32, "ctmp")
    cst = P.sb([128, 8], F32, "cst")
    junk = P.sb([128, 512], BF16, "junk")
    junkB = Buf()
    QTs = [(P.sb([64, 8, 128], BF16, "QTt"), Buf(), P.slot()) for _ in range(2)]
    PTs = [(P.sb([128, 4, 128], BF16, "PT"), Buf()) for _ in range(3)]
    onsas = [(P.sb([128, 8, 64], F32, "onsa"), Buf()) for _ in range(2)]
    obs = [(P.sb([128, 512], BF16, "ob"), Buf(), P.slot()) for _ in range(2)]
    imp = P.sb([128, NBLK], F32, "imp")
    score = P.sb([128, NBLK], F32, "score")
    score2 = P.sb([128, NBLK], F32, "score2")
    m8 = P.sb([128, 16], F32, "m8")
    negb = P.sb([128, NBLK], BF16, "negb")
    negT = P.sb([NBLK, 4, 128], BF16, "negT")
    selB = Buf()
    negTB = Buf()
    rc = P.sb([128, 16], F32, "rc")
    rcB = Buf()
    stt = P.sb([128, 4], F32, "stt")
    sttB = Buf()
    acc = k.banks[0:4]
    scb = k.banks[4:8]
    st_ = {"nsc": 0, "npt": 0}

    def unit(lhsT, lhsB, Qr, QB, mask_mm, sel_fn, Vrhs, VB, first, last):
        bs, bsB = scb[st_["nsc"] % 4]
        st_["nsc"] += 1
        PT, PTB = PTs[st_["npt"] % 3]
        st_["npt"] += 1
        P.mm(bs[:, 0:512], lhsT, Qr, True, mask_mm is None, lhsB + [QB], [bsB])
        if mask_mm is not None:
            P.mm(bs[:, 0:512], mask_mm[0], mask_mm[1], False, True, mask_mm[2], [bsB])
        P.act(PT[:].rearrange("p h q -> p (h q)"), bs[:, 0:512], AF.Exp, [bsB], [PTB])
        if sel_fn is not None:
            base, cm, step, op = sel_fn
            fw.op(pool, lambda hh, PT=PT: hh.affine_select(out=PT[:], in_=PT[:], pattern=[[0, 4], [step, 128]],
                                                         compare_op=op, fill=0.0, base=base,
                                                         channel_multiplier=cm), [PTB], [PTB])
        wv = Vrhs.shape[-1]
        for r in range(4):
            bo, boB = acc[r]
            P.mm(bo[:, 0:wv], PT[:, r, :], Vrhs, first, last, [PTB] + VB, [boB])

    nq = 0
    for b in range(NSEQ):
        fw.dma(sp, xT[0][:], sc["kcT"][b], [scB["kcT"]], [xTB], seqS)
        fw.dma(sp, xT[1][:], sc["vcT"][b], [scB["vcT"]], [xTB], seqS)
        for g in range(2):
            fw.dma(sp, KsT[:, g, :], sc["KsT"][b, :, g, :], [scB["KsT"]], [seqB], seqS)
            fw.dma(sp, KwT[:, g, :], sc["KwT"][b, :, g, :], [scB["KwT"]], [seqB], seqS)
        fw.dma(sp, Vs[:], sc["Vs"][b].rearrange("(t p) g e -> p t g e", p=128), [scB["Vs"]], [seqB], seqS)
        fw.dma(sp, Vw[:], sc["Vw"][b].rearrange("(t p) g e -> p t g e", p=128), [scB["Vw"]], [seqB], seqS)
        fw.dma(sp, gates[:], sc["gate"][b].rearrange("(t p) n -> p t n", p=128), [scB["gate"]], [seqB], seqS)
        fw.op(pool, lambda hh: hh.memset(KcT[:], 0.0), [], [cmpB])
        fw.op(pool, lambda hh: hh.memset(VcOv[:], 0.0), [], [cmpB])
        for nt in range(NNT):
            for g in range(2):
                P.cp(dve, VcOv[:, nt, g, 65:W], ovs[:, nt, :], [cB], [cmpB])
                fw.op(pool, lambda hh, nt=nt, g=g: hh.memset(VcOv[:, nt, g, 64:65], 1.0), [cmpB], [cmpB])
        for kv in range(2):
            for g in range(2):
                fw.op(pool, lambda hh: hh.memset(hT[:], 0.0), [wkB], [wkB])
                bh, bhB = nextbank(k)
                for l in range(32):
                    P.mm(bh[:, 0:NCMP], w1[g * 64:(g + 1) * 64, kv, l, :],
                         xT[kv][g * 64:(g + 1) * 64, l:l + 16 * (NCMP - 1) + 1:16], l == 0, l == 31, [cB, xTB], [bhB])
                P.act(xs[:, 0:NCMP], bh[:, 0:NCMP], AF.Identity, [bhB, cB], [wkB], bias=cbias[:, kv:kv + 1])
                P.tt(dve, x2[:, 0:NCMP], xs[:, 0:NCMP], xs[:, 0:NCMP], ALU.mult, [wkB], [wkB])
                P.ts(dve, x2[:, 0:NCMP], x2[:, 0:NCMP], 0.044715, ALU.mult, [wkB], [wkB], s2=1.0, op1=ALU.add)
                P.tt(dve, x2[:, 0:NCMP], x2[:, 0:NCMP], xs[:, 0:NCMP], ALU.mult, [wkB], [wkB])
                P.act(x2[:, 0:NCMP], x2[:, 0:NCMP], AF.Sigmoid, [wkB], [wkB], scale=2.0 * GC)
                P.tt(dve, hT[:, 0:NCMP], x2[:, 0:NCMP], xs[:, 0:NCMP], ALU.mult, [wkB], [wkB])
                for nt in range(NNT):
                    bo, boB = nextbank(k)
                    P.mm(bo[:, 0:64], hT[:, nt * 128:(nt + 1) * 128], w2[:, kv, :], True, True, [wkB, cB], [boB])
                    if kv == 1:
                        P.cp(dve, VcOv[:, nt, g, 0:64], bo[:, 0:64], [boB], [cmpB])
                        continue
                    P.act(junk[:, 0:64], bo[:, 0:64], AF.Square, [boB], [junkB, wkB], accum=cst[:, 0:1])
                    P.rstd(cst[:, 1:2], cst[:, 0:1], 64, [wkB], [wkB])
                    P.ts(dve, kc_f[:], bo[:, 0:64], cst[:, 1:2], ALU.mult, [boB, wkB], [wkB])
                    P.tt(dve, kc_f[:], kc_f[:], gk_c[:], ALU.mult, [wkB, cB], [wkB])
                    x1 = kc_f[:, 0:8]
                    x2_ = kc_f[:, 8:16]
                    cc = ropec[:, nt, 0:8]
                    sn = ropec[:, nt, 8:16]
                    t = [ctmp[:, j, 0, :] for j in range(4)]
                    P.tt(dve, t[0], x1, cc, ALU.mult, [wkB, cB], [wkB])
                    P.tt(dve, t[1], x2_, sn, ALU.mult, [wkB, cB], [wkB])
                    P.tt(dve, t[2], x2_, cc, ALU.mult, [wkB, cB], [wkB])
                    P.tt(dve, t[3], x1, sn, ALU.mult, [wkB, cB], [wkB])
                    P.tt(dve, kc_b[:, 0:8], t[0], t[1], ALU.subtract, [wkB], [wkB])
                    P.tt(dve, kc_b[:, 8:16], t[2], t[3], ALU.add, [wkB], [wkB])
                    P.cp(dve, kc_b[:, 16:64], kc_f[:, 16:64], [wkB], [wkB])
                    bt, btB = nextbank(k)
                    btv = bt[:].bitcast(BF16)
                    P.tr(btv[0:64, 0:128], kc_b[:], [wkB], [btB])
                    P.cp(dve, KcT[:, g, nt * 128:(nt + 1) * 128], btv[0:64, 0:128], [btB], [cmpB])
        for qi in range(NT):
            QTt, QB, QS = QTs[nq % 2]
            onsa, onB = onsas[nq % 2]
            ob, obB, obS = obs[nq % 2]
            nq += 1
            rows = slice(qi * 128, (qi + 1) * 128)
            fw.dma(sp, QTt[:], sc["QTn"][b, :, :, rows], [scB["QTn"]], [QB], QS)
            for g in range(2):
                Qr = QTt[:, g * 4:(g + 1) * 4, :]
                nts = [nt for nt in range(NNT) if 16 * 128 * nt + 31 <= 128 * qi + 127]
                for ii, nt in enumerate(nts):
                    full = 16 * (128 * nt + 127) + 31 <= 128 * qi
                    sel_fn = None if full else (128 * qi - 2048 * nt - 31, -16, 1, ALU.is_ge)
                    unit(KcT[:, g, nt * 128:(nt + 1) * 128], [cmpB], Qr, QB, None, sel_fn,
                         VcOv[:, nt, g, :], [cmpB], ii == 0, ii == len(nts) - 1)
                for r in range(4):
                    h = g * 4 + r
                    bo, boB = acc[r]
                    P.ts(dve, rc[:, r:r + 1], bo[:, 64:65], 1e-30, ALU.max, [boB], [rcB])
                    fw.op(dve, lambda hh, r=r: hh.reciprocal(out=rc[:, r:r + 1], in_=rc[:, r:r + 1]), [rcB], [rcB])
                    if r == 0:
                        P.ts(dve, imp[:], bo[:, 65:W], rc[:, r:r + 1], ALU.mult, [boB, rcB], [selB])
                    else:
                        fw.op(dve, lambda hh, bo=bo, r=r: hh.scalar_tensor_tensor(
                            out=imp[:], in0=bo[:, 65:W], scalar=rc[:, r:r + 1], in1=imp[:], op0=ALU.mult, op1=ALU.add),
                            [boB, rcB, selB], [selB])
                    P.tt(dve, rc[:, 4 + r:5 + r], rc[:, r:r + 1], gates[:, qi, h * 3:h * 3 + 1], ALU.mult, [rcB, seqB], [rcB])
                    if "c" not in NSA_BR:
                        P.ts(dve, rc[:, 4 + r:5 + r], rc[:, 4 + r:5 + r], 0.0, ALU.mult, [rcB], [rcB])
                    P.ts(dve, onsa[:, h, :], bo[:, 0:64], rc[:, 4 + r:5 + r], ALU.mult, [boB, rcB], [onB])
                P.tt(dve, score[:], imp[:], selmul[:, qi, :], ALU.mult, [selB, cB], [selB])
                P.tt(dve, score[:], score[:], seladd[:, qi, :], ALU.add, [selB, cB], [selB])
                fw.op(dve, lambda hh: hh.max(out=m8[:, 0:8], in_=score[:]), [selB], [selB])
                fw.op(dve, lambda hh: hh.match_replace(out=score2[:], in_to_replace=m8[:, 0:8], in_values=score[:],
                                                       imm_value=-3.0e38), [selB], [selB])
                fw.op(dve, lambda hh: hh.max(out=m8[:, 8:16], in_=score2[:]), [selB], [selB])
                P.ts(dve, m8[:, 15:16], m8[:, 15:16], -1.0e29, ALU.max, [selB], [selB])
                P.ts(dve, score2[:], score[:], m8[:, 15:16], ALU.is_ge, [selB], [selB])
                P.ts(dve, negb[:], score2[:], 30000.0, ALU.mult, [selB], [selB], s2=-30000.0, op1=ALU.add)
                bt, btB = nextbank_hi(k, st_)
                btv = bt[:].bitcast(BF16)
                P.tr(btv[0:NBLK, 0:128], negb[:], [selB], [btB])
                P.cp(dve, negT[:], btv[0:NBLK, 0:128].unsqueeze(1).to_broadcast([NBLK, 4, 128]), [btB], [negTB])
                for kt in range(qi + 1):
                    sel_fn = (0, -1, 1, ALU.is_ge) if kt == qi else None
                    unit(KsT[:, g, kt * 128:(kt + 1) * 128], [seqB], Qr, QB,
                         (emat[:, kt, :], negT[:], [cB, negTB]), sel_fn, Vs[:, kt, g, :], [seqB], kt == 0, kt == qi)
                for r in range(4):
                    h = g * 4 + r
                    bo, boB = acc[r]
                    fw.op(dve, lambda hh, bo=bo, r=r: hh.reciprocal(out=rc[:, 8 + r:9 + r], in_=bo[:, 64:65]), [boB], [rcB])
                    P.tt(dve, rc[:, 8 + r:9 + r], rc[:, 8 + r:9 + r], gates[:, qi, h * 3 + 1:h * 3 + 2], ALU.mult, [rcB, seqB], [rcB])
                    if "s" not in NSA_BR:
                        P.ts(dve, rc[:, 8 + r:9 + r], rc[:, 8 + r:9 + r], 0.0, ALU.mult, [rcB], [rcB])
                    fw.op(dve, lambda hh, bo=bo, r=r, h=h, onsa=onsa: hh.scalar_tensor_tensor(
                        out=onsa[:, h, :], in0=bo[:, 0:64], scalar=rc[:, 8 + r:9 + r], in1=onsa[:, h, :],
                        op0=ALU.mult, op1=ALU.add), [boB, rcB, onB], [onB])
                k0 = max(0, qi - 4)
                for kt in range(k0, qi + 1):
                    if kt == qi:
                        sel_fn = (0, -1, 1, ALU.is_ge)
                    elif kt == qi - 4:
                        sel_fn = (0, 1, -1, ALU.is_gt)
                    else:
                        sel_fn = None
                    unit(KwT[:, g, kt * 128:(kt + 1) * 128], [seqB], Qr, QB, None, sel_fn,
                         Vw[:, kt, g, :], [seqB], kt == k0, kt == qi)
                for r in range(4):
                    h = g * 4 + r
                    bo, boB = acc[r]
                    fw.op(dve, lambda hh, bo=bo, r=r: hh.reciprocal(out=rc[:, 12 + r:13 + r], in_=bo[:, 64:65]), [boB], [rcB])
                    P.tt(dve, rc[:, 12 + r:13 + r], rc[:, 12 + r:13 + r], gates[:, qi, h * 3 + 2:h * 3 + 3], ALU.mult, [rcB, seqB], [rcB])
                    if "w" not in NSA_BR:
                        P.ts(dve, rc[:, 12 + r:13 + r], rc[:, 12 + r:13 + r], 0.0, ALU.mult, [rcB], [rcB])
                    fw.op(dve, lambda hh, bo=bo, r=r, h=h, onsa=onsa: hh.scalar_tensor_tensor(
                        out=onsa[:, h, :], in0=bo[:, 0:64], scalar=rc[:, 12 + r:13 + r], in1=onsa[:, h, :],
                        op0=ALU.mult, op1=ALU.add), [boB, rcB, onB], [onB])
            of = onsa[:].rearrange("p h d -> p (h d)")
            if k.dbgt is not None and qi == DBG_QI and b == 0:
                dS = P.slot()
                fw.dma(sp, k.dbgt[:, 0:16], rc[:], [rcB], [], dS)
                fw.dma(sp, k.dbgt[:, 16:528], of, [onB], [], dS)
                fw.dma(sp, k.dbgt[:, 528:552], gates[:, qi, :], [seqB], [], dS)
            P.act(junk[:], of, AF.Square, [onB], [junkB, sttB], accum=stt[:, 0:1])
            P.rstd(stt[:, 1:2], stt[:, 0:1], 512, [sttB], [sttB])
            P.act(ob[:], of, AF.Copy, [onB, sttB], [obB], scale=stt[:, 1:2])
            fw.dma(sp, sc["mixed"][b, rows, 512:1024], ob[:], [obB], [], obS)
    P.close()


def nextbank_hi(k, st_):
    t, b = k.banks[4 + st_["nsc"] % 4]
    st_["nsc"] += 1
    return t, b


P4_MODE = "full"
PEER_DENSE = True


def phase4(k):
    fw, dr, sc, scB = k.fw, k.dr, k.sc, k.scB
    S, NSEQ, NT = k.S, k.NSEQ, k.NT
    nc = k.nc
    dve, act, pool, sp, pe = fw.dve, fw.act, fw.pool, fw.sp, fw.pe
    GC = 0.7978845608028654
    P0 = Ph(k, "p4")
    cB = Buf()
    cS = P0.slot()
    KmT = P0.sb([128, NSEQ, 4, 256], BF16, "KmT")
    Vx = P0.sb([128, NSEQ, 2, 4, 129], BF16, "Vx")
    memB = Buf()
    stage = (P0.sb([128, 1024], F32, "stage"), Buf(), P0.slot())
    gx = P0.sb([128, 128], F32, "gqx")
    gkx = P0.sb([128, 128], F32, "gkx")
    fw.dma(sp, gx[:], bcast_row(dr["xa_qk_norm"][0:1, :], 128), [], [cB], cS)
    fw.dma(sp, gkx[:], bcast_row(dr["xa_qk_norm"][1:2, :], 128), [], [cB], cS)
    P0.ts(dve, gx[:], gx[:], 128 ** -0.5, ALU.mult, [cB], [cB])
    junk = P0.sb([128, 1024], BF16, "junk")
    junkB = Buf()
    Pa = Ph(k, "p4a")
    g_mem = Pa.sb([128, 8], F32)
    fw.dma(sp, g_mem[:], dr["mem_norm"].rearrange("(c p) -> p c", p=128), [], [cB], cS)
    wkv = Pa.sb([128, 8, 1024], BF16, "wkv")
    wB = Buf()
    Pa.load_w(wkv, wB, dr["xa_wkv"], g_mem, stage=stage)
    mt_ = Pa.sb([128, 1024], F32, "mt")
    mtB = Buf()
    mS = Pa.slot()
    mn = Pa.sb([128, 1024], BF16, "mn")
    mnT = Pa.sb([128, 8, 128], BF16, "mnT")
    kvs = Pa.sb([128, 1024], F32, "kvs")
    sqm = Pa.sb([128, 512], F32, "sqm")
    kb_ = Pa.sb([128, 4, 128], BF16, "kb")
    st4 = Pa.sb([128, 16], F32, "st4")
    wk = Buf()
    fw.op(pool, lambda hh: hh.memset(Vx[:], 1.0), [], [memB])
    for b in range(NSEQ):
        for m in range(2):
            fw.dma(sp, mt_[:], dr["mem"][b, m * 128:(m + 1) * 128, :], [], [mtB], mS)
            Pa.act(junk[:], mt_[:], AF.Square, [mtB], [junkB, wk], accum=st4[:, 0:1])
            Pa.rstd(st4[:, 1:2], st4[:, 0:1], D, [wk], [wk])
            Pa.act(mn[:], mt_[:], AF.Copy, [mtB, wk], [wk], scale=st4[:, 1:2])
            bt, btB = nextbank(k)
            btv = bt[:].bitcast(BF16)
            for c in range(8):
                Pa.tr(btv[:, c * 128:(c + 1) * 128], mn[:, c * 128:(c + 1) * 128], [wk], [btB])
            Pa.cp(dve, mnT[:].rearrange("p c t -> p (c t)"), btv[:, 0:1024], [btB], [wk])
            for half in range(2):
                bk, bkB = nextbank(k)
                for c in range(8):
                    Pa.mm(bk[:, 0:512], mnT[:, c, :], wkv[:, c, half * 512:(half + 1) * 512], c == 0, c == 7, [wk, wB], [bkB])
                Pa.cp(act, kvs[:, half * 512:(half + 1) * 512], bk[:, 0:512], [bkB], [wk])
            Pa.tt(dve, sqm[:], kvs[:, 0:512], kvs[:, 0:512], ALU.mult, [wk], [wk])
            Pa.red(st4[:, 4:8], sqm[:].rearrange("p (h d) -> p h d", h=4), [wk], [wk])
            Pa.rstd(st4[:, 8:12], st4[:, 4:8], 128, [wk], [wk])
            k3 = kvs[:, 0:512].rearrange("p (h d) -> p h d", h=4)
            Pa.tt(dve, k3, k3, st4[:, 8:12].unsqueeze(2).to_broadcast([128, 4, 128]), ALU.mult, [wk], [wk])
            Pa.tt(dve, kb_[:], k3, gkx[:, :].unsqueeze(1).to_broadcast([128, 4, 128]), ALU.mult, [wk, cB], [wk])
            bt, btB = nextbank(k)
            btv = bt[:].bitcast(BF16)
            for h in range(4):
                Pa.tr(btv[:, h * 128:(h + 1) * 128], kb_[:, h, :], [wk], [btB])
            Pa.cp(dve, KmT[:, b, :, m * 128:(m + 1) * 128], btv[:, 0:512].rearrange("p (h t) -> p h t", h=4), [btB], [memB])
            Pa.cp(act, Vx[:, b, m, :, 0:128], kvs[:, 512:1024].rearrange("p (h d) -> p h d", h=4), [wk], [memB])
    Pa.close()
    P = Ph(k, "p4b")
    g_out = P.sb([128, 8], F32)
    g_xa = P.sb([128, 8], F32)
    g_ffn = P.sb([128, 8], F32)
    fw.dma(sp, g_out[:], dr["mix_out_norm"].rearrange("(c p) -> p c", p=128), [], [cB], cS)
    fw.dma(sp, g_xa[:], dr["xa_norm"].rearrange("(c p) -> p c", p=128), [], [cB], cS)
    fw.dma(sp, g_ffn[:], dr["ffn_norm"].rearrange("(c p) -> p c", p=128), [], [cB], cS)
    gf_rep = P.sb([128, 1024], F32, "gf_rep")
    fw.dma(sp, gf_rep[:], bcast_row(dr["ffn_norm"].rearrange("(o n) -> o n", o=1), 1024), [], [cB], cS)
    iot = P.sb([128, 32], F32, "iota")
    fw.dma(sp, iot[:], bcast_row(dr["iota"], 32), [], [cB], cS)
    w_out = P.sb([128, 8, 1024], BF16, "w_out")
    wq = P.sb([128, 8, 512], BF16, "wq")
    wo = P.sb([128, 4, 1024], BF16, "wo")
    pwq = P.sb([128, 8, 1024], BF16, "pwq")
    wB = Buf()
    P.load_w(w_out, wB, dr["w_out"], g_out, stage=stage)
    P.load_w(wq, wB, dr["xa_wq"], g_xa, stage=stage)
    P.load_w(wo, wB, dr["xa_wo"], None, stage=stage)
    P.load_w(pwq, wB, dr["peer_wq"], g_ffn, stage=stage)
    kst = stage[0][:].rearrange("p (h n) -> p h n", h=8)
    kB = stage[1]
    ksb = P.sb([128, 8, 128], BF16, "ksb")
    keysT = P.sb([128, 8, 128], BF16, "keysT")
    for p_ in range(2):
        fw.dma(sp, kst.rearrange("k h (p d) -> k h p d", p=2)[:, :, p_, :],
               dr["peer_keys"][:, p_].rearrange("h k d -> k h d"), [], [kB], stage[2])
    P.cp(dve, ksb[:], kst, [kB], [cB])
    for h in range(8):
        bt, btB = nextbank(k)
        btv = bt[:].bitcast(BF16)
        P.tr(btv[:, 0:128], ksb[:, h, :], [cB], [btB])
        P.cp(dve, keysT[:, h, :], btv[:, 0:128], [btB], [cB])
    def W2(shape, dt, name):
        return [(P.sb(shape, dt, name), Buf(), P.slot()) for _ in range(2)]

    xts = W2([128, 1024], F32, "xt")
    mxs = W2([128, 1024], BF16, "mx")
    outs = W2([128, 1024], F32, "outt")
    mT = P.sb([128, 8, 128], BF16, "mT")
    h1 = P.sb([128, 1024], F32, "h1")
    hx = P.sb([128, 1024], BF16, "hx")
    hxT = P.sb([128, 8, 128], BF16, "hxT")
    sqx = P.sb([128, 512], F32, "sqx")
    qx = P.sb([128, 4, 128], BF16, "qx")
    qxT = P.sb([128, 4, 128], BF16, "qxT")
    PTx = [P.sb([128, 4, 128], BF16, "PTx") for _ in range(2)]
    ox = P.sb([128, 4, 128], BF16, "ox")
    oxT = P.sb([128, 4, 128], BF16, "oxT")
    h2 = P.sb([128, 1024], F32, "h2")
    hn = P.sb([128, 1024], F32, "hn")
    hnb = P.sb([128, 1024], BF16, "hnb")
    hnT = P.sb([128, 8, 128], BF16, "hnT")
    qpT = P.sb([128, 8, 128], BF16, "qpT")
    s_sb = P.sb([128, 16, 128], F32, "s_sb")
    shr = P.sb([128, 2048], F32, "shr")
    s2 = shr[:].rearrange("p (j n) -> p j n", j=16)
    sv = P.sb([128, 16, 16], F32, "sv")
    si = P.sb([128, 16, 16], U32, "si")
    sif = P.sb([128, 16, 16], F32, "sif")
    cand = P.sb([128, 8, 16, 16], F32, "cand")
    cand2 = shr[:].rearrange("p (h a b) -> p h a b", h=8, a=16)
    top = P.sb([128, 8, 16], F32, "top")
    pos = P.sb([128, 8, 16], U32, "pos")
    posf = P.sb([128, 8, 16], F32, "posf")
    af = P.sb([128, 8, 16], F32, "af")
    bf = P.sb([128, 8, 16], F32, "bf")
    eq = shr[:].rearrange("p (h a b) -> p h a b", h=8, a=16)
    i1s = P.sb([128, 8, 16], F32, "i1s")
    i2s = P.sb([128, 8, 16], F32, "i2s")
    eidf = P.sb([128, 128], F32, "eidf")
    eidx = P.sb([128, 128], I32, "eidx")
    gsm = P.sb([128, 8, 16], F32, "gsm")
    zs = P.sb([128, 8], F32, "zs")
    actr = P.sb([128, 128], F32, "actr")
    ax2 = P.sb([128, 128], F32, "ax2")
    wgt = P.sb([128, 128], F32, "wgt")
    pacc = P.sb([128, 1024], F32, "pacc")
    junkf = P.sb([128, 1024], F32, "junkf")
    stt = P.sb([128, 16], F32, "stt")
    rcx = P.sb([128, 4], F32, "rcx")
    NG_ = 2
    Ugs = [(P.sb([128, 1024], F32, "Ug"), Buf(), P.slot()) for _ in range(NG_)]
    Vgs = [(P.sb([128, 1024], F32, "Vg"), Buf(), P.slot()) for _ in range(NG_)]
    wk = Buf()
    pk = Buf()
    actB = Buf()
    paccB = Buf()
    acc = k.banks[0:4]
    hi = {"n": 0}

    def hib():
        t, b_ = k.banks[4 + hi["n"] % 4]
        hi["n"] += 1
        return t, b_

    cnt = 0
    ng = 0
    for b in range(NSEQ):
        for i in range(NT):
            s_ = cnt % 2
            cnt += 1
            rows = slice(i * 128, (i + 1) * 128)
            xt, xtB, xtS = xts[s_]
            mx, mxB, mxS = mxs[s_]
            outt, outB, outS = outs[s_]
            fw.dma(sp, xt[:], dr["x"][b, rows, :], [], [xtB], xtS)
            fw.dma(sp, mx[:], sc["mixed"][b, rows, :], [scB["mixed"]], [mxB], mxS)
            bt, btB = hib()
            btv = bt[:].bitcast(BF16)
            for c in range(8):
                P.tr(btv[:, c * 128:(c + 1) * 128], mx[:, c * 128:(c + 1) * 128], [mxB], [btB])
            P.cp(dve, mT[:].rearrange("p c t -> p (c t)"), btv[:, 0:1024], [btB], [wk])
            for half in range(2):
                bo, boB = hib()
                for c in range(8):
                    P.mm(bo[:, 0:512], mT[:, c, :], w_out[:, c, half * 512:(half + 1) * 512], c == 0, c == 7, [wk, wB], [boB])
                P.tt(dve, h1[:, half * 512:(half + 1) * 512], bo[:, 0:512], xt[:, half * 512:(half + 1) * 512], ALU.add,
                     [boB, xtB], [wk])
            P.act(junk[:], h1[:], AF.Square, [wk], [junkB, wk], accum=stt[:, 0:1])
            P.rstd(stt[:, 1:2], stt[:, 0:1], D, [wk], [wk])
            P.act(hx[:], h1[:], AF.Copy, [wk], [wk], scale=stt[:, 1:2])
            bt, btB = hib()
            btv = bt[:].bitcast(BF16)
            for c in range(8):
                P.tr(btv[:, c * 128:(c + 1) * 128], hx[:, c * 128:(c + 1) * 128], [wk], [btB])
            P.cp(dve, hxT[:].rearrange("p c t -> p (c t)"), btv[:, 0:1024], [btB], [wk])
            bq, bqB = hib()
            for c in range(8):
                P.mm(bq[:, 0:512], hxT[:, c, :], wq[:, c, :], c == 0, c == 7, [wk, wB], [bqB])
            P.act(sqx[:], bq[:, 0:512], AF.Square, [bqB], [wk])
            P.red(stt[:, 4:8], sqx[:].rearrange("p (h d) -> p h d", h=4), [wk], [wk])
            P.rstd(stt[:, 8:12], stt[:, 4:8], 128, [wk], [wk])
            q3 = sqx[:].rearrange("p (h d) -> p h d", h=4)
            P.tt(dve, q3, bq[:, 0:512].rearrange("p (h d) -> p h d", h=4),
                 stt[:, 8:12].unsqueeze(2).to_broadcast([128, 4, 128]), ALU.mult, [bqB, wk], [wk])
            P.tt(dve, qx[:], q3, gx[:, :].unsqueeze(1).to_broadcast([128, 4, 128]), ALU.mult, [wk, cB], [wk])
            bt, btB = hib()
            btv = bt[:].bitcast(BF16)
            for h in range(4):
                P.tr(btv[:, h * 128:(h + 1) * 128], qx[:, h, :], [wk], [btB])
            P.cp(dve, qxT[:].rearrange("p h t -> p (h t)"), btv[:, 0:512], [btB], [wk])
            for m in range(2):
                bs, bsB = hib()
                for h in range(4):
                    P.mm(bs[:, h * 128:(h + 1) * 128], KmT[:, b, h, m * 128:(m + 1) * 128], qxT[:, h, :], True, True,
                         [memB, wk], [bsB])
                P.act(PTx[m][:].rearrange("p h q -> p (h q)"), bs[:, 0:512], AF.Exp, [bsB], [wk])
                for h in range(4):
                    bo, boB = acc[h]
                    P.mm(bo[:, 0:129], PTx[m][:, h, :], Vx[:, b, m, h, :], m == 0, m == 1, [wk, memB], [boB])
            for h in range(4):
                bo, boB = acc[h]
                fw.op(dve, lambda hh, bo=bo, h=h: hh.reciprocal(out=rcx[:, h:h + 1], in_=bo[:, 128:129]), [boB], [wk])
                P.ts(dve, ox[:, h, :], bo[:, 0:128], rcx[:, h:h + 1], ALU.mult, [boB, wk], [wk])
            bt, btB = hib()
            btv = bt[:].bitcast(BF16)
            for h in range(4):
                P.tr(btv[:, h * 128:(h + 1) * 128], ox[:, h, :], [wk], [btB])
            P.cp(dve, oxT[:].rearrange("p h t -> p (h t)"), btv[:, 0:512], [btB], [wk])
            for half in range(2):
                bo, boB = hib()
                for c in range(4):
                    P.mm(bo[:, 0:512], oxT[:, c, :], wo[:, c, half * 512:(half + 1) * 512], c == 0, c == 3, [wk, wB], [boB])
                P.tt(dve, h2[:, half * 512:(half + 1) * 512], bo[:, 0:512], h1[:, half * 512:(half + 1) * 512], ALU.add,
                     [boB, wk], [wk])
            if P4_MODE == "xa":
                P.cp(dve, outt[:], h2[:], [wk], [outB])
                fw.dma(sp, k.out[b, rows, :], outt[:], [outB], [], outS)
                continue
            P.act(junk[:], h2[:], AF.Square, [wk], [junkB, wk], accum=stt[:, 2:3])
            P.rstd(stt[:, 3:4], stt[:, 2:3], D, [wk], [wk])
            P.act(hnb[:], h2[:], AF.Copy, [wk], [wk], scale=stt[:, 3:4])
            P.ts(dve, hn[:], h2[:], stt[:, 3:4], ALU.mult, [wk], [actB])
            P.tt(dve, hn[:], hn[:], gf_rep[:], ALU.mult, [actB, cB], [actB])
            bt, btB = hib()
            btv = bt[:].bitcast(BF16)
            for c in range(8):
                P.tr(btv[:, c * 128:(c + 1) * 128], hnb[:, c * 128:(c + 1) * 128], [wk], [btB])
            P.cp(dve, hnT[:].rearrange("p c t -> p (c t)"), btv[:, 0:1024], [btB], [wk])
            for hh4 in range(2):
                bq, bqB = hib()
                for h in range(4):
                    hh_ = hh4 * 4 + h
                    for c in range(8):
                        P.mm(bq[:, h * 128:(h + 1) * 128], pwq[:, c, hh_ * 128:(hh_ + 1) * 128], hnT[:, c, :], c == 0, c == 7,
                             [wk, wB], [bqB])
                P.cp(act, qpT[:, hh4 * 4:(hh4 + 1) * 4, :].rearrange("p h t -> p (h t)"), bq[:, 0:512], [bqB], [pk])
            s_sb4 = s_sb[:].rearrange("p (h t) n -> p h t n", t=2)
            for hh4 in range(2):
                for p_ in range(2):
                    bs, bsB = hib()
                    for jj in range(4):
                        h = hh4 * 4 + jj
                        P.mm(bs[:, jj * 128:(jj + 1) * 128], qpT[p_ * 64:(p_ + 1) * 64, h, :],
                             keysT[p_ * 64:(p_ + 1) * 64, h, :], True, True, [pk, cB], [bsB])
                    P.cp(act, s_sb4[:, hh4 * 4:(hh4 + 1) * 4, p_, :], bs[:, 0:512].rearrange("p (j n) -> p j n", j=4), [bsB], [pk])
            for j in range(16):
                fw.op(dve, lambda hh, j=j: hh.max(out=sv[:, j, 0:8], in_=s_sb[:, j, :]), [pk], [pk])
                fw.op(dve, lambda hh, j=j: hh.max_index(out=si[:, j, 0:8], in_max=sv[:, j, 0:8], in_values=s_sb[:, j, :]), [pk], [pk])
                fw.op(dve, lambda hh, j=j: hh.match_replace(out=s2[:, j, :], in_to_replace=sv[:, j, 0:8], in_values=s_sb[:, j, :],
                                                            imm_value=-3.0e38), [pk], [pk])
                fw.op(dve, lambda hh, j=j: hh.max(out=sv[:, j, 8:16], in_=s2[:, j, :]), [pk], [pk])
                fw.op(dve, lambda hh, j=j: hh.max_index(out=si[:, j, 8:16], in_max=sv[:, j, 8:16], in_values=s2[:, j, :]), [pk], [pk])
            P.cp(dve, sif[:], si[:], [pk], [pk])
            sv4 = sv[:].rearrange("p (h t) n -> p h t n", t=2)
            sif4 = sif[:].rearrange("p (h t) n -> p h t n", t=2)
            P.tt(dve, cand[:], sv4[:, :, 0, :].unsqueeze(3).to_broadcast([128, 8, 16, 16]),
                 sv4[:, :, 1, :].unsqueeze(2).to_broadcast([128, 8, 16, 16]), ALU.add, [pk], [pk])
            for h in range(8):
                ch = cand[:, h].rearrange("p a b -> p (a b)")
                c2h = cand2[:, h].rearrange("p a b -> p (a b)")
                fw.op(dve, lambda hh, h=h, ch=ch: hh.max(out=top[:, h, 0:8], in_=ch), [pk], [pk])
                fw.op(dve, lambda hh, h=h, ch=ch: hh.max_index(out=pos[:, h, 0:8], in_max=top[:, h, 0:8], in_values=ch), [pk], [pk])
                fw.op(dve, lambda hh, h=h, ch=ch, c2h=c2h: hh.match_replace(out=c2h, in_to_replace=top[:, h, 0:8], in_values=ch,
                                                                             imm_value=-3.0e38), [pk], [pk])
                fw.op(dve, lambda hh, h=h, c2h=c2h: hh.max(out=top[:, h, 8:16], in_=c2h), [pk], [pk])
                fw.op(dve, lambda hh, h=h, c2h=c2h: hh.max_index(out=pos[:, h, 8:16], in_max=top[:, h, 8:16], in_values=c2h), [pk], [pk])
            P.cp(dve, posf[:], pos[:], [pk], [pk])
            P.tt(dve, eq[:, :, :, 0:15], posf[:, :, :].unsqueeze(3).to_broadcast([128, 8, 16, 15]),
                 iot[:, 16:31].unsqueeze(1).unsqueeze(1).to_broadcast([128, 8, 16, 15]), ALU.is_ge, [pk, cB], [pk])
            P.red(af[:], eq[:, :, :, 0:15], [pk], [pk])
            fw.op(dve, lambda hh: hh.scalar_tensor_tensor(out=bf[:], in0=af[:], scalar=-16.0, in1=posf[:], op0=ALU.mult, op1=ALU.add),
                  [pk], [pk])
            for (src, t_, dst) in ((af, 0, i1s), (bf, 1, i2s)):
                P.tt(dve, eq[:], src[:, :, :].unsqueeze(3).to_broadcast([128, 8, 16, 16]),
                     iot[:, 0:16].unsqueeze(1).unsqueeze(1).to_broadcast([128, 8, 16, 16]), ALU.is_equal, [pk, cB], [pk])
                P.tt(dve, eq[:], eq[:], sif4[:, :, t_, :].unsqueeze(2).to_broadcast([128, 8, 16, 16]), ALU.mult, [pk], [pk])
                P.red(dst[:], eq[:], [pk], [pk])
            fw.op(dve, lambda hh: hh.scalar_tensor_tensor(out=eidf[:].rearrange("p (h k) -> p h k", h=8), in0=i1s[:], scalar=128.0,
                                                          in1=i2s[:], op0=ALU.mult, op1=ALU.add), [pk], [pk])
            P.ts(dve, eidx[:], eidf[:], 0.0, ALU.add, [pk], [pk])
            if P4_MODE == "route":
                P.cp(dve, outt[:, 0:128], eidf[:], [pk], [outB])
                P.cp(dve, outt[:, 128:256], top[:].rearrange("p h k -> p (h k)"), [pk], [outB])
                P.cp(dve, outt[:, 256:1024], h2[:, 256:1024], [wk], [outB])
                fw.dma(sp, k.out[b, rows, :], outt[:], [outB], [], outS)
                continue
            P.tt(dve, gsm[:], top[:], top[:, :, 0:1].to_broadcast([128, 8, 16]), ALU.subtract, [pk], [pk])
            P.act(gsm[:], gsm[:], AF.Exp, [pk], [pk])
            P.red(zs[:], gsm[:], [pk], [pk])
            fw.op(dve, lambda hh: hh.reciprocal(out=zs[:], in_=zs[:]), [pk], [pk])
            P.tt(dve, gsm[:], gsm[:], zs[:, :].unsqueeze(2).to_broadcast([128, 8, 16]), ALU.mult, [pk], [pk])
            for c in range(128):
                Ug, UgB, UgS = Ugs[ng % NG_]
                ng += 1
                fw.dma(pool, None, None, [pk], [UgB], UgS,
                       fn=lambda hh, Ug=Ug, c=c: hh.indirect_dma_start(
                           out=Ug[:], out_offset=None, in_=dr["peer_u"],
                           in_offset=bass.IndirectOffsetOnAxis(ap=eidx[:, c:c + 1], axis=0)))
                fw.op(dve, lambda hh, Ug=Ug, c=c: hh.scalar_tensor_tensor(
                    out=junkf[:], in0=Ug[:], scalar=1.0, in1=hn[:], op0=ALU.mult, op1=ALU.mult,
                    accum_out=actr[:, c:c + 1]), [UgB, actB], [actB])
            P.tt(dve, ax2[:], actr[:], actr[:], ALU.mult, [actB], [actB])
            P.ts(dve, ax2[:], ax2[:], 0.044715, ALU.mult, [actB], [actB], s2=1.0, op1=ALU.add)
            P.tt(dve, ax2[:], ax2[:], actr[:], ALU.mult, [actB], [actB])
            P.act(ax2[:], ax2[:], AF.Sigmoid, [actB], [actB], scale=2.0 * GC)
            P.tt(dve, ax2[:], ax2[:], actr[:], ALU.mult, [actB], [actB])
            P.tt(dve, wgt[:], ax2[:], gsm[:].rearrange("p h k -> p (h k)"), ALU.mult, [actB, pk], [actB])
            for c in range(128):
                Vg, VgB, VgS = Vgs[c % NG_]
                fw.dma(pool, None, None, [pk], [VgB], VgS,
                       fn=lambda hh, Vg=Vg, c=c: hh.indirect_dma_start(
                           out=Vg[:], out_offset=None, in_=dr["peer_v"],
                           in_offset=bass.IndirectOffsetOnAxis(ap=eidx[:, c:c + 1], axis=0)))
                if c == 0:
                    P.ts(dve, pacc[:], Vg[:], wgt[:, 0:1], ALU.mult, [VgB, actB], [paccB])
                else:
                    fw.op(dve, lambda hh, Vg=Vg, c=c: hh.scalar_tensor_tensor(
                        out=pacc[:], in0=Vg[:], scalar=wgt[:, c:c + 1], in1=pacc[:], op0=ALU.mult, op1=ALU.add),
                        [VgB, actB, paccB], [paccB])
            P.tt(dve, outt[:], pacc[:], h2[:], ALU.add, [paccB, wk], [outB])
            fw.dma(sp, k.out[b, rows, :], outt[:], [outB], [], outS)
    P.close()
    P0.close()


def phase4d(k):
    fw, dr, sc, scB = k.fw, k.dr, k.sc, k.scB
    S, NSEQ, NT = k.S, k.NSEQ, k.NT
    nc = k.nc
    dve, act, pool, sp, pe = fw.dve, fw.act, fw.pool, fw.sp, fw.pe
    GC = 0.7978845608028654
    P0 = Ph(k, "p4")
    cB = Buf()
    cS = P0.slot()
    KmT = P0.sb([128, NSEQ, 4, 256], BF16, "KmT")
    Vx = P0.sb([128, NSEQ, 2, 4, 129], BF16, "Vx")
    memB = Buf()
    stage = (P0.sb([128, 1024], F32, "stage"), Buf(), P0.slot())
    gx = P0.sb([128, 128], F32, "gqx")
    gkx = P0.sb([128, 128], F32, "gkx")
    fw.dma(sp, gx[:], bcast_row(dr["xa_qk_norm"][0:1, :], 128), [], [cB], cS)
    fw.dma(sp, gkx[:], bcast_row(dr["xa_qk_norm"][1:2, :], 128), [], [cB], cS)
    P0.ts(dve, gx[:], gx[:], 128 ** -0.5, ALU.mult, [cB], [cB])
    junkB = Buf()
    Pa = Ph(k, "p4a")
    junk = Pa.sb([128, 1024], BF16, "junk")
    g_mem = Pa.sb([128, 8], F32)
    fw.dma(sp, g_mem[:], dr["mem_norm"].rearrange("(c p) -> p c", p=128), [], [cB], cS)
    wkv = Pa.sb([128, 8, 1024], BF16, "wkv")
    wB = Buf()
    Pa.load_w(wkv, wB, dr["xa_wkv"], g_mem, stage=stage)
    mt_ = Pa.sb([128, 1024], F32, "mt")
    mtB = Buf()
    mS = Pa.slot()
    mn = Pa.sb([128, 1024], BF16, "mn")
    mnT = Pa.sb([128, 8, 128], BF16, "mnT")
    kvs = Pa.sb([128, 1024], F32, "kvs")
    sqm = Pa.sb([128, 512], F32, "sqm")
    kb_ = Pa.sb([128, 4, 128], BF16, "kb")
    st4 = Pa.sb([128, 16], F32, "st4")
    wk = Buf()
    fw.op(pool, lambda hh: hh.memset(Vx[:], 1.0), [], [memB])
    for b in range(NSEQ):
        for m in range(2):
            fw.dma(sp, mt_[:], dr["mem"][b, m * 128:(m + 1) * 128, :], [], [mtB], mS)
            Pa.act(junk[:], mt_[:], AF.Square, [mtB], [junkB, wk], accum=st4[:, 0:1])
            Pa.rstd(st4[:, 1:2], st4[:, 0:1], D, [wk], [wk])
            Pa.act(mn[:], mt_[:], AF.Copy, [mtB, wk], [wk], scale=st4[:, 1:2])
            bt, btB = nextbank(k)
            btv = bt[:].bitcast(BF16)
            for c in range(8):
                Pa.tr(btv[:, c * 128:(c + 1) * 128], mn[:, c * 128:(c + 1) * 128], [wk], [btB])
            Pa.cp(dve, mnT[:].rearrange("p c t -> p (c t)"), btv[:, 0:1024], [btB], [wk])
            for half in range(2):
                bk, bkB = nextbank(k)
                for c in range(8):
                    Pa.mm(bk[:, 0:512], mnT[:, c, :], wkv[:, c, half * 512:(half + 1) * 512], c == 0, c == 7, [wk, wB], [bkB])
                Pa.cp(act, kvs[:, half * 512:(half + 1) * 512], bk[:, 0:512], [bkB], [wk])
            Pa.tt(dve, sqm[:], kvs[:, 0:512], kvs[:, 0:512], ALU.mult, [wk], [wk])
            Pa.red(st4[:, 4:8], sqm[:].rearrange("p (h d) -> p h d", h=4), [wk], [wk])
            Pa.rstd(st4[:, 8:12], st4[:, 4:8], 128, [wk], [wk])
            k3 = kvs[:, 0:512].rearrange("p (h d) -> p h d", h=4)
            Pa.tt(dve, k3, k3, st4[:, 8:12].unsqueeze(2).to_broadcast([128, 4, 128]), ALU.mult, [wk], [wk])
            Pa.tt(dve, kb_[:], k3, gkx[:, :].unsqueeze(1).to_broadcast([128, 4, 128]), ALU.mult, [wk, cB], [wk])
            bt, btB = nextbank(k)
            btv = bt[:].bitcast(BF16)
            for h in range(4):
                Pa.tr(btv[:, h * 128:(h + 1) * 128], kb_[:, h, :], [wk], [btB])
            Pa.cp(dve, KmT[:, b, :, m * 128:(m + 1) * 128], btv[:, 0:512].rearrange("p (h t) -> p h t", h=4), [btB], [memB])
            Pa.cp(act, Vx[:, b, m, :, 0:128], kvs[:, 512:1024].rearrange("p (h d) -> p h d", h=4), [wk], [memB])
    gfr = Pa.sb([128, 1024], F32, "gfr")
    fw.dma(sp, gfr[:], bcast_row(dr["ffn_norm"].rearrange("(o n) -> o n", o=1), 1024), [], [cB], cS)
    usts = [(Pa.sb([128, 1024], F32, "ust"), Buf(), Pa.slot()) for _ in range(2)]
    vsts = [(Pa.sb([128, 1024], F32, "vst"), Buf(), Pa.slot()) for _ in range(2)]
    ubs = [(Pa.sb([128, 1024], BF16, "ub"), Buf()) for _ in range(2)]
    utss = [(Pa.sb([128, 8, 128], BF16, "uts"), Buf(), Pa.slot()) for _ in range(2)]
    vb2s = [(Pa.sb([128, 1024], BF16, "vb2"), Buf(), Pa.slot()) for _ in range(2)]
    for i in range(128):
        ust, ustB, ustS = usts[i % 2]
        vst, vstB, vstS = vsts[i % 2]
        ub, ubB = ubs[i % 2]
        uts, utsB, utsS = utss[i % 2]
        vb2, vb2B, vb2S = vb2s[i % 2]
        fw.dma(sp, ust[:], dr["peer_u"][i * 128:(i + 1) * 128, :], [], [ustB], ustS)
        fw.dma(sp, vst[:], dr["peer_v"][i * 128:(i + 1) * 128, :], [], [vstB], vstS)
        Pa.tt(dve, ub[:], ust[:], gfr[:], ALU.mult, [ustB, cB], [ubB])
        bt, btB = nextbank(k)
        btv = bt[:].bitcast(BF16)
        for c in range(8):
            Pa.tr(btv[:, c * 128:(c + 1) * 128], ub[:, c * 128:(c + 1) * 128], [ubB], [btB])
        Pa.cp(act, uts[:].rearrange("p c j -> p (c j)"), btv[:, 0:1024], [btB], [utsB])
        fw.dma(sp, k.sc["uT"][i], uts[:], [utsB], [], utsS)
        Pa.cp(pool, vb2[:], vst[:], [vstB], [vb2B])
        fw.dma(sp, k.sc["vbf"][i * 128:(i + 1) * 128, :], vb2[:], [vb2B], [], vb2S)
    Pa.close()
    P = Ph(k, "p4b")
    g_out = P.sb([128, 8], F32)
    g_xa = P.sb([128, 8], F32)
    g_ffn = P.sb([128, 8], F32)
    fw.dma(sp, g_out[:], dr["mix_out_norm"].rearrange("(c p) -> p c", p=128), [], [cB], cS)
    fw.dma(sp, g_xa[:], dr["xa_norm"].rearrange("(c p) -> p c", p=128), [], [cB], cS)
    fw.dma(sp, g_ffn[:], dr["ffn_norm"].rearrange("(c p) -> p c", p=128), [], [cB], cS)
    iot = P.sb([128, 32], F32, "iota")
    fw.dma(sp, iot[:], bcast_row(dr["iota"], 32), [], [cB], cS)
    w_out = P.sb([128, 8, 1024], BF16, "w_out")
    wq = P.sb([128, 8, 512], BF16, "wq")
    wo = P.sb([128, 4, 1024], BF16, "wo")
    pwq = P.sb([128, 8, 1024], BF16, "pwq")
    wB = Buf()
    P.load_w(w_out, wB, dr["w_out"], g_out, stage=stage)
    P.load_w(wq, wB, dr["xa_wq"], g_xa, stage=stage)
    P.load_w(wo, wB, dr["xa_wo"], None, stage=stage)
    P.load_w(pwq, wB, dr["peer_wq"], g_ffn, stage=stage)
    kst = stage[0][:].rearrange("p (h n) -> p h n", h=8)
    kB = stage[1]
    hx = P.sb([128, 1024], BF16, "hx")
    wk = Buf()
    ksb = hx[:].rearrange("p (h n) -> p h n", h=8)
    keysT = P.sb([128, 8, 128], BF16, "keysT")
    for p_ in range(2):
        fw.dma(sp, kst.rearrange("k h (p d) -> k h p d", p=2)[:, :, p_, :],
               dr["peer_keys"][:, p_].rearrange("h k d -> k h d"), [], [kB], stage[2])
    P.cp(dve, ksb, kst, [kB], [cB, wk])
    for h in range(8):
        bt, btB = nextbank(k)
        btv = bt[:].bitcast(BF16)
        P.tr(btv[:, 0:128], ksb[:, h, :], [cB, wk], [btB])
        P.cp(dve, keysT[:, h, :], btv[:, 0:128], [btB], [cB])
    def W2(shape, dt, name):
        return [(P.sb(shape, dt, name), Buf(), P.slot()) for _ in range(2)]

    xts = W2([128, 1024], F32, "xt")
    mxs = [(P.sb([128, 1024], BF16, "mx"), Buf(), P.slot())] * 2
    outs = W2([128, 1024], F32, "outt")
    mT = P.sb([128, 8, 128], BF16, "mT")
    h1 = stage[0]
    hxT = mT
    qx = P.sb([128, 4, 128], BF16, "qx")
    qxT = P.sb([128, 4, 128], BF16, "qxT")
    PTx = [P.sb([128, 4, 128], BF16, "PTx") for _ in range(2)]
    ox = qx
    oxT = qxT
    h2 = P.sb([128, 1024], F32, "h2")
    h2S = P.slot()
    hnTgs = [P.sb([128, 8, 256], BF16, "hnTg") for _ in range(2)]
    grpBs = [Buf(), Buf()]
    hnb = hx
    hnT = mT
    qpT = P.sb([128, 8, 128], BF16, "qpT")
    s_sb = P.sb([128, 16, 128], F32, "s_sb")
    shr = P.sb([128, 2048], F32, "shr")
    sqx = shr[:, 0:512]
    junk = shr[:].bitcast(BF16)[:, 2048:3072]
    s2 = shr[:].rearrange("p (j n) -> p j n", j=16)
    sv = P.sb([128, 16, 16], F32, "sv")
    si = P.sb([128, 16, 16], U32, "si")
    sif = P.sb([128, 16, 16], F32, "sif")
    cand = s_sb[:].rearrange("p j n -> p (j n)").rearrange("p (h a b) -> p h a b", h=8, a=16)
    cand2 = shr[:].rearrange("p (h a b) -> p h a b", h=8, a=16)
    top = P.sb([128, 8, 16], F32, "top")
    pos = P.sb([128, 8, 16], U32, "pos")
    posf = P.sb([128, 8, 16], F32, "posf")
    af = P.sb([128, 8, 16], F32, "af")
    bf = P.sb([128, 8, 16], F32, "bf")
    eq = shr[:].rearrange("p (h a b) -> p h a b", h=8, a=16)
    i1s = P.sb([128, 8, 16], F32, "i1s")
    i2s = P.sb([128, 8, 16], F32, "i2s")
    gsm = P.sb([128, 8, 16], F32, "gsm")
    zs = P.sb([128, 8], F32, "zs")
    stt = P.sb([128, 16], F32, "stt")
    rcx = P.sb([128, 4], F32, "rcx")
    G_all = P.sb([128, 128, 256], BF16, "G_all")
    ITJ = P.sb([128, 3, 256], F32, "ITJ")
    iorow = P.sb([128, 128], F32, "iorow")
    fw.dma(sp, iorow[:], bcast_row(dr["iota128"], 128), [], [cB], cS)
    OHs = [(P.sb([128, 2, 128], BF16, "OH"), Buf()) for _ in range(4)]
    uTbs = [(P.sb([128, 8, 128], BF16, "uTb"), Buf(), P.slot()) for _ in range(2)]
    vbfs = [(P.sb([128, 1024], BF16, "vbf"), Buf(), P.slot()) for _ in range(2)]
    ges = [(P.sb([128, 256], BF16, "ge"), Buf()) for _ in range(2)]
    Wts = [(P.sb([128, 256], BF16, "Wt"), Buf()) for _ in range(2)]
    itjB = Buf()
    h2sB = Buf()
    GB = Buf()
    st4_ = {"cnt": 0}
    pk = Buf()
    actB = Buf()
    paccB = Buf()
    acc = k.banks[0:4]
    hi = {"n": 0}

    def hib():
        t, b_ = k.banks[4 + hi["n"] % 4]
        hi["n"] += 1
        return t, b_

    def front(b, i, tt, gp):
        hnTg = hnTgs[gp]
        grpB = grpBs[gp]
        s_ = st4_["cnt"] % 2
        st4_["cnt"] += 1
        rows = slice(i * 128, (i + 1) * 128)
        xt, xtB, xtS = xts[s_]
        mx, mxB, mxS = mxs[s_]
        outt, outB, outS = outs[s_]
        fw.dma(sp, xt[:], dr["x"][b, rows, :], [], [xtB], xtS)
        fw.dma(sp, mx[:], sc["mixed"][b, rows, :], [scB["mixed"]], [mxB], mxS)
        bt, btB = hib()
        btv = bt[:].bitcast(BF16)
        for c in range(8):
            P.tr(btv[:, c * 128:(c + 1) * 128], mx[:, c * 128:(c + 1) * 128], [mxB], [btB])
        P.cp(dve, mT[:].rearrange("p c t -> p (c t)"), btv[:, 0:1024], [btB], [wk])
        for half in range(2):
            bo, boB = hib()
            for c in range(8):
                P.mm(bo[:, 0:512], mT[:, c, :], w_out[:, c, half * 512:(half + 1) * 512], c == 0, c == 7, [wk, wB], [boB])
            P.tt(dve, h1[:, half * 512:(half + 1) * 512], bo[:, 0:512], xt[:, half * 512:(half + 1) * 512], ALU.add,
                 [boB, xtB], [wk])
        yield
        P.act(junk[:], h1[:], AF.Square, [wk], [junkB, wk, pk], accum=stt[:, 0:1])
        P.rstd(stt[:, 1:2], stt[:, 0:1], D, [wk], [wk])
        P.act(hx[:], h1[:], AF.Copy, [wk], [wk], scale=stt[:, 1:2])
        bt, btB = hib()
        btv = bt[:].bitcast(BF16)
        for c in range(8):
            P.tr(btv[:, c * 128:(c + 1) * 128], hx[:, c * 128:(c + 1) * 128], [wk], [btB])
        P.cp(dve, hxT[:].rearrange("p c t -> p (c t)"), btv[:, 0:1024], [btB], [wk])
        yield
        bq, bqB = hib()
        for c in range(8):
            P.mm(bq[:, 0:512], hxT[:, c, :], wq[:, c, :], c == 0, c == 7, [wk, wB], [bqB])
        P.act(sqx, bq[:, 0:512], AF.Square, [bqB], [wk, pk])
        P.red(stt[:, 4:8], sqx.rearrange("p (h d) -> p h d", h=4), [wk], [wk])
        P.rstd(stt[:, 8:12], stt[:, 4:8], 128, [wk], [wk])
        q3 = sqx.rearrange("p (h d) -> p h d", h=4)
        P.tt(dve, q3, bq[:, 0:512].rearrange("p (h d) -> p h d", h=4),
             stt[:, 8:12].unsqueeze(2).to_broadcast([128, 4, 128]), ALU.mult, [bqB, wk], [wk])
        P.tt(dve, qx[:], q3, gx[:, :].unsqueeze(1).to_broadcast([128, 4, 128]), ALU.mult, [wk, cB], [wk])
        bt, btB = hib()
        btv = bt[:].bitcast(BF16)
        for h in range(4):
            P.tr(btv[:, h * 128:(h + 1) * 128], qx[:, h, :], [wk], [btB])
        P.cp(dve, qxT[:].rearrange("p h t -> p (h t)"), btv[:, 0:512], [btB], [wk])
        yield
        for m in range(2):
            bs, bsB = hib()
            for h in range(4):
                P.mm(bs[:, h * 128:(h + 1) * 128], KmT[:, b, h, m * 128:(m + 1) * 128], qxT[:, h, :], True, True,
                     [memB, wk], [bsB])
            P.act(PTx[m][:].rearrange("p h q -> p (h q)"), bs[:, 0:512], AF.Exp, [bsB], [wk])
        yield
        xb = [hib(), hib()]
        for h in range(4):
            bo, boB = xb[h // 2]
            c0 = (h % 2) * 256
            for m in range(2):
                P.mm(bo[:, c0:c0 + 129], PTx[m][:, h, :], Vx[:, b, m, h, :], m == 0, m == 1, [wk, memB], [boB])
        for h in range(4):
            bo, boB = xb[h // 2]
            c0 = (h % 2) * 256
            fw.op(dve, lambda hh, bo=bo, h=h, c0=c0: hh.reciprocal(out=rcx[:, h:h + 1], in_=bo[:, c0 + 128:c0 + 129]), [boB], [wk])
            P.ts(dve, ox[:, h, :], bo[:, c0:c0 + 128], rcx[:, h:h + 1], ALU.mult, [boB, wk], [wk])
        yield
        bt, btB = hib()
        btv = bt[:].bitcast(BF16)
        for h in range(4):
            P.tr(btv[:, h * 128:(h + 1) * 128], ox[:, h, :], [wk], [btB])
        P.cp(dve, oxT[:].rearrange("p h t -> p (h t)"), btv[:, 0:512], [btB], [wk])
        for half in range(2):
            bo, boB = hib()
            for c in range(4):
                P.mm(bo[:, 0:512], oxT[:, c, :], wo[:, c, half * 512:(half + 1) * 512], c == 0, c == 3, [wk, wB], [boB])
            P.tt(dve, h2[:, half * 512:(half + 1) * 512], bo[:, 0:512], h1[:, half * 512:(half + 1) * 512], ALU.add,
                 [boB, wk], [wk])
        if P4_MODE == "xa":
            P.cp(dve, outt[:], h2[:], [wk], [outB])
            fw.dma(sp, k.out[b, rows, :], outt[:], [outB], [], outS)
            return
        fw.dma(sp, k.sc["h2s"][b, rows, :], h2[:], [wk], [h2sB], h2S)
        yield
        P.act(junk[:], h2[:], AF.Square, [wk], [junkB, wk, pk], accum=stt[:, 2:3])
        P.rstd(stt[:, 3:4], stt[:, 2:3], D, [wk], [wk])
        P.act(hnb[:], h2[:], AF.Copy, [wk], [wk], scale=stt[:, 3:4])
        bt, btB = hib()
        btv = bt[:].bitcast(BF16)
        for c in range(8):
            P.tr(btv[:, c * 128:(c + 1) * 128], hnb[:, c * 128:(c + 1) * 128], [wk], [btB])
        P.cp(dve, hnTg[:, :, tt * 128:(tt + 1) * 128], btv[:, 0:1024].rearrange("p (c t) -> p c t", c=8), [btB], [grpB])
        yield
        for hh4 in range(2):
            bq, bqB = hib()
            for h in range(4):
                hh_ = hh4 * 4 + h
                for c in range(8):
                    P.mm(bq[:, h * 128:(h + 1) * 128], pwq[:, c, hh_ * 128:(hh_ + 1) * 128],
                         hnTg[:, c, tt * 128:(tt + 1) * 128], c == 0, c == 7, [grpB, wB], [bqB])
            P.cp(act, qpT[:, hh4 * 4:(hh4 + 1) * 4, :].rearrange("p h t -> p (h t)"), bq[:, 0:512], [bqB], [pk])
        if P4_MODE == "r0c":
            return
        yield
        s_sb4 = s_sb[:].rearrange("p (h t) n -> p h t n", t=2)
        for hh4 in range(2):
            for p_ in range(2):
                bs, bsB = hib()
                for jj in range(4):
                    h = hh4 * 4 + jj
                    P.mm(bs[:, jj * 128:(jj + 1) * 128], qpT[p_ * 64:(p_ + 1) * 64, h, :],
                         keysT[p_ * 64:(p_ + 1) * 64, h, :], True, True, [pk, cB], [bsB])
                P.cp(act, s_sb4[:, hh4 * 4:(hh4 + 1) * 4, p_, :], bs[:, 0:512].rearrange("p (j n) -> p j n", j=4), [bsB], [pk])
        if P4_MODE == "r1":
            return
        for j in range(16):
            if j % 4 == 0:
                yield
            fw.op(dve, lambda hh, j=j: hh.max(out=sv[:, j, 0:8], in_=s_sb[:, j, :]), [pk], [pk])
            fw.op(dve, lambda hh, j=j: hh.max_index(out=si[:, j, 0:8], in_max=sv[:, j, 0:8], in_values=s_sb[:, j, :]), [pk], [pk])
            fw.op(dve, lambda hh, j=j: hh.match_replace(out=s2[:, j, :], in_to_replace=sv[:, j, 0:8], in_values=s_sb[:, j, :],
                                                        imm_value=-3.0e38), [pk], [pk])
            fw.op(dve, lambda hh, j=j: hh.max(out=sv[:, j, 8:16], in_=s2[:, j, :]), [pk], [pk])
            fw.op(dve, lambda hh, j=j: hh.max_index(out=si[:, j, 8:16], in_max=sv[:, j, 8:16], in_values=s2[:, j, :]), [pk], [pk])
        P.cp(dve, sif[:], si[:], [pk], [pk])
        sv4 = sv[:].rearrange("p (h t) n -> p h t n", t=2)
        sif4 = sif[:].rearrange("p (h t) n -> p h t n", t=2)
        P.tt(dve, cand[:], sv4[:, :, 0, :].unsqueeze(3).to_broadcast([128, 8, 16, 16]),
             sv4[:, :, 1, :].unsqueeze(2).to_broadcast([128, 8, 16, 16]), ALU.add, [pk], [pk])
        for h in range(8):
            if h % 2 == 0:
                yield
            ch = cand[:, h].rearrange("p a b -> p (a b)")
            c2h = cand2[:, h].rearrange("p a b -> p (a b)")
            fw.op(dve, lambda hh, h=h, ch=ch: hh.max(out=top[:, h, 0:8], in_=ch), [pk], [pk])
            fw.op(dve, lambda hh, h=h, ch=ch: hh.max_index(out=pos[:, h, 0:8], in_max=top[:, h, 0:8], in_values=ch), [pk], [pk])
            fw.op(dve, lambda hh, h=h, ch=ch, c2h=c2h: hh.match_replace(out=c2h, in_to_replace=top[:, h, 0:8], in_values=ch,
                                                                         imm_value=-3.0e38), [pk], [pk])
            fw.op(dve, lambda hh, h=h, c2h=c2h: hh.max(out=top[:, h, 8:16], in_=c2h), [pk], [pk])
            fw.op(dve, lambda hh, h=h, c2h=c2h: hh.max_index(out=pos[:, h, 8:16], in_max=top[:, h, 8:16], in_values=c2h), [pk], [pk])
        yield
        P.cp(dve, posf[:], pos[:], [pk], [pk])
        P.tt(dve, eq[:, :, :, 0:15], posf[:, :, :].unsqueeze(3).to_broadcast([128, 8, 16, 15]),
             iot[:, 16:31].unsqueeze(1).unsqueeze(1).to_broadcast([128, 8, 16, 15]), ALU.is_ge, [pk, cB], [pk])
        P.red(af[:], eq[:, :, :, 0:15], [pk], [pk])
        fw.op(dve, lambda hh: hh.scalar_tensor_tensor(out=bf[:], in0=af[:], scalar=-16.0, in1=posf[:], op0=ALU.mult, op1=ALU.add),
              [pk], [pk])
        for (src, t_, dst) in ((af, 0, i1s), (bf, 1, i2s)):
            P.tt(dve, eq[:], src[:, :, :].unsqueeze(3).to_broadcast([128, 8, 16, 16]),
                 iot[:, 0:16].unsqueeze(1).unsqueeze(1).to_broadcast([128, 8, 16, 16]), ALU.is_equal, [pk, cB], [pk])
            P.tt(dve, eq[:], eq[:], sif4[:, :, t_, :].unsqueeze(2).to_broadcast([128, 8, 16, 16]), ALU.mult, [pk], [pk])
            P.red(dst[:], eq[:], [pk], [pk])
        if P4_MODE == "route":
            P.cp(dve, outt[:, 0:128], eidf[:], [pk], [outB])
            P.cp(dve, outt[:, 128:256], top[:].rearrange("p h k -> p (h k)"), [pk], [outB])
            P.cp(dve, outt[:, 256:1024], h2[:, 256:1024], [wk], [outB])
            fw.dma(sp, k.out[b, rows, :], outt[:], [outB], [], outS)
            return
        P.tt(dve, gsm[:], top[:], top[:, :, 0:1].to_broadcast([128, 8, 16]), ALU.subtract, [pk], [pk])
        P.act(gsm[:], gsm[:], AF.Exp, [pk], [pk])
        P.red(zs[:], gsm[:], [pk], [pk])
        fw.op(dve, lambda hh: hh.reciprocal(out=zs[:], in_=zs[:]), [pk], [pk])
        P.tt(dve, gsm[:], gsm[:], zs[:, :].unsqueeze(2).to_broadcast([128, 8, 16]), ALU.mult, [pk], [pk])
        if P4_MODE == "r2":
            return
        yield
        btf, btfB = hib()
        for n_, src_ in enumerate((i1s, i2s, gsm)):
            fw.op(pe, lambda hh, n_=n_, src_=src_, btf=btf: hh.transpose(
                out=btf[:, n_ * 128:(n_ + 1) * 128], in_=src_[:].rearrange("p h k -> p (h k)"), identity=k.identf[:]),
                [pk, k.cB], [btfB])
        P.cp(dve, ITJ[:, :, tt * 128:(tt + 1) * 128], btf[:, 0:384].rearrange("p (n t) -> p n t", n=3), [btfB], [itjB])

    def group_peer(b, g2, gp, stepper):
        T = 256
        hnTg = hnTgs[gp]
        grpB = grpBs[gp]
        for t0 in range(0, T, 4):
            bg, bgB = hib()
            for t in range(t0, t0 + 4):
                OH, OHB = OHs[t % 4]
                P.ts(dve, OH[:, 0, :], iorow[:], ITJ[:, 1, t:t + 1], ALU.is_equal, [cB, itjB], [OHB])
                P.ts(dve, OH[:, 1, :], iorow[:], ITJ[:, 0, t:t + 1], ALU.is_equal, [cB, itjB], [OHB],
                     s2=ITJ[:, 2, t:t + 1], op1=ALU.mult)
                P.mm(bg[:, (t - t0) * 128:(t - t0 + 1) * 128], OH[:, 0, :], OH[:, 1, :], True, True, [OHB], [bgB])
            P.cp(act, G_all[:, :, t0:t0 + 4].rearrange("j i t -> j t i"),
                 bg[:, 0:512].rearrange("j (t i) -> j t i", t=4), [bgB], [GB])
        for i in range(128):
            uTb, uTbB, uTbS = uTbs[i % 2]
            vbf, vbfB, vbfS = vbfs[i % 2]
            ge, geB = ges[i % 2]
            Wt, WtB = Wts[i % 2]
            fw.dma(sp, uTb[:], k.sc["uT"][i], [], [uTbB], uTbS)
            fw.dma(sp, vbf[:], k.sc["vbf"][i * 128:(i + 1) * 128, :], [], [vbfB], vbfS)
            ba, baB = hib()
            for c in range(8):
                P.mm(ba[:, 0:T], uTb[:, c, :], hnTg[:, c, :], c == 0, c == 7, [uTbB, grpB], [baB])
            P.act(ge[:], ba[:, 0:T], AF.Gelu_apprx_tanh, [baB], [geB])
            P.tt(pool, Wt[:], ge[:], G_all[:, i, :], ALU.mult, [geB, GB], [WtB])
            for tt in range(2):
                for half in range(2):
                    bo, boB = acc[tt * 2 + half]
                    P.mm(bo[:, 0:512], Wt[:, tt * 128:(tt + 1) * 128], vbf[:, half * 512:(half + 1) * 512], i == 0, i == 127,
                         [WtB, vbfB], [boB])
            stepper(i)
        for tt in range(2):
            outt, outB, outS = outs[tt]
            rows = slice((g2 * 2 + tt) * 128, (g2 * 2 + tt + 1) * 128)
            fw.dma(sp, outt[:], k.sc["h2s"][b, rows, :], [h2sB], [outB], outS)
            for half in range(2):
                bo, boB = acc[tt * 2 + half]
                P.tt(dve, outt[:, half * 512:(half + 1) * 512], bo[:, 0:512], outt[:, half * 512:(half + 1) * 512], ALU.add,
                     [boB, outB], [outB])
            fw.dma(sp, k.out[b, rows, :], outt[:], [outB], [], outS)

    groups = [(b, g2) for b in range(NSEQ) for g2 in range(NT // 2)]

    def front_gen(gi):
        b, g2 = groups[gi]
        for tt in range(2):
            yield from front(b, g2 * 2 + tt, tt, gi % 2)

    def run_all(gen):
        for _ in gen:
            pass

    run_all(front_gen(0))
    for gi, (b, g2) in enumerate(groups):
        nxt = front_gen(gi + 1) if gi + 1 < len(groups) else None
        state = {"gen": nxt}

        def stepper(i, state=state):
            if state["gen"] is None or i % 2 == 1:
                return
            try:
                next(state["gen"])
            except StopIteration:
                state["gen"] = None

        if P4_MODE == "full":
            group_peer(b, g2, gi % 2, stepper)
        if state["gen"] is not None:
            run_all(state["gen"])
    P.close()
    P0.close()


_NC_CACHE = {}


def kernel(**inputs):
    S, NSEQ, NCORE = 4096, 2, 8
    if "nc" not in _NC_CACHE:
        _NC_CACHE["nc"] = build(S, NSEQ)
    nc = _NC_CACHE["nc"]
    consts = host_consts(S)
    x = np.ascontiguousarray(np.asarray(inputs["x"], dtype=np.float32))
    mem = np.ascontiguousarray(np.asarray(inputs["mem"], dtype=np.float32))
    params = {n: np.ascontiguousarray(np.asarray(inputs[n], dtype=np.float32)[0]) for n in PARAM_NAMES}
    in_maps = []
    for c in range(NCORE):
        m = {"x": x[c * NSEQ:(c + 1) * NSEQ], "mem": mem[c * NSEQ:(c + 1) * NSEQ]}
        m.update(params)
        m.update(consts)
        in_maps.append(m)
    res = run_bass_kernel_spmd(nc, in_maps, core_ids=list(range(NCORE)))
    return np.concatenate([np.asarray(r["out"]) for r in res.results], axis=0).astype(np.float32)
```

```python
import numpy as np
import concourse.bass as bass
import concourse.mybir as mybir
from concourse.bass_utils import run_bass_kernel_spmd
from contextlib import ExitStack

F32 = mybir.dt.float32
BF16 = mybir.dt.bfloat16
U32 = mybir.dt.uint32
I32 = mybir.dt.int32
AF = mybir.ActivationFunctionType
ALU = mybir.AluOpType
AX = mybir.AxisListType

SEM_LIMIT = 1 << 30


class Buf:
    __slots__ = ("w", "r")

    def __init__(self):
        self.w = None
        self.r = {}


class Slot:
    __slots__ = ("ctr",)

    def __init__(self, ctr):
        self.ctr = ctr


class Eng:
    def __init__(self, fw, name, handle, is_pe=False):
        self.fw = fw
        self.name = name
        self.h = handle
        self.sem = None
        self.cnt = 0
        self.known = {}
        self.q = []
        self.is_pe = is_pe

    def new_event(self):
        if self.sem is None or self.cnt >= SEM_LIMIT:
            self.sem = self.fw.new_sem()
            self.cnt = 0
        self.cnt += 1
        return (self.sem, self.cnt)


class FW:
    def __init__(self, nc, stack):
        self.nc = nc
        self.stack = stack
        self.nsem = 0
        self.pe = Eng(self, "pe", nc.tensor, True)
        self.act = Eng(self, "act", nc.scalar)
        self.dve = Eng(self, "dve", nc.vector)
        self.pool = Eng(self, "pool", nc.gpsimd)
        self.sp = Eng(self, "sp", nc.sync)
        self.engs = [self.pe, self.act, self.dve, self.pool, self.sp]
        self.ctrs = []
        self.free_ctrs = []

    def new_sem(self):
        self.nsem += 1
        return self.stack.enter_context(self.nc.semaphore(f"fs{self.nsem}"))

    def slot(self):
        if self.free_ctrs:
            ctr = self.free_ctrs.pop()
        else:
            ctr = [None, 0]
            self.ctrs.append(ctr)
        return Slot(ctr)

    def _waits(self, eng, reads, writes):
        evs = {}

        def add(ev):
            if ev is None:
                return
            s, v = ev
            if evs.get(s, 0) < v:
                evs[s] = v

        for b in reads:
            add(b.w)
        for b in writes:
            add(b.w)
            for s, v in b.r.items():
                add((s, v))
        out = []
        for s, v in evs.items():
            if eng.known.get(s, 0) >= v:
                continue
            if eng.is_pe and s is eng.sem:
                continue
            out.append((s, v))
            eng.known[s] = v
        return out

    def _commit(self, ev, reads, writes):
        s, v = ev
        for b in reads:
            if b.r.get(s, 0) < v:
                b.r[s] = v
        for b in writes:
            b.w = ev
            b.r = {}

    def op(self, eng, fn, reads=(), writes=()):
        waits = self._waits(eng, reads, writes)
        ev = eng.new_event()
        eng.q.append((waits, fn, ev, 1))
        self._commit(ev, reads, writes)
        return ev

    def dma(self, eng, out, in_, reads, writes, slot, fn=None):
        waits = self._waits(eng, reads, writes)
        ctr = slot.ctr
        if ctr[0] is None:
            ctr[0] = self.new_sem()
            ctr[1] = 0
        if ctr[1] > 0 and eng.known.get(ctr[0], 0) < ctr[1]:
            if not any(s_ is ctr[0] for s_, _ in waits):
                waits.append((ctr[0], ctr[1]))
            else:
                waits = [((s_, max(v_, ctr[1])) if s_ is ctr[0] else (s_, v_)) for s_, v_ in waits]
            eng.known[ctr[0]] = ctr[1]
        ctr[1] += 16
        ev = (ctr[0], ctr[1])
        if fn is None:
            fn = lambda h: h.dma_start(out=out, in_=in_)
        eng.q.append((waits, fn, ev, 16))
        self._commit(ev, reads, writes)
        return ev

    def barrier(self):
        evs = []
        for e in self.engs:
            if e.sem is not None and e.cnt > 0:
                evs.append((e.sem, e.cnt))
        for c in self.ctrs:
            if c[0] is not None and c[1] > 0:
                evs.append((c[0], c[1]))
        for e in self.engs:
            waits = []
            for s, v in evs:
                if e.known.get(s, 0) >= v:
                    continue
                if s is e.sem:
                    continue
                waits.append((s, v))
                e.known[s] = v
            if waits:
                e.q.append((waits, None, None, 0))

    def finish(self):
        self.barrier()
        with self.nc.Block() as block:
            for eng, deco in [(self.pe, block.tensor), (self.act, block.scalar),
                              (self.dve, block.vector), (self.pool, block.gpsimd),
                              (self.sp, block.sync)]:
                def body(h, eng=eng):
                    for waits, fn, ev, inc in eng.q:
                        for s, v in waits:
                            h.wait_ge(s, v)
                        if fn is None:
                            continue
                        ins = fn(h)
                        ins.then_inc(ev[0], inc)
                deco(body)


D = 1024
IN_COLS = 1720
EPS = 1e-6
ROPE_THETA = 500000.0

PARAM_NAMES = ["mix_norm", "w_in", "mla_q_norm", "mla_kv_norm", "mla_w_uq", "mla_w_ukv",
               "mla_qk_norm", "nsa_q_norm", "nsa_k_norm", "nsa_cmp_pe", "nsa_cmp_w1",
               "nsa_cmp_w2", "mix_out_norm", "w_out", "xa_norm", "mem_norm", "xa_wq",
               "xa_wkv", "xa_qk_norm", "xa_wo", "ffn_norm", "peer_wq", "peer_keys",
               "peer_u", "peer_v"]
PARAM_SHAPES = {
    "mix_norm": [D], "w_in": [D, IN_COLS], "mla_q_norm": [256], "mla_kv_norm": [128],
    "mla_w_uq": [256, 768], "mla_w_ukv": [128, 1024], "mla_qk_norm": [2, 96],
    "nsa_q_norm": [64], "nsa_k_norm": [3, 64], "nsa_cmp_pe": [2, 32, 64],
    "nsa_cmp_w1": [2, 2048, 128], "nsa_cmp_w2": [2, 128, 64], "mix_out_norm": [D],
    "w_out": [D, D], "xa_norm": [D], "mem_norm": [D], "xa_wq": [D, 512],
    "xa_wkv": [D, 1024], "xa_qk_norm": [2, 128], "xa_wo": [512, D], "ffn_norm": [D],
    "peer_wq": [D, D], "peer_keys": [8, 2, 128, 64], "peer_u": [16384, D],
    "peer_v": [16384, D],
}


def host_consts(S):
    c = {}
    pos = np.arange(S, dtype=np.float32)
    inv_m = (1.0 / (ROPE_THETA ** (np.arange(0, 32, 2, dtype=np.float32) / 32))).astype(np.float32)
    ang = pos[:, None] * inv_m[None, :]
    c["rope_m"] = np.concatenate([np.cos(ang), np.sin(ang)], axis=1).astype(np.float32)
    inv_n = (1.0 / (ROPE_THETA ** (np.arange(0, 16, 2, dtype=np.float32) / 16))).astype(np.float32)
    ang = pos[:, None] * inv_n[None, :]
    c["rope_n"] = np.concatenate([np.cos(ang), np.sin(ang)], axis=1).astype(np.float32)
    ncmp = (S - 32) // 16 + 1
    ncp = ((ncmp + 127) // 128) * 128
    cend = (np.arange(ncp) * 16 + 31).astype(np.float32)
    ang = cend[:, None] * inv_n[None, :]
    c["rope_c"] = np.concatenate([np.cos(ang), np.sin(ang)], axis=1).astype(np.float32)
    nblk = S // 64
    cs = np.arange(ncp) * 16
    bs = np.arange(nblk) * 64
    ov = ((cs[:, None] < bs[None, :] + 64) & (cs[:, None] + 32 > bs[None, :])).astype(np.float32)
    ov[ncmp:] = 0.0
    c["overlap"] = ov
    q = np.arange(S)
    cur = q // 64
    b = np.arange(nblk)
    forced = (b[None, :] == 0) | (b[None, :] == cur[:, None]) | (b[None, :] == cur[:, None] - 1)
    valid = b[None, :] <= cur[:, None]
    c["selmul"] = (valid & ~forced).astype(np.float32)
    c["seladd"] = np.where(valid, np.where(forced, 1e4, 0.0), -1e30).astype(np.float32)
    nt = S // 128
    E = np.zeros((nblk, nt, 128), np.float32)
    for kt in range(nt):
        for k in range(128):
            E[2 * kt + k // 64, kt, k] = 1.0
    c["emat"] = E
    c["iota128"] = np.arange(128, dtype=np.float32).reshape(1, 128)
    c["iota"] = np.concatenate([np.arange(16), np.arange(1, 16) * 16, [0]]).astype(np.float32).reshape(1, 32)
    return c


class KB:
    pass


def build(S, NSEQ, dbg=False, phases=(1, 2, 3, 4)):
    nc = bass.Bass("TRN2", target_bir_lowering=False)
    NT = S // 128
    NBLK = S // 64
    NCMP = (S - 32) // 16 + 1
    NCP = ((NCMP + 127) // 128) * 128
    TOK = NSEQ * S
    okind = "ExternalOutput" if dbg else "Internal"
    dr = {}
    dr["x"] = nc.dram_tensor("x", [NSEQ, S, D], F32, kind="ExternalInput").ap()
    dr["mem"] = nc.dram_tensor("mem", [NSEQ, 256, D], F32, kind="ExternalInput").ap()
    for n in PARAM_NAMES:
        dr[n] = nc.dram_tensor(n, PARAM_SHAPES[n], F32, kind="ExternalInput").ap()
    cshape = {"rope_m": [S, 32], "rope_n": [S, 16], "rope_c": [NCP, 16], "overlap": [NCP, NBLK],
              "selmul": [S, NBLK], "seladd": [S, NBLK], "emat": [NBLK, NT, 128], "iota": [1, 32], "iota128": [1, 128]}
    for n, sh in cshape.items():
        dr[n] = nc.dram_tensor(n, sh, F32, kind="ExternalInput").ap()
    out = nc.dram_tensor("out", [NSEQ, S, D], F32, kind="ExternalOutput").ap()
    dbgt = nc.dram_tensor("dbg", [128, 1024], F32, kind="ExternalOutput").ap() if dbg else None
    sc = {}

    def scr(name, shape, dt):
        sc[name] = nc.dram_tensor("sc_" + name, shape, dt, kind=okind).ap()

    scr("QTm", [NSEQ, 96, 8, S], BF16)
    scr("KTm", [NSEQ, 96, 8, S], BF16)
    scr("Vm", [NSEQ, S, 8, 65], BF16)
    scr("QTn", [NSEQ, 64, 8, S], BF16)
    scr("KsT", [NSEQ, 64, 2, S], BF16)
    scr("KwT", [NSEQ, 64, 2, S], BF16)
    scr("Vs", [NSEQ, S, 2, 65], BF16)
    scr("Vw", [NSEQ, S, 2, 65], BF16)
    scr("gate", [NSEQ, S, 24], F32)
    scr("kcT", [NSEQ, 128, S], BF16)
    scr("vcT", [NSEQ, 128, S], BF16)
    scr("mixed", [NSEQ, S, D], BF16)
    sc["uT"] = nc.dram_tensor("sc_uT", [128, 128, 8, 128], BF16, kind="Internal").ap()
    sc["vbf"] = nc.dram_tensor("sc_vbf", [16384, D], BF16, kind="Internal").ap()
    sc["h2s"] = nc.dram_tensor("sc_h2s", [NSEQ, S, D], F32, kind="Internal").ap()
    scB = {k: Buf() for k in sc}

    with ExitStack() as gst:
        fw = FW(nc, gst)
        gst.enter_context(nc.allow_non_contiguous_dma(reason="strided param / scratch layouts"))
        gst.enter_context(nc.allow_low_precision(reason="bf16 matmul operands, fp32 accumulation"))
        k = KB()
        k.nc, k.fw, k.dr, k.sc, k.scB, k.out = nc, fw, dr, sc, scB, out
        k.S, k.NSEQ, k.NT, k.NBLK, k.NCMP, k.NCP = S, NSEQ, NT, NBLK, NCMP, NCP
        k.dbgt = dbgt
        k.banks = []
        for i in range(8):
            t = gst.enter_context(nc.psum_tensor(f"bank{i}", [128, 512], F32))
            k.banks.append((t, Buf()))
        k.bank_i = 0
        k.ident = gst.enter_context(nc.sbuf_tensor("ident", [128, 128], BF16))
        k.identf = gst.enter_context(nc.sbuf_tensor("identf", [128, 128], F32))
        k.eps = gst.enter_context(nc.sbuf_tensor("eps", [128, 1], F32))
        k.cB = Buf()
        fw.op(fw.pool, lambda h: h.memset(k.eps[:], EPS), [], [k.cB])
        fw.op(fw.pool, lambda h: h.memset(k.identf[:], 0.0), [], [k.cB])
        fw.op(fw.pool, lambda h: h.affine_select(out=k.identf[:], in_=k.identf[:], pattern=[[-1, 128]],
                                                 compare_op=ALU.not_equal, fill=1.0, base=0,
                                                 channel_multiplier=1), [], [k.cB])
        fw.op(fw.dve, lambda h: h.tensor_copy(out=k.ident[:], in_=k.identf[:]), [k.cB], [k.cB])
        if 1 in phases:
            phase1(k)
        if 2 in phases:
            phase2(k)
        if 3 in phases:
            phase3(k)
        if 4 in phases:
            (phase4d if PEER_DENSE else phase4)(k)
        fw.finish()
    return nc


def nextbank(k):
    t, b = k.banks[k.bank_i % 8]
    k.bank_i += 1
    return t, b


class Ph:
    def __init__(self, k, name):
        self.k = k
        self.fw = k.fw
        self.nc = k.nc
        self.name = name
        self.st = ExitStack()
        self.n = 0
        self.slots = []

    def slot(self):
        s = self.fw.slot()
        self.slots.append(s)
        return s

    def sb(self, shape, dt, name=None):
        self.n += 1
        t = self.st.enter_context(self.nc.sbuf_tensor(f"{self.name}_{name or 't'}{self.n}", shape, dt))
        return t

    def sbn(self, n, shape, dt, name=None):
        return [(self.sb(shape, dt, name), Buf()) for _ in range(n)]

    def close(self):
        self.fw.barrier()
        for s in self.slots:
            self.fw.free_ctrs.append(s.ctr)
        self.slots = []
        self.st.close()

    def tt(self, eng, out, a, b, op, r, w):
        return self.fw.op(eng, lambda h: h.tensor_tensor(out=out, in0=a, in1=b, op=op), r, w)

    def ts(self, eng, out, a, s1, op0, r, w, s2=None, op1=None):
        if op1 is None:
            return self.fw.op(eng, lambda h: h.tensor_scalar(out=out, in0=a, scalar1=s1, scalar2=None, op0=op0), r, w)
        return self.fw.op(eng, lambda h: h.tensor_scalar(out=out, in0=a, scalar1=s1, scalar2=s2, op0=op0, op1=op1), r, w)

    def act(self, out, in_, func, r, w, scale=1.0, bias=None, accum=None):
        kw = {}
        if bias is not None:
            kw["bias"] = bias
        if accum is not None:
            kw["accum_out"] = accum
        return self.fw.op(self.fw.act, lambda h: h.activation(out=out, in_=in_, func=func, scale=scale, **kw), r, w)

    def cp(self, eng, out, in_, r, w):
        if eng is self.fw.act:
            return self.fw.op(eng, lambda h: h.copy(out=out, in_=in_), r, w)
        return self.fw.op(eng, lambda h: h.tensor_copy(out=out, in_=in_), r, w)

    def red(self, out, in_, r, w, op=ALU.add):
        return self.fw.op(self.fw.dve, lambda h: h.tensor_reduce(out=out, in_=in_, axis=AX.X, op=op), r, w)

    def mm(self, out, lhsT, rhs, start, stop, r, w):
        return self.fw.op(self.fw.pe, lambda h: h.matmul(out=out, lhsT=lhsT, rhs=rhs, start=start, stop=stop), r, w)

    def tr(self, out, in_, r, w):
        ident = self.k.ident
        p = in_.partition_size()
        return self.fw.op(self.fw.pe, lambda h: h.transpose(out=out, in_=in_, identity=ident[0:p, 0:p]), list(r) + [self.k.cB], w)

    def rstd(self, out, ss, d, r, w):
        p = ss.partition_size()
        self.act(out, ss, AF.Sqrt, list(r) + [self.k.cB], w, scale=1.0 / d, bias=self.k.eps[0:p, :])
        self.fw.op(self.fw.dve, lambda h: h.reciprocal(out=out, in_=out), w, w)

    def load_w(self, dst, dstB, src, gain=None, rows=128, stage=None):
        C = dst.shape[1]
        N = dst.shape[2]
        srcv = src.rearrange("(c p) n -> p c n", p=rows)
        for c in range(C):
            st_t, st_b, slot = stage
            self.fw.dma(self.fw.sp, st_t[0:rows, 0:N], srcv[:, c, :], [], [st_b], slot)
            if gain is not None:
                self.ts(self.fw.dve, dst[:, c, :], st_t[0:rows, 0:N], gain[:, c:c + 1], ALU.mult, [st_b], [dstB])
            else:
                self.cp(self.fw.dve, dst[:, c, :], st_t[0:rows, 0:N], [st_b], [dstB])


def bcast_row(ap_row, n):
    return ap_row.to_broadcast([128, n])


def phase1(k):
    fw, dr, sc, scB = k.fw, k.dr, k.sc, k.scB
    S, NSEQ, NT = k.S, k.NSEQ, k.NT
    P = Ph(k, "p1")
    dve, act, pool, sp, pe = fw.dve, fw.act, fw.pool, fw.sp, fw.pe
    parB = Buf()
    parS = P.slot()
    stage = (P.sb([128, 1720], F32, "stage"), Buf(), P.slot())
    g_mix = P.sb([128, 8], F32)
    g_q = P.sb([128, 2], F32)
    g_kv = P.sb([128, 1], F32)
    gq_m = P.sb([128, 96], F32)
    gk_m = P.sb([128, 96], F32)
    gq_n = P.sb([128, 64], F32)
    gk_n = P.sb([128, 2, 64], F32)
    ropem = P.sb([128, NT, 32], F32)
    ropen = P.sb([128, NT, 16], F32)
    fw.dma(sp, g_mix[:], dr["mix_norm"].rearrange("(c p) -> p c", p=128), [], [parB], parS)
    fw.dma(sp, g_q[:], dr["mla_q_norm"].rearrange("(c p) -> p c", p=128), [], [parB], parS)
    fw.dma(sp, g_kv[:], dr["mla_kv_norm"].rearrange("(c p) -> p c", p=128), [], [parB], parS)
    fw.dma(sp, gq_m[:], bcast_row(dr["mla_qk_norm"][0:1, :], 96), [], [parB], parS)
    fw.dma(sp, gk_m[:], bcast_row(dr["mla_qk_norm"][1:2, :], 96), [], [parB], parS)
    fw.dma(sp, gq_n[:], bcast_row(dr["nsa_q_norm"].rearrange("(o n) -> o n", o=1), 64), [], [parB], parS)
    fw.dma(sp, gk_n[:, 0, :], bcast_row(dr["nsa_k_norm"][1:2, :], 64), [], [parB], parS)
    fw.dma(sp, gk_n[:, 1, :], bcast_row(dr["nsa_k_norm"][2:3, :], 64), [], [parB], parS)
    fw.dma(sp, ropem[:], dr["rope_m"].rearrange("(t p) n -> p t n", p=128), [], [parB], parS)
    fw.dma(sp, ropen[:], dr["rope_n"].rearrange("(t p) n -> p t n", p=128), [], [parB], parS)
    P.ts(dve, gq_m[:], gq_m[:], 96 ** -0.5, ALU.mult, [parB], [parB])
    P.ts(dve, gq_n[:], gq_n[:], 64 ** -0.5, ALU.mult, [parB], [parB])
    w_in = P.sb([128, 8, IN_COLS], BF16, "w_in")
    w_uq = P.sb([128, 2, 768], BF16, "w_uq")
    w_ukv = P.sb([128, 1, 1024], BF16, "w_ukv")
    wB = Buf()
    P.load_w(w_in, wB, dr["w_in"], g_mix, stage=stage)
    P.load_w(w_uq, wB, dr["mla_w_uq"], g_q, stage=stage)
    P.load_w(w_ukv, wB, dr["mla_w_ukv"], g_kv, stage=stage)

    def wset(shape, dt, name):
        return [(P.sb(shape, dt, name), Buf(), P.slot()) for _ in range(2)]

    xts = wset([128, D], F32, "xt")
    junks = wset([128, D], BF16, "junk")
    stts = wset([128, 80], F32, "stt")
    xns = wset([128, D], BF16, "xn")
    nTs = wset([128, 8, 128], BF16, "nT")
    cqns = wset([128, 384], BF16, "cqn")
    kpes = wset([128, 32], F32, "kpe")
    cTs = wset([128, 3, 128], BF16, "cT")
    qms = wset([128, 8, 96], F32, "qm")
    kvms = wset([128, 8, 128], F32, "kvm")
    sqs = wset([128, D], F32, "sq")
    tmps = wset([128, 4, 8, 16], F32, "tmp")
    qbs = wset([128, 8, 96], BF16, "qb")
    kbs = wset([128, 8, 96], BF16, "kb")
    kps = wset([128, 8, 32], F32, "kp")
    vbs = wset([128, 8, 65], BF16, "vb")
    qTs = wset([96, 8, 128], BF16, "qT")
    kTs = wset([96, 8, 128], BF16, "kT")
    qns = wset([128, 8, 64], F32, "qn")
    qnbs = wset([128, 8, 64], BF16, "qnb")
    qnTs = wset([64, 8, 128], BF16, "qnT")
    c1s = wset([128, 512], F32, "c1")
    kkns = wset([128, 4, 64], F32, "kkn")
    kkbs = wset([128, 4, 64], BF16, "kkb")
    kkTs = wset([64, 4, 128], BF16, "kkT")
    vsws = wset([128, 2, 2, 65], BF16, "vsw")
    gts = wset([128, 24], F32, "gt")
    kvTs = wset([128, 256], BF16, "kvT")
    xS = [[P.slot() for _ in range(3)] for _ in range(2)]
    for s in range(2):
        fw.op(pool, lambda h, s=s: h.memset(vbs[s][0][:], 1.0), [], [vbs[s][1]])
        fw.op(pool, lambda h, s=s: h.memset(vsws[s][0][:], 1.0), [], [vsws[s][1]])

    def rope(src3, dst3, o1, o2, hw, cos, sin, tmp, tmpB, srcB, dstB, nh):
        x1 = src3[:, :, o1:o1 + hw]
        x2 = src3[:, :, o2:o2 + hw]
        c = cos.unsqueeze(1).to_broadcast([128, nh, hw])
        sn = sin.unsqueeze(1).to_broadcast([128, nh, hw])
        t = [tmp[:, j, 0:nh, 0:hw] for j in range(4)]
        P.tt(dve, t[0], x1, c, ALU.mult, [srcB, parB], [tmpB])
        P.tt(dve, t[1], x2, sn, ALU.mult, [srcB, parB], [tmpB])
        P.tt(dve, t[2], x2, c, ALU.mult, [srcB, parB], [tmpB])
        P.tt(dve, t[3], x1, sn, ALU.mult, [srcB, parB], [tmpB])
        P.tt(dve, dst3[:, :, o1:o1 + hw], t[0], t[1], ALU.subtract, [tmpB], [dstB])
        P.tt(dve, dst3[:, :, o2:o2 + hw], t[2], t[3], ALU.add, [tmpB], [dstB])

    cnt = 0
    for b in range(NSEQ):
        for i in range(NT):
            s = cnt % 2
            cnt += 1
            rows = slice(i * 128, (i + 1) * 128)
            xt, xtB, xtS = xts[s]
            junk, junkB, _ = junks[s]
            stt, sttB, _ = stts[s]
            xn, xnB, _ = xns[s]
            nT, nTB, _ = nTs[s]
            sq, sqB, _ = sqs[s]
            tmp, tmpB, _ = tmps[s]
            fw.dma(sp, xt[:], dr["x"][b, rows, :], [], [xtB], xtS)
            P.act(junk[:], xt[:], AF.Square, [xtB], [junkB, sttB], accum=stt[:, 0:1])
            P.rstd(stt[:, 1:2], stt[:, 0:1], D, [sttB], [sttB])
            P.act(xn[:], xt[:], AF.Copy, [xtB, sttB], [xnB], scale=stt[:, 1:2])
            bt, btB = nextbank(k)
            btv = bt[:].bitcast(BF16)
            for c in range(8):
                P.tr(btv[:, c * 128:(c + 1) * 128], xn[:, c * 128:(c + 1) * 128], [xnB], [btB])
            P.cp(dve, nT[:].rearrange("p c t -> p (c t)"), btv[:, 0:1024], [btB], [nTB])
            bA, bAB = nextbank(k)
            for c in range(8):
                P.mm(bA[:, 0:416], nT[:, c, :], w_in[:, c, 0:416], c == 0, c == 7, [nTB, wB], [bAB])
            bB, bBB = nextbank(k)
            for c in range(8):
                P.mm(bB[:, 0:512], nT[:, c, :], w_in[:, c, 416:928], c == 0, c == 7, [nTB, wB], [bBB])
            bC, bCB = nextbank(k)
            for c in range(8):
                P.mm(bC[:, 0:512], nT[:, c, :], w_in[:, c, 1184:1696], c == 0, c == 7, [nTB, wB], [bCB])
            bG, bGB = nextbank(k)
            for c in range(8):
                P.mm(bG[:, 0:24], nT[:, c, :], w_in[:, c, 1696:1720], c == 0, c == 7, [nTB, wB], [bGB])
            for j in range(2):
                for c in range(8):
                    P.mm(bG[:, 32 + j * 128:160 + j * 128], w_in[:, c, 928 + j * 128:1056 + j * 128], nT[:, c, :],
                         c == 0, c == 7, [nTB, wB], [bGB])
            qn, qnB, _ = qns[s]
            qnb, qnbB, _ = qnbs[s]
            qnT, qnTB, qnTS = qnTs[s]
            bB3 = bB[:, 0:512].rearrange("p (h d) -> p h d", h=8)
            P.act(sq[:, 0:512], bB[:, 0:512], AF.Square, [bBB], [sqB])
            P.red(stt[:, 48:56], sq[:, 0:512].rearrange("p (h d) -> p h d", h=8), [sqB], [sttB])
            P.rstd(stt[:, 56:64], stt[:, 48:56], 64, [sttB], [sttB])
            P.tt(dve, qn[:], bB3, stt[:, 56:64].unsqueeze(2).to_broadcast([128, 8, 64]), ALU.mult, [bBB, sttB], [qnB])
            P.tt(dve, qn[:], qn[:], gq_n[:, :].unsqueeze(1).to_broadcast([128, 8, 64]), ALU.mult, [qnB, parB], [qnB])
            rope(qn, qnb, 0, 8, 8, ropen[:, i, 0:8], ropen[:, i, 8:16], tmp, tmpB, qnB, qnbB, 8)
            P.cp(act, qnb[:, :, 16:64], qn[:, :, 16:64], [qnB], [qnbB])
            bq, bqB = nextbank(k)
            bqv = bq[:].bitcast(BF16)
            for h in range(8):
                P.tr(bqv[0:64, h * 128:(h + 1) * 128], qnb[:, h, :], [qnbB], [bqB])
            P.cp(act, qnT[:].rearrange("p h t -> p (h t)"), bqv[0:64, 0:1024], [bqB], [qnTB])
            fw.dma(sp, sc["QTn"][b, :, :, rows], qnT[:], [qnTB], [], qnTS)
            c1, c1B, _ = c1s[s]
            kkn, kknB, _ = kkns[s]
            kkb, kkbB, _ = kkbs[s]
            kkT, kkTB, kkTS = kkTs[s]
            vsw, vswB, vswS = vsws[s]
            P.cp(act, c1[:], bC[:, 0:512], [bCB], [c1B])
            kk4 = c1[:].rearrange("p (a r) -> p a r", a=2)[:, :, 0:128].rearrange("p a (g d) -> p a g d", g=2)
            sq4 = sq[:, 0:256].rearrange("p (a g d) -> p a g d", a=2, g=2)
            P.tt(dve, sq4, kk4, kk4, ALU.mult, [c1B], [sqB])
            P.red(stt[:, 64:68], sq[:, 0:256].rearrange("p (j d) -> p j d", j=4), [sqB], [sttB])
            P.rstd(stt[:, 68:72], stt[:, 64:68], 64, [sttB], [sttB])
            kkn4 = kkn[:].rearrange("p (a g) d -> p a g d", a=2)
            P.tt(dve, kkn4, kk4, stt[:, 68:72].rearrange("p (a g) -> p a g", a=2).unsqueeze(3).to_broadcast([128, 2, 2, 64]),
                 ALU.mult, [c1B, sttB], [kknB])
            P.tt(dve, kkn4, kkn4, gk_n[:, :, :].unsqueeze(2).to_broadcast([128, 2, 2, 64]), ALU.mult, [kknB, parB], [kknB])
            rope(kkn, kkb, 0, 8, 8, ropen[:, i, 0:8], ropen[:, i, 8:16], tmp, tmpB, kknB, kkbB, 4)
            P.cp(act, kkb[:, :, 16:64], kkn[:, :, 16:64], [kknB], [kkbB])
            bq, bqB = nextbank(k)
            bqv = bq[:].bitcast(BF16)
            for j in range(4):
                P.tr(bqv[0:64, j * 128:(j + 1) * 128], kkb[:, j, :], [kkbB], [bqB])
            P.cp(act, kkT[:].rearrange("p h t -> p (h t)"), bqv[0:64, 0:512], [bqB], [kkTB])
            fw.dma(sp, sc["KsT"][b, :, :, rows], kkT[:, 0:2, :], [kkTB], [], kkTS)
            fw.dma(sp, sc["KwT"][b, :, :, rows], kkT[:, 2:4, :], [kkTB], [], xS[s][0])
            P.cp(act, vsw[:, 0, :, 0:64], c1[:, 128:256].rearrange("p (g d) -> p g d", g=2), [c1B], [vswB])
            P.cp(act, vsw[:, 1, :, 0:64], c1[:, 384:512].rearrange("p (g d) -> p g d", g=2), [c1B], [vswB])
            fw.dma(sp, sc["Vs"][b, rows], vsw[:, 0], [vswB], [], vswS)
            fw.dma(sp, sc["Vw"][b, rows], vsw[:, 1], [vswB], [], xS[s][1])
            gt, gtB, gtS = gts[s]
            kvT, kvTB, kvTS = kvTs[s]
            P.act(gt[:], bG[:, 0:24], AF.Sigmoid, [bGB], [gtB])
            fw.dma(sp, sc["gate"][b, rows], gt[:], [gtB], [], gtS)
            P.cp(dve, kvT[:], bG[:, 32:288], [bGB], [kvTB])
            fw.dma(sp, sc["kcT"][b, :, rows], kvT[:, 0:128], [kvTB], [], kvTS)
            fw.dma(sp, sc["vcT"][b, :, rows], kvT[:, 128:256], [kvTB], [], xS[s][2])
            cqn, cqnB, _ = cqns[s]
            kpe, kpeB, _ = kpes[s]
            cT, cTB, _ = cTs[s]
            P.act(junk[:, 0:256], bA[:, 0:256], AF.Square, [bAB], [junkB, sttB], accum=stt[:, 2:3])
            P.act(junk[:, 0:128], bA[:, 256:384], AF.Square, [bAB], [junkB, sttB], accum=stt[:, 3:4])
            P.rstd(stt[:, 4:5], stt[:, 2:3], 256, [sttB], [sttB])
            P.rstd(stt[:, 5:6], stt[:, 3:4], 128, [sttB], [sttB])
            P.act(cqn[:, 0:256], bA[:, 0:256], AF.Copy, [bAB, sttB], [cqnB], scale=stt[:, 4:5])
            P.act(cqn[:, 256:384], bA[:, 256:384], AF.Copy, [bAB, sttB], [cqnB], scale=stt[:, 5:6])
            P.cp(dve, kpe[:], bA[:, 384:416], [bAB], [kpeB])
            bt2, bt2B = nextbank(k)
            bt2v = bt2[:].bitcast(BF16)
            for c in range(3):
                P.tr(bt2v[:, c * 128:(c + 1) * 128], cqn[:, c * 128:(c + 1) * 128], [cqnB], [bt2B])
            P.cp(dve, cT[:].rearrange("p c t -> p (c t)"), bt2v[:, 0:384], [bt2B], [cTB])
            qm, qmB, _ = qms[s]
            kvm, kvmB, _ = kvms[s]
            qmf = qm[:].rearrange("p h d -> p (h d)")
            kvf = kvm[:].rearrange("p h d -> p (h d)")
            bQ1, bQ1B = nextbank(k)
            for c in range(2):
                P.mm(bQ1[:, 0:512], cT[:, c, :], w_uq[:, c, 0:512], c == 0, c == 1, [cTB, wB], [bQ1B])
            bQ2, bQ2B = nextbank(k)
            for c in range(2):
                P.mm(bQ2[:, 0:256], cT[:, c, :], w_uq[:, c, 512:768], c == 0, c == 1, [cTB, wB], [bQ2B])
            P.cp(act, qmf[:, 0:512], bQ1[:, 0:512], [bQ1B], [qmB])
            P.cp(act, qmf[:, 512:768], bQ2[:, 0:256], [bQ2B], [qmB])
            bK1, bK1B = nextbank(k)
            P.mm(bK1[:, 0:512], cT[:, 2, :], w_ukv[:, 0, 0:512], True, True, [cTB, wB], [bK1B])
            bK2, bK2B = nextbank(k)
            P.mm(bK2[:, 0:512], cT[:, 2, :], w_ukv[:, 0, 512:1024], True, True, [cTB, wB], [bK2B])
            P.cp(act, kvf[:, 0:512], bK1[:, 0:512], [bK1B], [kvmB])
            P.cp(dve, kvf[:, 512:1024], bK2[:, 0:512], [bK2B], [kvmB])
            qb, qbB, _ = qbs[s]
            sq768 = sq[:, 0:768]
            P.tt(dve, sq768, qmf, qmf, ALU.mult, [qmB], [sqB])
            P.red(stt[:, 8:16], sq768.rearrange("p (h d) -> p h d", h=8), [sqB], [sttB])
            P.rstd(stt[:, 16:24], stt[:, 8:16], 96, [sttB], [sttB])
            P.tt(dve, qm[:], qm[:], stt[:, 16:24].unsqueeze(2).to_broadcast([128, 8, 96]), ALU.mult, [qmB, sttB], [qmB])
            P.tt(dve, qm[:], qm[:], gq_m[:, :].unsqueeze(1).to_broadcast([128, 8, 96]), ALU.mult, [qmB, parB], [qmB])
            rope(qm, qb, 64, 80, 16, ropem[:, i, 0:16], ropem[:, i, 16:32], tmp, tmpB, qmB, qbB, 8)
            P.cp(act, qb[:, :, 0:64], qm[:, :, 0:64], [qmB], [qbB])
            kb, kbB, _ = kbs[s]
            kp, kpB, _ = kps[s]
            vb, vbB, vbS = vbs[s]
            sq512 = sq[:, 0:512].rearrange("p (h d) -> p h d", h=8)
            P.tt(dve, sq512, kvm[:, :, 0:64], kvm[:, :, 0:64], ALU.mult, [kvmB], [sqB])
            P.red(stt[:, 24:32], sq512, [sqB], [sttB])
            P.act(junk[:, 0:32], kpe[:], AF.Square, [kpeB], [junkB, sttB], accum=stt[:, 32:33])
            P.ts(dve, stt[:, 24:32], stt[:, 24:32], stt[:, 32:33], ALU.add, [sttB], [sttB])
            P.rstd(stt[:, 40:48], stt[:, 24:32], 96, [sttB], [sttB])
            P.tt(dve, kvm[:, :, 0:64], kvm[:, :, 0:64], stt[:, 40:48].unsqueeze(2).to_broadcast([128, 8, 64]),
                 ALU.mult, [kvmB, sttB], [kvmB])
            P.tt(dve, kb[:, :, 0:64], kvm[:, :, 0:64], gk_m[:, 0:64].unsqueeze(1).to_broadcast([128, 8, 64]),
                 ALU.mult, [kvmB, parB], [kbB])
            P.tt(dve, kp[:], kpe[:, :].unsqueeze(1).to_broadcast([128, 8, 32]),
                 stt[:, 40:48].unsqueeze(2).to_broadcast([128, 8, 32]), ALU.mult, [kpeB, sttB], [kpB])
            P.tt(dve, kp[:], kp[:], gk_m[:, 64:96].unsqueeze(1).to_broadcast([128, 8, 32]), ALU.mult, [kpB, parB], [kpB])
            x1 = kp[:, :, 0:16]
            x2 = kp[:, :, 16:32]
            cc = ropem[:, i, 0:16].unsqueeze(1).to_broadcast([128, 8, 16])
            sn = ropem[:, i, 16:32].unsqueeze(1).to_broadcast([128, 8, 16])
            t = [tmp[:, j, 0:8, 0:16] for j in range(4)]
            P.tt(dve, t[0], x1, cc, ALU.mult, [kpB, parB], [tmpB])
            P.tt(dve, t[1], x2, sn, ALU.mult, [kpB, parB], [tmpB])
            P.tt(dve, t[2], x2, cc, ALU.mult, [kpB, parB], [tmpB])
            P.tt(dve, t[3], x1, sn, ALU.mult, [kpB, parB], [tmpB])
            P.tt(dve, kb[:, :, 64:80], t[0], t[1], ALU.subtract, [tmpB], [kbB])
            P.tt(dve, kb[:, :, 80:96], t[2], t[3], ALU.add, [tmpB], [kbB])
            P.cp(act, vb[:, :, 0:64], kvm[:, :, 64:128], [kvmB], [vbB])
            fw.dma(sp, sc["Vm"][b, rows], vb[:], [vbB], [], vbS)
            qT, qTB, qTS = qTs[s]
            kT, kTB, kTS = kTs[s]
            for (src, srcB, dst, dstB, dstS, name) in ((qb, qbB, qT, qTB, qTS, "QTm"), (kb, kbB, kT, kTB, kTS, "KTm")):
                bq, bqB = nextbank(k)
                bqv = bq[:].bitcast(BF16)
                for h in range(8):
                    P.tr(bqv[0:96, h * 128:(h + 1) * 128], src[:, h, :], [srcB], [bqB])
                P.cp(act, dst[:].rearrange("p h t -> p (h t)"), bqv[0:96, 0:1024], [bqB], [dstB])
                fw.dma(sp, sc[name][b, :, :, rows], dst[:], [dstB], [], dstS)
    P.close()


def phase2(k):
    fw, dr, sc, scB = k.fw, k.dr, k.sc, k.scB
    S, NSEQ, NT = k.S, k.NSEQ, k.NT
    P = Ph(k, "p2")
    dve, act, pool, sp, pe = fw.dve, fw.act, fw.pool, fw.sp, fw.pe
    GQ = min(512, S)
    TG = GQ // 128
    NG = S // GQ
    KT = P.sb([96, 8, S], BF16, "KT")
    V = P.sb([128, NT, 8, 65], BF16, "V")
    KTB, VB = Buf(), Buf()
    kvS = P.slot()
    QTs = [(P.sb([96, 8, GQ], BF16, "QT"), Buf(), P.slot()) for _ in range(2)]
    PTs = [(P.sb([128, GQ], BF16, "PT"), Buf()) for _ in range(3)]
    os_ = [(P.sb([128, TG, 512], F32, "o"), Buf()) for _ in range(2)]
    obs = [(P.sb([128, 512], BF16, "ob"), Buf(), P.slot()) for _ in range(2)]
    junk = P.sb([128, 512], BF16, "junk")
    junkB = Buf()
    stt = P.sb([128, 16], F32, "stt")
    sttB = Buf()
    rc = P.sb([128, 32], F32, "rc")
    rcB = [Buf() for _ in range(32)]
    pv = k.banks[0:4]
    scb = k.banks[4:8]
    nsc = 0
    npt = 0
    nob = 0
    gi = 0
    for b in range(NSEQ):
        for h in range(8):
            fw.dma(sp, KT[:, h, :], sc["KTm"][b, :, h, :], [scB["KTm"]], [KTB], kvS)
        vsrc = sc["Vm"][b].rearrange("(t p) h e -> p t h e", p=128)
        for t0 in range(0, NT, 8):
            t1 = min(NT, t0 + 8)
            fw.dma(sp, V[:, t0:t1], vsrc[:, t0:t1], [scB["Vm"]], [VB], kvS)
        for g in range(NG):
            QT, QTB, QTS = QTs[gi % 2]
            o, oB = os_[gi % 2]
            gi += 1
            fw.dma(sp, QT[:], sc["QTm"][b, :, :, g * GQ:(g + 1) * GQ], [scB["QTm"]], [QTB], QTS)
            for h in range(8):
                nk = (g + 1) * TG
                for kt in range(nk):
                    j0 = max(0, kt - g * TG)
                    bs, bsB = scb[nsc % 4]
                    nsc += 1
                    PT, PTB = PTs[npt % 3]
                    npt += 1
                    P.mm(bs[:, j0 * 128:GQ], KT[:, h, kt * 128:(kt + 1) * 128], QT[:, h, j0 * 128:GQ], True, True,
                         [KTB, QTB], [bsB])
                    P.act(PT[:, j0 * 128:GQ], bs[:, j0 * 128:GQ], AF.Exp, [bsB], [PTB])
                    if kt >= g * TG:
                        dsl = PT[:, j0 * 128:(j0 + 1) * 128]
                        fw.op(pool, lambda hh, dsl=dsl: hh.affine_select(out=dsl, in_=dsl, pattern=[[1, 128]],
                                                                         compare_op=ALU.is_ge, fill=0.0, base=0,
                                                                         channel_multiplier=-1), [PTB], [PTB])
                    for j in range(j0, TG):
                        bo, boB = pv[j]
                        P.mm(bo[:, 0:65], PT[:, j * 128:(j + 1) * 128], V[:, kt, h, :], kt == 0, kt == g * TG + j,
                             [PTB, VB], [boB])
                for j in range(TG):
                    bo, boB = pv[j]
                    c = (h * TG + j) % 32
                    fw.op(dve, lambda hh, bo=bo, c=c: hh.reciprocal(out=rc[:, c:c + 1], in_=bo[:, 64:65]), [boB], [rcB[c]])
                    P.act(o[:, j, h * 64:(h + 1) * 64], bo[:, 0:64], AF.Copy, [boB, rcB[c]], [oB], scale=rc[:, c:c + 1])
            for j in range(TG):
                ob, obB, obS = obs[nob % 2]
                nob += 1
                rows = slice((g * TG + j) * 128, (g * TG + j + 1) * 128)
                P.act(junk[:], o[:, j, :], AF.Square, [oB], [junkB, sttB], accum=stt[:, 0:1])
                P.rstd(stt[:, 1:2], stt[:, 0:1], 512, [sttB], [sttB])
                P.act(ob[:], o[:, j, :], AF.Copy, [oB, sttB], [obB], scale=stt[:, 1:2])
                fw.dma(sp, sc["mixed"][b, rows, 0:512], ob[:], [obB], [], obS)
    P.close()


NSA_BR = "csw"
DBG_QI = 2


def phase3(k):
    fw, dr, sc, scB = k.fw, k.dr, k.sc, k.scB
    S, NSEQ, NT, NBLK, NCMP, NCP = k.S, k.NSEQ, k.NT, k.NBLK, k.NCMP, k.NCP
    P = Ph(k, "p3")
    dve, act, pool, sp, pe = fw.dve, fw.act, fw.pool, fw.sp, fw.pe
    NNT = NCP // 128
    W = 65 + NBLK
    GC = 0.7978845608028654
    cB = Buf()
    cS = P.slot()
    stB = Buf()
    stS = P.slot()
    stage = P.sb([128, 4096], F32, "stage")
    w1 = P.sb([128, 2, 32, 128], BF16, "w1")
    for kv in range(2):
        src = dr["nsa_cmp_w1"][kv].rearrange("(l d) n -> d l n", d=64)
        for half in range(2):
            fw.dma(sp, stage[half * 64:(half + 1) * 64, :].rearrange("p (l n) -> p l n", l=32), src, [], [stB], stS)
        P.cp(dve, w1[:, kv].rearrange("p l n -> p (l n)"), stage[:, 0:4096], [stB], [cB])
    pst = P.sb([64, 2, 32], F32, "pst")
    peT = P.sb([64, 2, 32], BF16, "peT")
    fw.dma(sp, pst[:], dr["nsa_cmp_pe"].rearrange("k l d -> d k l"), [], [cB], cS)
    P.cp(dve, peT[:], pst[:], [cB], [cB])
    w2s = P.sb([128, 2, 64], F32, "w2s")
    w2 = P.sb([128, 2, 64], BF16, "w2")
    fw.dma(sp, w2s[:], dr["nsa_cmp_w2"].rearrange("k h d -> h k d"), [], [cB], cS)
    P.cp(dve, w2[:], w2s[:], [cB], [cB])
    gk_c = P.sb([128, 64], F32, "gk_c")
    fw.dma(sp, gk_c[:], bcast_row(dr["nsa_k_norm"][0:1, :], 64), [], [cB], cS)
    ropec = P.sb([128, NNT, 16], F32, "ropec")
    fw.dma(sp, ropec[:], dr["rope_c"].rearrange("(t p) n -> p t n", p=128), [], [cB], cS)
    ovs = P.sb([128, NNT, NBLK], F32, "ovs")
    fw.dma(sp, ovs[:], dr["overlap"].rearrange("(t p) n -> p t n", p=128), [], [cB], cS)
    selmul = P.sb([128, NT, NBLK], F32, "selmul")
    seladd = P.sb([128, NT, NBLK], F32, "seladd")
    fw.dma(sp, selmul[:], dr["selmul"].rearrange("(t p) n -> p t n", p=128), [], [cB], cS)
    fw.dma(sp, seladd[:], dr["seladd"].rearrange("(t p) n -> p t n", p=128), [], [cB], cS)
    ems = P.sb([NBLK, NT * 128], F32, "ems")
    emat = P.sb([NBLK, NT, 128], BF16, "emat")
    fw.dma(sp, ems[:], dr["emat"].rearrange("b t k -> b (t k)"), [], [cB], cS)
    P.cp(dve, emat[:].rearrange("b t k -> b (t k)"), ems[:], [cB], [cB])
    cbias = P.sb([128, 2], F32, "cbias")
    for kv in range(2):
        bb, bbB = nextbank(k)
        for l in range(32):
            P.mm(bb[:, 0:1], w1[0:64, kv, l, :], peT[:, kv, l:l + 1], l == 0, l == 31, [cB], [bbB])
        P.cp(dve, cbias[:, kv:kv + 1], bb[:, 0:1], [bbB], [cB])
    xT = [P.sb([128, S], BF16, "kcT_in"), P.sb([128, S], BF16, "vcT_in")]
    xTB = Buf()
    KcT = P.sb([64, 2, NCP], BF16, "KcT")
    VcOv = P.sb([128, NNT, 2, W], BF16, "VcOv")
    hT = P.sb([128, NCP], BF16, "hT")
    cmpB = Buf()
    KsT = P.sb([64, 2, S], BF16, "KsT")
    KwT = P.sb([64, 2, S], BF16, "KwT")
    Vs = P.sb([128, NT, 2, 65], BF16, "Vs")
    Vw = P.sb([128, NT, 2, 65], BF16, "Vw")
    gates = P.sb([128, NT, 24], F32, "gates")
    seqB = Buf()
    seqS = P.slot()
    xs = P.sb([128, NCP], F32, "xs")
    x2 = P.sb([128, NCP], F32, "x2")
    wkB = Buf()
    kc_f = P.sb([128, 64], F32, "kc_f")
    kc_b = P.sb([128, 64], BF16, "kc_b")
    ctmp = P.sb([128, 4, 1, 8], F32, "ctmp")
    cst = P.sb([128, 8], F32, "cst")
    junk = P.sb([128, 512], BF16, "junk")
    junkB = Buf()
    QTs = [(P.sb([64, 8, 128], BF16, "QTt"), Buf(), P.slot()) for _ in range(2)]
    PTs = [(P.sb([128, 4, 128], BF16, "PT"), Buf()) for _ in range(3)]
    onsas = [(P.sb([128, 8, 64], F32, "onsa"), Buf()) for _ in range(2)]
    obs = [(P.sb([128, 512], BF16, "ob"), Buf(), P.slot()) for _ in range(2)]
    imp = P.sb([128, NBLK], F32, "imp")
    score = P.sb([128, NBLK], F32, "score")
    score2 = P.sb([128, NBLK], F32, "score2")
    m8 = P.sb([128, 16], F32, "m8")
    negb = P.sb([128, NBLK], BF16, "negb")
    negT = P.sb([NBLK, 4, 128], BF16, "negT")
    selB = Buf()
    negTB = Buf()
    rc = P.sb([128, 16], F32, "rc")
    rcB = Buf()
    stt = P.sb([128, 4], F32, "stt")
    sttB = Buf()
    acc = k.banks[0:4]
    scb = k.banks[4:8]
    st_ = {"nsc": 0, "npt": 0}

    def unit(lhsT, lhsB, Qr, QB, mask_mm, sel_fn, Vrhs, VB, first, last):
        bs, bsB = scb[st_["nsc"] % 4]
        st_["nsc"] += 1
        PT, PTB = PTs[st_["npt"] % 3]
        st_["npt"] += 1
        P.mm(bs[:, 0:512], lhsT, Qr, True, mask_mm is None, lhsB + [QB], [bsB])
        if mask_mm is not None:
            P.mm(bs[:, 0:512], mask_mm[0], mask_mm[1], False, True, mask_mm[2], [bsB])
        P.act(PT[:].rearrange("p h q -> p (h q)"), bs[:, 0:512], AF.Exp, [bsB], [PTB])
        if sel_fn is not None:
            base, cm, step, op = sel_fn
            fw.op(pool, lambda hh, PT=PT: hh.affine_select(out=PT[:], in_=PT[:], pattern=[[0, 4], [step, 128]],
                                                         compare_op=op, fill=0.0, base=base,
                                                         channel_multiplier=cm), [PTB], [PTB])
        wv = Vrhs.shape[-1]
        for r in range(4):
            bo, boB = acc[r]
            P.mm(bo[:, 0:wv], PT[:, r, :], Vrhs, first, last, [PTB] + VB, [boB])

    nq = 0
    for b in range(NSEQ):
        fw.dma(sp, xT[0][:], sc["kcT"][b], [scB["kcT"]], [xTB], seqS)
        fw.dma(sp, xT[1][:], sc["vcT"][b], [scB["vcT"]], [xTB], seqS)
        for g in range(2):
            fw.dma(sp, KsT[:, g, :], sc["KsT"][b, :, g, :], [scB["KsT"]], [seqB], seqS)
            fw.dma(sp, KwT[:, g, :], sc["KwT"][b, :, g, :], [scB["KwT"]], [seqB], seqS)
        fw.dma(sp, Vs[:], sc["Vs"][b].rearrange("(t p) g e -> p t g e", p=128), [scB["Vs"]], [seqB], seqS)
        fw.dma(sp, Vw[:], sc["Vw"][b].rearrange("(t p) g e -> p t g e", p=128), [scB["Vw"]], [seqB], seqS)
        fw.dma(sp, gates[:], sc["gate"][b].rearrange("(t p) n -> p t n", p=128), [scB["gate"]], [seqB], seqS)
        fw.op(pool, lambda hh: hh.memset(KcT[:], 0.0), [], [cmpB])
        fw.op(pool, lambda hh: hh.memset(VcOv[:], 0.0), [], [cmpB])
        for nt in range(NNT):
            for g in range(2):
                P.cp(dve, VcOv[:, nt, g, 65:W], ovs[:, nt, :], [cB], [cmpB])
                fw.op(pool, lambda hh, nt=nt, g=g: hh.memset(VcOv[:, nt, g, 64:65], 1.0), [cmpB], [cmpB])
        for kv in range(2):
            for g in range(2):
                fw.op(pool, lambda hh: hh.memset(hT[:], 0.0), [wkB], [wkB])
                bh, bhB = nextbank(k)
                for l in range(32):
                    P.mm(bh[:, 0:NCMP], w1[g * 64:(g + 1) * 64, kv, l, :],
                         xT[kv][g * 64:(g + 1) * 64, l:l + 16 * (NCMP - 1) + 1:16], l == 0, l == 31, [cB, xTB], [bhB])
                P.act(xs[:, 0:NCMP], bh[:, 0:NCMP], AF.Identity, [bhB, cB], [wkB], bias=cbias[:, kv:kv + 1])
                P.tt(dve, x2[:, 0:NCMP], xs[:, 0:NCMP], xs[:, 0:NCMP], ALU.mult, [wkB], [wkB])
                P.ts(dve, x2[:, 0:NCMP], x2[:, 0:NCMP], 0.044715, ALU.mult, [wkB], [wkB], s2=1.0, op1=ALU.add)
                P.tt(dve, x2[:, 0:NCMP], x2[:, 0:NCMP], xs[:, 0:NCMP], ALU.mult, [wkB], [wkB])
                P.act(x2[:, 0:NCMP], x2[:, 0:NCMP], AF.Sigmoid, [wkB], [wkB], scale=2.0 * GC)
                P.tt(dve, hT[:, 0:NCMP], x2[:, 0:NCMP], xs[:, 0:NCMP], ALU.mult, [wkB], [wkB])
                for nt in range(NNT):
                    bo, boB = nextbank(k)
                    P.mm(bo[:, 0:64], hT[:, nt * 128:(nt + 1) * 128], w2[:, kv, :], True, True, [wkB, cB], [boB])
                    if kv == 1:
                        P.cp(dve, VcOv[:, nt, g, 0:64], bo[:, 0:64], [boB], [cmpB])
                        continue
                    P.act(junk[:, 0:64], bo[:, 0:64], AF.Square, [boB], [junkB, wkB], accum=cst[:, 0:1])
                    P.rstd(cst[:, 1:2], cst[:, 0:1], 64, [wkB], [wkB])
                    P.ts(dve, kc_f[:], bo[:, 0:64], cst[:, 1:2], ALU.mult, [boB, wkB], [wkB])
                    P.tt(dve, kc_f[:], kc_f[:], gk_c[:], ALU.mult, [wkB, cB], [wkB])
                    x1 = kc_f[:, 0:8]
                    x2_ = kc_f[:, 8:16]
                    cc = ropec[:, nt, 0:8]
                    sn = ropec[:, nt, 8:16]
                    t = [ctmp[:, j, 0, :] for j in range(4)]
                    P.tt(dve, t[0], x1, cc, ALU.mult, [wkB, cB], [wkB])
                    P.tt(dve, t[1], x2_, sn, ALU.mult, [wkB, cB], [wkB])
                    P.tt(dve, t[2], x2_, cc, ALU.mult, [wkB, cB], [wkB])
                    P.tt(dve, t[3], x1, sn, ALU.mult, [wkB, cB], [wkB])
                    P.tt(dve, kc_b[:, 0:8], t[0], t[1], ALU.subtract, [wkB], [wkB])
                    P.tt(dve, kc_b[:, 8:16], t[2], t[3], ALU.add, [wkB], [wkB])
                    P.cp(dve, kc_b[:, 16:64], kc_f[:, 16:64], [wkB], [wkB])
                    bt, btB = nextbank(k)
                    btv = bt[:].bitcast(BF16)
                    P.tr(btv[0:64, 0:128], kc_b[:], [wkB], [btB])
                    P.cp(dve, KcT[:, g, nt * 128:(nt + 1) * 128], btv[0:64, 0:128], [btB], [cmpB])
        for qi in range(NT):
            QTt, QB, QS = QTs[nq % 2]
            onsa, onB = onsas[nq % 2]
            ob, obB, obS = obs[nq % 2]
            nq += 1
            rows = slice(qi * 128, (qi + 1) * 128)
            fw.dma(sp, QTt[:], sc["QTn"][b, :, :, rows], [scB["QTn"]], [QB], QS)
            for g in range(2):
                Qr = QTt[:, g * 4:(g + 1) * 4, :]
                nts = [nt for nt in range(NNT) if 16 * 128 * nt + 31 <= 128 * qi + 127]
                for ii, nt in enumerate(nts):
                    full = 16 * (128 * nt + 127) + 31 <= 128 * qi
                    sel_fn = None if full else (128 * qi - 2048 * nt - 31, -16, 1, ALU.is_ge)
                    unit(KcT[:, g, nt * 128:(nt + 1) * 128], [cmpB], Qr, QB, None, sel_fn,
                         VcOv[:, nt, g, :], [cmpB], ii == 0, ii == len(nts) - 1)
                for r in range(4):
                    h = g * 4 + r
                    bo, boB = acc[r]
                    P.ts(dve, rc[:, r:r + 1], bo[:, 64:65], 1e-30, ALU.max, [boB], [rcB])
                    fw.op(dve, lambda hh, r=r: hh.reciprocal(out=rc[:, r:r + 1], in_=rc[:, r:r + 1]), [rcB], [rcB])
                    if r == 0:
                        P.ts(dve, imp[:], bo[:, 65:W], rc[:, r:r + 1], ALU.mult, [boB, rcB], [selB])
                    else:
                        fw.op(dve, lambda hh, bo=bo, r=r: hh.scalar_tensor_tensor(
                            out=imp[:], in0=bo[:, 65:W], scalar=rc[:, r:r + 1], in1=imp[:], op0=ALU.mult, op1=ALU.add),
                            [boB, rcB, selB], [selB])
                    P.tt(dve, rc[:, 4 + r:5 + r], rc[:, r:r + 1], gates[:, qi, h * 3:h * 3 + 1], ALU.mult, [rcB, seqB], [rcB])
                    if "c" not in NSA_BR:
                        P.ts(dve, rc[:, 4 + r:5 + r], rc[:, 4 + r:5 + r], 0.0, ALU.mult, [rcB], [rcB])
                    P.ts(dve, onsa[:, h, :], bo[:, 0:64], rc[:, 4 + r:5 + r], ALU.mult, [boB, rcB], [onB])
                P.tt(dve, score[:], imp[:], selmul[:, qi, :], ALU.mult, [selB, cB], [selB])
                P.tt(dve, score[:], score[:], seladd[:, qi, :], ALU.add, [selB, cB], [selB])
                fw.op(dve, lambda hh: hh.max(out=m8[:, 0:8], in_=score[:]), [selB], [selB])
                fw.op(dve, lambda hh: hh.match_replace(out=score2[:], in_to_replace=m8[:, 0:8], in_values=score[:],
                                                       imm_value=-3.0e38), [selB], [selB])
                fw.op(dve, lambda hh: hh.max(out=m8[:, 8:16], in_=score2[:]), [selB], [selB])
                P.ts(dve, m8[:, 15:16], m8[:, 15:16], -1.0e29, ALU.max, [selB], [selB])
                P.ts(dve, score2[:], score[:], m8[:, 15:16], ALU.is_ge, [selB], [selB])
                P.ts(dve, negb[:], score2[:], 30000.0, ALU.mult, [selB], [selB], s2=-30000.0, op1=ALU.add)
                bt, btB = nextbank_hi(k, st_)
                btv = bt[:].bitcast(BF16)
                P.tr(btv[0:NBLK, 0:128], negb[:], [selB], [btB])
                P.cp(dve, negT[:], btv[0:NBLK, 0:128].unsqueeze(1).to_broadcast([NBLK, 4, 128]), [btB], [negTB])
                for kt in range(qi + 1):
                    sel_fn = (0, -1, 1, ALU.is_ge) if kt == qi else None
                    unit(KsT[:, g, kt * 128:(kt + 1) * 128], [seqB], Qr, QB,
                         (emat[:, kt, :], negT[:], [cB, negTB]), sel_fn, Vs[:, kt, g, :], [seqB], kt == 0, kt == qi)
                for r in range(4):
                    h = g * 4 + r
                    bo, boB = acc[r]
                    fw.op(dve, lambda hh, bo=bo, r=r: hh.reciprocal(out=rc[:, 8 + r:9 + r], in_=bo[:, 64:65]), [boB], [rcB])
                    P.tt(dve, rc[:, 8 + r:9 + r], rc[:, 8 + r:9 + r], gates[:, qi, h * 3 + 1:h * 3 + 2], ALU.mult, [rcB, seqB], [rcB])
                    if "s" not in NSA_BR:
                        P.ts(dve, rc[:, 8 + r:9 + r], rc[:, 8 + r:9 + r], 0.0, ALU.mult, [rcB], [rcB])
                    fw.op(dve, lambda hh, bo=bo, r=r, h=h, onsa=onsa: hh.scalar_tensor_tensor(
                        out=onsa[:, h, :], in0=bo[:, 0:64], scalar=rc[:, 8 + r:9 + r], in1=onsa[:, h, :],
                        op0=ALU.mult, op1=ALU.add), [boB, rcB, onB], [onB])
                k0 = max(0, qi - 4)
                for kt in range(k0, qi + 1):
                    if kt == qi:
                        sel_fn = (0, -1, 1, ALU.is_ge)
                    elif kt == qi - 4:
                        sel_fn = (0, 1, -1, ALU.is_gt)
                    else:
                        sel_fn = None
                    unit(KwT[:, g, kt * 128:(kt + 1) * 128], [seqB], Qr, QB, None, sel_fn,
                         Vw[:, kt, g, :], [seqB], kt == k0, kt == qi)
                for r in range(4):
                    h = g * 4 + r
                    bo, boB = acc[r]
                    fw.op(dve, lambda hh, bo=bo, r=r: hh.reciprocal(out=rc[:, 12 + r:13 + r], in_=bo[:, 64:65]), [boB], [rcB])
                    P.tt(dve, rc[:, 12 + r:13 + r], rc[:, 12 + r:13 + r], gates[:, qi, h * 3 + 2:h * 3 + 3], ALU.mult, [rcB, seqB], [rcB])
                    if "w" not in NSA_BR:
                        P.ts(dve, rc[:, 12 + r:13 + r], rc[:, 12 + r:13 + r], 0.0, ALU.mult, [rcB], [rcB])
                    fw.op(dve, lambda hh, bo=bo, r=r, h=h, onsa=onsa: hh.scalar_tensor_tensor(
                        out=onsa[:, h, :], in0=bo[:, 0:64], scalar=rc[:, 12 + r:13 + r], in1=onsa[:, h, :],
                        op0=ALU.mult, op1=ALU.add), [boB, rcB, onB], [onB])
            of = onsa[:].rearrange("p h d -> p (h d)")
            if k.dbgt is not None and qi == DBG_QI and b == 0:
                dS = P.slot()
                fw.dma(sp, k.dbgt[:, 0:16], rc[:], [rcB], [], dS)
                fw.dma(sp, k.dbgt[:, 16:528], of, [onB], [], dS)
                fw.dma(sp, k.dbgt[:, 528:552], gates[:, qi, :], [seqB], [], dS)
            P.act(junk[:], of, AF.Square, [onB], [junkB, sttB], accum=stt[:, 0:1])
            P.rstd(stt[:, 1:2], stt[:, 0:1], 512, [sttB], [sttB])
            P.act(ob[:], of, AF.Copy, [onB, sttB], [obB], scale=stt[:, 1:2])
            fw.dma(sp, sc["mixed"][b, rows, 512:1024], ob[:], [obB], [], obS)
    P.close()


def nextbank_hi(k, st_):
    t, b = k.banks[4 + st_["nsc"] % 4]
    st_["nsc"] += 1
    return t, b


P4_MODE = "full"
PEER_DENSE = True


def phase4(k):
    fw, dr, sc, scB = k.fw, k.dr, k.sc, k.scB
    S, NSEQ, NT = k.S, k.NSEQ, k.NT
    nc = k.nc
    dve, act, pool, sp, pe = fw.dve, fw.act, fw.pool, fw.sp, fw.pe
    GC = 0.7978845608028654
    P0 = Ph(k, "p4")
    cB = Buf()
    cS = P0.slot()
    KmT = P0.sb([128, NSEQ, 4, 256], BF16, "KmT")
    Vx = P0.sb([128, NSEQ, 2, 4, 129], BF16, "Vx")
    memB = Buf()
    stage = (P0.sb([128, 1024], F32, "stage"), Buf(), P0.slot())
    gx = P0.sb([128, 128], F32, "gqx")
    gkx = P0.sb([128, 128], F32, "gkx")
    fw.dma(sp, gx[:], bcast_row(dr["xa_qk_norm"][0:1, :], 128), [], [cB], cS)
    fw.dma(sp, gkx[:], bcast_row(dr["xa_qk_norm"][1:2, :], 128), [], [cB], cS)
    P0.ts(dve, gx[:], gx[:], 128 ** -0.5, ALU.mult, [cB], [cB])
    junk = P0.sb([128, 1024], BF16, "junk")
    junkB = Buf()
    Pa = Ph(k, "p4a")
    g_mem = Pa.sb([128, 8], F32)
    fw.dma(sp, g_mem[:], dr["mem_norm"].rearrange("(c p) -> p c", p=128), [], [cB], cS)
    wkv = Pa.sb([128, 8, 1024], BF16, "wkv")
    wB = Buf()
    Pa.load_w(wkv, wB, dr["xa_wkv"], g_mem, stage=stage)
    mt_ = Pa.sb([128, 1024], F32, "mt")
    mtB = Buf()
    mS = Pa.slot()
    mn = Pa.sb([128, 1024], BF16, "mn")
    mnT = Pa.sb([128, 8, 128], BF16, "mnT")
    kvs = Pa.sb([128, 1024], F32, "kvs")
    sqm = Pa.sb([128, 512], F32, "sqm")
    kb_ = Pa.sb([128, 4, 128], BF16, "kb")
    st4 = Pa.sb([128, 16], F32, "st4")
    wk = Buf()
    fw.op(pool, lambda hh: hh.memset(Vx[:], 1.0), [], [memB])
    for b in range(NSEQ):
        for m in range(2):
            fw.dma(sp, mt_[:], dr["mem"][b, m * 128:(m + 1) * 128, :], [], [mtB], mS)
            Pa.act(junk[:], mt_[:], AF.Square, [mtB], [junkB, wk], accum=st4[:, 0:1])
            Pa.rstd(st4[:, 1:2], st4[:, 0:1], D, [wk], [wk])
            Pa.act(mn[:], mt_[:], AF.Copy, [mtB, wk], [wk], scale=st4[:, 1:2])
            bt, btB = nextbank(k)
            btv = bt[:].bitcast(BF16)
            for c in range(8):
                Pa.tr(btv[:, c * 128:(c + 1) * 128], mn[:, c * 128:(c + 1) * 128], [wk], [btB])
            Pa.cp(dve, mnT[:].rearrange("p c t -> p (c t)"), btv[:, 0:1024], [btB], [wk])
            for half in range(2):
                bk, bkB = nextbank(k)
                for c in range(8):
                    Pa.mm(bk[:, 0:512], mnT[:, c, :], wkv[:, c, half * 512:(half + 1) * 512], c == 0, c == 7, [wk, wB], [bkB])
                Pa.cp(act, kvs[:, half * 512:(half + 1) * 512], bk[:, 0:512], [bkB], [wk])
            Pa.tt(dve, sqm[:], kvs[:, 0:512], kvs[:, 0:512], ALU.mult, [wk], [wk])
            Pa.red(st4[:, 4:8], sqm[:].rearrange("p (h d) -> p h d", h=4), [wk], [wk])
            Pa.rstd(st4[:, 8:12], st4[:, 4:8], 128, [wk], [wk])
            k3 = kvs[:, 0:512].rearrange("p (h d) -> p h d", h=4)
            Pa.tt(dve, k3, k3, st4[:, 8:12].unsqueeze(2).to_broadcast([128, 4, 128]), ALU.mult, [wk], [wk])
            Pa.tt(dve, kb_[:], k3, gkx[:, :].unsqueeze(1).to_broadcast([128, 4, 128]), ALU.mult, [wk, cB], [wk])
            bt, btB = nextbank(k)
            btv = bt[:].bitcast(BF16)
            for h in range(4):
                Pa.tr(btv[:, h * 128:(h + 1) * 128], kb_[:, h, :], [wk], [btB])
            Pa.cp(dve, KmT[:, b, :, m * 128:(m + 1) * 128], btv[:, 0:512].rearrange("p (h t) -> p h t", h=4), [btB], [memB])
            Pa.cp(act, Vx[:, b, m, :, 0:128], kvs[:, 512:1024].rearrange("p (h d) -> p h d", h=4), [wk], [memB])
    Pa.close()
    P = Ph(k, "p4b")
    g_out = P.sb([128, 8], F32)
    g_xa = P.sb([128, 8], F32)
    g_ffn = P.sb([128, 8], F32)
    fw.dma(sp, g_out[:], dr["mix_out_norm"].rearrange("(c p) -> p c", p=128), [], [cB], cS)
    fw.dma(sp, g_xa[:], dr["xa_norm"].rearrange("(c p) -> p c", p=128), [], [cB], cS)
    fw.dma(sp, g_ffn[:], dr["ffn_norm"].rearrange("(c p) -> p c", p=128), [], [cB], cS)
    gf_rep = P.sb([128, 1024], F32, "gf_rep")
    fw.dma(sp, gf_rep[:], bcast_row(dr["ffn_norm"].rearrange("(o n) -> o n", o=1), 1024), [], [cB], cS)
    iot = P.sb([128, 32], F32, "iota")
    fw.dma(sp, iot[:], bcast_row(dr["iota"], 32), [], [cB], cS)
    w_out = P.sb([128, 8, 1024], BF16, "w_out")
    wq = P.sb([128, 8, 512], BF16, "wq")
    wo = P.sb([128, 4, 1024], BF16, "wo")
    pwq = P.sb([128, 8, 1024], BF16, "pwq")
    wB = Buf()
    P.load_w(w_out, wB, dr["w_out"], g_out, stage=stage)
    P.load_w(wq, wB, dr["xa_wq"], g_xa, stage=stage)
    P.load_w(wo, wB, dr["xa_wo"], None, stage=stage)
    P.load_w(pwq, wB, dr["peer_wq"], g_ffn, stage=stage)
    kst = stage[0][:].rearrange("p (h n) -> p h n", h=8)
    kB = stage[1]
    ksb = P.sb([128, 8, 128], BF16, "ksb")
    keysT = P.sb([128, 8, 128], BF16, "keysT")
    for p_ in range(2):
        fw.dma(sp, kst.rearrange("k h (p d) -> k h p d", p=2)[:, :, p_, :],
               dr["peer_keys"][:, p_].rearrange("h k d -> k h d"), [], [kB], stage[2])
    P.cp(dve, ksb[:], kst, [kB], [cB])
    for h in range(8):
        bt, btB = nextbank(k)
        btv = bt[:].bitcast(BF16)
        P.tr(btv[:, 0:128], ksb[:, h, :], [cB], [btB])
        P.cp(dve, keysT[:, h, :], btv[:, 0:128], [btB], [cB])
    def W2(shape, dt, name):
        return [(P.sb(shape, dt, name), Buf(), P.slot()) for _ in range(2)]

    xts = W2([128, 1024], F32, "xt")
    mxs = W2([128, 1024], BF16, "mx")
    outs = W2([128, 1024], F32, "outt")
    mT = P.sb([128, 8, 128], BF16, "mT")
    h1 = P.sb([128, 1024], F32, "h1")
    hx = P.sb([128, 1024], BF16, "hx")
    hxT = P.sb([128, 8, 128], BF16, "hxT")
    sqx = P.sb([128, 512], F32, "sqx")
    qx = P.sb([128, 4, 128], BF16, "qx")
    qxT = P.sb([128, 4, 128], BF16, "qxT")
    PTx = [P.sb([128, 4, 128], BF16, "PTx") for _ in range(2)]
    ox = P.sb([128, 4, 128], BF16, "ox")
    oxT = P.sb([128, 4, 128], BF16, "oxT")
    h2 = P.sb([128, 1024], F32, "h2")
    hn = P.sb([128, 1024], F32, "hn")
    hnb = P.sb([128, 1024], BF16, "hnb")
    hnT = P.sb([128, 8, 128], BF16, "hnT")
    qpT = P.sb([128, 8, 128], BF16, "qpT")
    s_sb = P.sb([128, 16, 128], F32, "s_sb")
    shr = P.sb([128, 2048], F32, "shr")
    s2 = shr[:].rearrange("p (j n) -> p j n", j=16)
    sv = P.sb([128, 16, 16], F32, "sv")
    si = P.sb([128, 16, 16], U32, "si")
    sif = P.sb([128, 16, 16], F32, "sif")
    cand = P.sb([128, 8, 16, 16], F32, "cand")
    cand2 = shr[:].rearrange("p (h a b) -> p h a b", h=8, a=16)
    top = P.sb([128, 8, 16], F32, "top")
    pos = P.sb([128, 8, 16], U32, "pos")
    posf = P.sb([128, 8, 16], F32, "posf")
    af = P.sb([128, 8, 16], F32, "af")
    bf = P.sb([128, 8, 16], F32, "bf")
    eq = shr[:].rearrange("p (h a b) -> p h a b", h=8, a=16)
    i1s = P.sb([128, 8, 16], F32, "i1s")
    i2s = P.sb([128, 8, 16], F32, "i2s")
    eidf = P.sb([128, 128], F32, "eidf")
    eidx = P.sb([128, 128], I32, "eidx")
    gsm = P.sb([128, 8, 16], F32, "gsm")
    zs = P.sb([128, 8], F32, "zs")
    actr = P.sb([128, 128], F32, "actr")
    ax2 = P.sb([128, 128], F32, "ax2")
    wgt = P.sb([128, 128], F32, "wgt")
    pacc = P.sb([128, 1024], F32, "pacc")
    junkf = P.sb([128, 1024], F32, "junkf")
    stt = P.sb([128, 16], F32, "stt")
    rcx = P.sb([128, 4], F32, "rcx")
    NG_ = 2
    Ugs = [(P.sb([128, 1024], F32, "Ug"), Buf(), P.slot()) for _ in range(NG_)]
    Vgs = [(P.sb([128, 1024], F32, "Vg"), Buf(), P.slot()) for _ in range(NG_)]
    wk = Buf()
    pk = Buf()
    actB = Buf()
    paccB = Buf()
    acc = k.banks[0:4]
    hi = {"n": 0}

    def hib():
        t, b_ = k.banks[4 + hi["n"] % 4]
        hi["n"] += 1
        return t, b_

    cnt = 0
    ng = 0
    for b in range(NSEQ):
        for i in range(NT):
            s_ = cnt % 2
            cnt += 1
            rows = slice(i * 128, (i + 1) * 128)
            xt, xtB, xtS = xts[s_]
            mx, mxB, mxS = mxs[s_]
            outt, outB, outS = outs[s_]
            fw.dma(sp, xt[:], dr["x"][b, rows, :], [], [xtB], xtS)
            fw.dma(sp, mx[:], sc["mixed"][b, rows, :], [scB["mixed"]], [mxB], mxS)
            bt, btB = hib()
            btv = bt[:].bitcast(BF16)
            for c in range(8):
                P.tr(btv[:, c * 128:(c + 1) * 128], mx[:, c * 128:(c + 1) * 128], [mxB], [btB])
            P.cp(dve, mT[:].rearrange("p c t -> p (c t)"), btv[:, 0:1024], [btB], [wk])
            for half in range(2):
                bo, boB = hib()
                for c in range(8):
                    P.mm(bo[:, 0:512], mT[:, c, :], w_out[:, c, half * 512:(half + 1) * 512], c == 0, c == 7, [wk, wB], [boB])
                P.tt(dve, h1[:, half * 512:(half + 1) * 512], bo[:, 0:512], xt[:, half * 512:(half + 1) * 512], ALU.add,
                     [boB, xtB], [wk])
            P.act(junk[:], h1[:], AF.Square, [wk], [junkB, wk], accum=stt[:, 0:1])
            P.rstd(stt[:, 1:2], stt[:, 0:1], D, [wk], [wk])
            P.act(hx[:], h1[:], AF.Copy, [wk], [wk], scale=stt[:, 1:2])
            bt, btB = hib()
            btv = bt[:].bitcast(BF16)
            for c in range(8):
                P.tr(btv[:, c * 128:(c + 1) * 128], hx[:, c * 128:(c + 1) * 128], [wk], [btB])
            P.cp(dve, hxT[:].rearrange("p c t -> p (c t)"), btv[:, 0:1024], [btB], [wk])
            bq, bqB = hib()
            for c in range(8):
                P.mm(bq[:, 0:512], hxT[:, c, :], wq[:, c, :], c == 0, c == 7, [wk, wB], [bqB])
            P.act(sqx[:], bq[:, 0:512], AF.Square, [bqB], [wk])
            P.red(stt[:, 4:8], sqx[:].rearrange("p (h d) -> p h d", h=4), [wk], [wk])
            P.rstd(stt[:, 8:12], stt[:, 4:8], 128, [wk], [wk])
            q3 = sqx[:].rearrange("p (h d) -> p h d", h=4)
            P.tt(dve, q3, bq[:, 0:512].rearrange("p (h d) -> p h d", h=4),
                 stt[:, 8:12].unsqueeze(2).to_broadcast([128, 4, 128]), ALU.mult, [bqB, wk], [wk])
            P.tt(dve, qx[:], q3, gx[:, :].unsqueeze(1).to_broadcast([128, 4, 128]), ALU.mult, [wk, cB], [wk])
            bt, btB = hib()
            btv = bt[:].bitcast(BF16)
            for h in range(4):
                P.tr(btv[:, h * 128:(h + 1) * 128], qx[:, h, :], [wk], [btB])
            P.cp(dve, qxT[:].rearrange("p h t -> p (h t)"), btv[:, 0:512], [btB], [wk])
            for m in range(2):
                bs, bsB = hib()
                for h in range(4):
                    P.mm(bs[:, h * 128:(h + 1) * 128], KmT[:, b, h, m * 128:(m + 1) * 128], qxT[:, h, :], True, True,
                         [memB, wk], [bsB])
                P.act(PTx[m][:].rearrange("p h q -> p (h q)"), bs[:, 0:512], AF.Exp, [bsB], [wk])
                for h in range(4):
                    bo, boB = acc[h]
                    P.mm(bo[:, 0:129], PTx[m][:, h, :], Vx[:, b, m, h, :], m == 0, m == 1, [wk, memB], [boB])
            for h in range(4):
                bo, boB = acc[h]
                fw.op(dve, lambda hh, bo=bo, h=h: hh.reciprocal(out=rcx[:, h:h + 1], in_=bo[:, 128:129]), [boB], [wk])
                P.ts(dve, ox[:, h, :], bo[:, 0:128], rcx[:, h:h + 1], ALU.mult, [boB, wk], [wk])
            bt, btB = hib()
            btv = bt[:].bitcast(BF16)
            for h in range(4):
                P.tr(btv[:, h * 128:(h + 1) * 128], ox[:, h, :], [wk], [btB])
            P.cp(dve, oxT[:].rearrange("p h t -> p (h t)"), btv[:, 0:512], [btB], [wk])
            for half in range(2):
                bo, boB = hib()
                for c in range(4):
                    P.mm(bo[:, 0:512], oxT[:, c, :], wo[:, c, half * 512:(half + 1) * 512], c == 0, c == 3, [wk, wB], [boB])
                P.tt(dve, h2[:, half * 512:(half + 1) * 512], bo[:, 0:512], h1[:, half * 512:(half + 1) * 512], ALU.add,
                     [boB, wk], [wk])
            if P4_MODE == "xa":
                P.cp(dve, outt[:], h2[:], [wk], [outB])
                fw.dma(sp, k.out[b, rows, :], outt[:], [outB], [], outS)
                continue
            P.act(junk[:], h2[:], AF.Square, [wk], [junkB, wk], accum=stt[:, 2:3])
            P.rstd(stt[:, 3:4], stt[:, 2:3], D, [wk], [wk])
            P.act(hnb[:], h2[:], AF.Copy, [wk], [wk], scale=stt[:, 3:4])
            P.ts(dve, hn[:], h2[:], stt[:, 3:4], ALU.mult, [wk], [actB])
            P.tt(dve, hn[:], hn[:], gf_rep[:], ALU.mult, [actB, cB], [actB])
            bt, btB = hib()
            btv = bt[:].bitcast(BF16)
            for c in range(8):
                P.tr(btv[:, c * 128:(c + 1) * 128], hnb[:, c * 128:(c + 1) * 128], [wk], [btB])
            P.cp(dve, hnT[:].rearrange("p c t -> p (c t)"), btv[:, 0:1024], [btB], [wk])
            for hh4 in range(2):
                bq, bqB = hib()
                for h in range(4):
                    hh_ = hh4 * 4 + h
                    for c in range(8):
                        P.mm(bq[:, h * 128:(h + 1) * 128], pwq[:, c, hh_ * 128:(hh_ + 1) * 128], hnT[:, c, :], c == 0, c == 7,
                             [wk, wB], [bqB])
                P.cp(act, qpT[:, hh4 * 4:(hh4 + 1) * 4, :].rearrange("p h t -> p (h t)"), bq[:, 0:512], [bqB], [pk])
            s_sb4 = s_sb[:].rearrange("p (h t) n -> p h t n", t=2)
            for hh4 in range(2):
                for p_ in range(2):
                    bs, bsB = hib()
                    for jj in range(4):
                        h = hh4 * 4 + jj
                        P.mm(bs[:, jj * 128:(jj + 1) * 128], qpT[p_ * 64:(p_ + 1) * 64, h, :],
                             keysT[p_ * 64:(p_ + 1) * 64, h, :], True, True, [pk, cB], [bsB])
                    P.cp(act, s_sb4[:, hh4 * 4:(hh4 + 1) * 4, p_, :], bs[:, 0:512].rearrange("p (j n) -> p j n", j=4), [bsB], [pk])
            for j in range(16):
                fw.op(dve, lambda hh, j=j: hh.max(out=sv[:, j, 0:8], in_=s_sb[:, j, :]), [pk], [pk])
                fw.op(dve, lambda hh, j=j: hh.max_index(out=si[:, j, 0:8], in_max=sv[:, j, 0:8], in_values=s_sb[:, j, :]), [pk], [pk])
                fw.op(dve, lambda hh, j=j: hh.match_replace(out=s2[:, j, :], in_to_replace=sv[:, j, 0:8], in_values=s_sb[:, j, :],
                                                            imm_value=-3.0e38), [pk], [pk])
                fw.op(dve, lambda hh, j=j: hh.max(out=sv[:, j, 8:16], in_=s2[:, j, :]), [pk], [pk])
                fw.op(dve, lambda hh, j=j: hh.max_index(out=si[:, j, 8:16], in_max=sv[:, j, 8:16], in_values=s2[:, j, :]), [pk], [pk])
            P.cp(dve, sif[:], si[:], [pk], [pk])
            sv4 = sv[:].rearrange("p (h t) n -> p h t n", t=2)
            sif4 = sif[:].rearrange("p (h t) n -> p h t n", t=2)
            P.tt(dve, cand[:], sv4[:, :, 0, :].unsqueeze(3).to_broadcast([128, 8, 16, 16]),
                 sv4[:, :, 1, :].unsqueeze(2).to_broadcast([128, 8, 16, 16]), ALU.add, [pk], [pk])
            for h in range(8):
                ch = cand[:, h].rearrange("p a b -> p (a b)")
                c2h = cand2[:, h].rearrange("p a b -> p (a b)")
                fw.op(dve, lambda hh, h=h, ch=ch: hh.max(out=top[:, h, 0:8], in_=ch), [pk], [pk])
                fw.op(dve, lambda hh, h=h, ch=ch: hh.max_index(out=pos[:, h, 0:8], in_max=top[:, h, 0:8], in_values=ch), [pk], [pk])
                fw.op(dve, lambda hh, h=h, ch=ch, c2h=c2h: hh.match_replace(out=c2h, in_to_replace=top[:, h, 0:8], in_values=ch,
                                                                             imm_value=-3.0e38), [pk], [pk])
                fw.op(dve, lambda hh, h=h, c2h=c2h: hh.max(out=top[:, h, 8:16], in_=c2h), [pk], [pk])
                fw.op(dve, lambda hh, h=h, c2h=c2h: hh.max_index(out=pos[:, h, 8:16], in_max=top[:, h, 8:16], in_values=c2h), [pk], [pk])
            P.cp(dve, posf[:], pos[:], [pk], [pk])
            P.tt(dve, eq[:, :, :, 0:15], posf[:, :, :].unsqueeze(3).to_broadcast([128, 8, 16, 15]),
                 iot[:, 16:31].unsqueeze(1).unsqueeze(1).to_broadcast([128, 8, 16, 15]), ALU.is_ge, [pk, cB], [pk])
            P.red(af[:], eq[:, :, :, 0:15], [pk], [pk])
            fw.op(dve, lambda hh: hh.scalar_tensor_tensor(out=bf[:], in0=af[:], scalar=-16.0, in1=posf[:], op0=ALU.mult, op1=ALU.add),
                  [pk], [pk])
            for (src, t_, dst) in ((af, 0, i1s), (bf, 1, i2s)):
                P.tt(dve, eq[:], src[:, :, :].unsqueeze(3).to_broadcast([128, 8, 16, 16]),
                     iot[:, 0:16].unsqueeze(1).unsqueeze(1).to_broadcast([128, 8, 16, 16]), ALU.is_equal, [pk, cB], [pk])
                P.tt(dve, eq[:], eq[:], sif4[:, :, t_, :].unsqueeze(2).to_broadcast([128, 8, 16, 16]), ALU.mult, [pk], [pk])
                P.red(dst[:], eq[:], [pk], [pk])
            fw.op(dve, lambda hh: hh.scalar_tensor_tensor(out=eidf[:].rearrange("p (h k) -> p h k", h=8), in0=i1s[:], scalar=128.0,
                                                          in1=i2s[:], op0=ALU.mult, op1=ALU.add), [pk], [pk])
            P.ts(dve, eidx[:], eidf[:], 0.0, ALU.add, [pk], [pk])
            if P4_MODE == "route":
                P.cp(dve, outt[:, 0:128], eidf[:], [pk], [outB])
                P.cp(dve, outt[:, 128:256], top[:].rearrange("p h k -> p (h k)"), [pk], [outB])
                P.cp(dve, outt[:, 256:1024], h2[:, 256:1024], [wk], [outB])
                fw.dma(sp, k.out[b, rows, :], outt[:], [outB], [], outS)
                continue
            P.tt(dve, gsm[:], top[:], top[:, :, 0:1].to_broadcast([128, 8, 16]), ALU.subtract, [pk], [pk])
            P.act(gsm[:], gsm[:], AF.Exp, [pk], [pk])
            P.red(zs[:], gsm[:], [pk], [pk])
            fw.op(dve, lambda hh: hh.reciprocal(out=zs[:], in_=zs[:]), [pk], [pk])
            P.tt(dve, gsm[:], gsm[:], zs[:, :].unsqueeze(2).to_broadcast([128, 8, 16]), ALU.mult, [pk], [pk])
            for c in range(128):
                Ug, UgB, UgS = Ugs[ng % NG_]
                ng += 1
                fw.dma(pool, None, None, [pk], [UgB], UgS,
                       fn=lambda hh, Ug=Ug, c=c: hh.indirect_dma_start(
                           out=Ug[:], out_offset=None, in_=dr["peer_u"],
                           in_offset=bass.IndirectOffsetOnAxis(ap=eidx[:, c:c + 1], axis=0)))
                fw.op(dve, lambda hh, Ug=Ug, c=c: hh.scalar_tensor_tensor(
                    out=junkf[:], in0=Ug[:], scalar=1.0, in1=hn[:], op0=ALU.mult, op1=ALU.mult,
                    accum_out=actr[:, c:c + 1]), [UgB, actB], [actB])
            P.tt(dve, ax2[:], actr[:], actr[:], ALU.mult, [actB], [actB])
            P.ts(dve, ax2[:], ax2[:], 0.044715, ALU.mult, [actB], [actB], s2=1.0, op1=ALU.add)
            P.tt(dve, ax2[:], ax2[:], actr[:], ALU.mult, [actB], [actB])
            P.act(ax2[:], ax2[:], AF.Sigmoid, [actB], [actB], scale=2.0 * GC)
            P.tt(dve, ax2[:], ax2[:], actr[:], ALU.mult, [actB], [actB])
            P.tt(dve, wgt[:], ax2[:], gsm[:].rearrange("p h k -> p (h k)"), ALU.mult, [actB, pk], [actB])
            for c in range(128):
                Vg, VgB, VgS = Vgs[c % NG_]
                fw.dma(pool, None, None, [pk], [VgB], VgS,
                       fn=lambda hh, Vg=Vg, c=c: hh.indirect_dma_start(
                           out=Vg[:], out_offset=None, in_=dr["peer_v"],
                           in_offset=bass.IndirectOffsetOnAxis(ap=eidx[:, c:c + 1], axis=0)))
                if c == 0:
                    P.ts(dve, pacc[:], Vg[:], wgt[:, 0:1], ALU.mult, [VgB, actB], [paccB])
                else:
                    fw.op(dve, lambda hh, Vg=Vg, c=c: hh.scalar_tensor_tensor(
                        out=pacc[:], in0=Vg[:], scalar=wgt[:, c:c + 1], in1=pacc[:], op0=ALU.mult, op1=ALU.add),
                        [VgB, actB, paccB], [paccB])
            P.tt(dve, outt[:], pacc[:], h2[:], ALU.add, [paccB, wk], [outB])
            fw.dma(sp, k.out[b, rows, :], outt[:], [outB], [], outS)
    P.close()
    P0.close()


def phase4d(k):
    fw, dr, sc, scB = k.fw, k.dr, k.sc, k.scB
    S, NSEQ, NT = k.S, k.NSEQ, k.NT
    nc = k.nc
    dve, act, pool, sp, pe = fw.dve, fw.act, fw.pool, fw.sp, fw.pe
    GC = 0.7978845608028654
    P0 = Ph(k, "p4")
    cB = Buf()
    cS = P0.slot()
    KmT = P0.sb([128, NSEQ, 4, 256], BF16, "KmT")
    Vx = P0.sb([128, NSEQ, 2, 4, 129], BF16, "Vx")
    memB = Buf()
    stage = (P0.sb([128, 1024], F32, "stage"), Buf(), P0.slot())
    gx = P0.sb([128, 128], F32, "gqx")
    gkx = P0.sb([128, 128], F32, "gkx")
    fw.dma(sp, gx[:], bcast_row(dr["xa_qk_norm"][0:1, :], 128), [], [cB], cS)
    fw.dma(sp, gkx[:], bcast_row(dr["xa_qk_norm"][1:2, :], 128), [], [cB], cS)
    P0.ts(dve, gx[:], gx[:], 128 ** -0.5, ALU.mult, [cB], [cB])
    junkB = Buf()
    Pa = Ph(k, "p4a")
    junk = Pa.sb([128, 1024], BF16, "junk")
    g_mem = Pa.sb([128, 8], F32)
    fw.dma(sp, g_mem[:], dr["mem_norm"].rearrange("(c p) -> p c", p=128), [], [cB], cS)
    wkv = Pa.sb([128, 8, 1024], BF16, "wkv")
    wB = Buf()
    Pa.load_w(wkv, wB, dr["xa_wkv"], g_mem, stage=stage)
    mt_ = Pa.sb([128, 1024], F32, "mt")
    mtB = Buf()
    mS = Pa.slot()
    mn = Pa.sb([128, 1024], BF16, "mn")
    mnT = Pa.sb([128, 8, 128], BF16, "mnT")
    kvs = Pa.sb([128, 1024], F32, "kvs")
    sqm = Pa.sb([128, 512], F32, "sqm")
    kb_ = Pa.sb([128, 4, 128], BF16, "kb")
    st4 = Pa.sb([128, 16], F32, "st4")
    wk = Buf()
    fw.op(pool, lambda hh: hh.memset(Vx[:], 1.0), [], [memB])
    for b in range(NSEQ):
        for m in range(2):
            fw.dma(sp, mt_[:], dr["mem"][b, m * 128:(m + 1) * 128, :], [], [mtB], mS)
            Pa.act(junk[:], mt_[:], AF.Square, [mtB], [junkB, wk], accum=st4[:, 0:1])
            Pa.rstd(st4[:, 1:2], st4[:, 0:1], D, [wk], [wk])
            Pa.act(mn[:], mt_[:], AF.Copy, [mtB, wk], [wk], scale=st4[:, 1:2])
            bt, btB = nextbank(k)
            btv = bt[:].bitcast(BF16)
            for c in range(8):
                Pa.tr(btv[:, c * 128:(c + 1) * 128], mn[:, c * 128:(c + 1) * 128], [wk], [btB])
            Pa.cp(dve, mnT[:].rearrange("p c t -> p (c t)"), btv[:, 0:1024], [btB], [wk])
            for half in range(2):
                bk, bkB = nextbank(k)
                for c in range(8):
                    Pa.mm(bk[:, 0:512], mnT[:, c, :], wkv[:, c, half * 512:(half + 1) * 512], c == 0, c == 7, [wk, wB], [bkB])
                Pa.cp(act, kvs[:, half * 512:(half + 1) * 512], bk[:, 0:512], [bkB], [wk])
            Pa.tt(dve, sqm[:], kvs[:, 0:512], kvs[:, 0:512], ALU.mult, [wk], [wk])
            Pa.red(st4[:, 4:8], sqm[:].rearrange("p (h d) -> p h d", h=4), [wk], [wk])
            Pa.rstd(st4[:, 8:12], st4[:, 4:8], 128, [wk], [wk])
            k3 = kvs[:, 0:512].rearrange("p (h d) -> p h d", h=4)
            Pa.tt(dve, k3, k3, st4[:, 8:12].unsqueeze(2).to_broadcast([128, 4, 128]), ALU.mult, [wk], [wk])
            Pa.tt(dve, kb_[:], k3, gkx[:, :].unsqueeze(1).to_broadcast([128, 4, 128]), ALU.mult, [wk, cB], [wk])
            bt, btB = nextbank(k)
            btv = bt[:].bitcast(BF16)
            for h in range(4):
                Pa.tr(btv[:, h * 128:(h + 1) * 128], kb_[:, h, :], [wk], [btB])
            Pa.cp(dve, KmT[:, b, :, m * 128:(m + 1) * 128], btv[:, 0:512].rearrange("p (h t) -> p h t", h=4), [btB], [memB])
            Pa.cp(act, Vx[:, b, m, :, 0:128], kvs[:, 512:1024].rearrange("p (h d) -> p h d", h=4), [wk], [memB])
    gfr = Pa.sb([128, 1024], F32, "gfr")
    fw.dma(sp, gfr[:], bcast_row(dr["ffn_norm"].rearrange("(o n) -> o n", o=1), 1024), [], [cB], cS)
    usts = [(Pa.sb([128, 1024], F32, "ust"), Buf(), Pa.slot()) for _ in range(2)]
    vsts = [(Pa.sb([128, 1024], F32, "vst"), Buf(), Pa.slot()) for _ in range(2)]
    ubs = [(Pa.sb([128, 1024], BF16, "ub"), Buf()) for _ in range(2)]
    utss = [(Pa.sb([128, 8, 128], BF16, "uts"), Buf(), Pa.slot()) for _ in range(2)]
    vb2s = [(Pa.sb([128, 1024], BF16, "vb2"), Buf(), Pa.slot()) for _ in range(2)]
    for i in range(128):
        ust, ustB, ustS = usts[i % 2]
        vst, vstB, vstS = vsts[i % 2]
        ub, ubB = ubs[i % 2]
        uts, utsB, utsS = utss[i % 2]
        vb2, vb2B, vb2S = vb2s[i % 2]
        fw.dma(sp, ust[:], dr["peer_u"][i * 128:(i + 1) * 128, :], [], [ustB], ustS)
        fw.dma(sp, vst[:], dr["peer_v"][i * 128:(i + 1) * 128, :], [], [vstB], vstS)
        Pa.tt(dve, ub[:], ust[:], gfr[:], ALU.mult, [ustB, cB], [ubB])
        bt, btB = nextbank(k)
        btv = bt[:].bitcast(BF16)
        for c in range(8):
            Pa.tr(btv[:, c * 128:(c + 1) * 128], ub[:, c * 128:(c + 1) * 128], [ubB], [btB])
        Pa.cp(act, uts[:].rearrange("p c j -> p (c j)"), btv[:, 0:1024], [btB], [utsB])
        fw.dma(sp, k.sc["uT"][i], uts[:], [utsB], [], utsS)
        Pa.cp(pool, vb2[:], vst[:], [vstB], [vb2B])
        fw.dma(sp, k.sc["vbf"][i * 128:(i + 1) * 128, :], vb2[:], [vb2B], [], vb2S)
    Pa.close()
    P = Ph(k, "p4b")
    g_out = P.sb([128, 8], F32)
    g_xa = P.sb([128, 8], F32)
    g_ffn = P.sb([128, 8], F32)
    fw.dma(sp, g_out[:], dr["mix_out_norm"].rearrange("(c p) -> p c", p=128), [], [cB], cS)
    fw.dma(sp, g_xa[:], dr["xa_norm"].rearrange("(c p) -> p c", p=128), [], [cB], cS)
    fw.dma(sp, g_ffn[:], dr["ffn_norm"].rearrange("(c p) -> p c", p=128), [], [cB], cS)
    iot = P.sb([128, 32], F32, "iota")
    fw.dma(sp, iot[:], bcast_row(dr["iota"], 32), [], [cB], cS)
    w_out = P.sb([128, 8, 1024], BF16, "w_out")
    wq = P.sb([128, 8, 512], BF16, "wq")
    wo = P.sb([128, 4, 1024], BF16, "wo")
    pwq = P.sb([128, 8, 1024], BF16, "pwq")
    wB = Buf()
    P.load_w(w_out, wB, dr["w_out"], g_out, stage=stage)
    P.load_w(wq, wB, dr["xa_wq"], g_xa, stage=stage)
    P.load_w(wo, wB, dr["xa_wo"], None, stage=stage)
    P.load_w(pwq, wB, dr["peer_wq"], g_ffn, stage=stage)
    kst = stage[0][:].rearrange("p (h n) -> p h n", h=8)
    kB = stage[1]
    hx = P.sb([128, 1024], BF16, "hx")
    wk = Buf()
    ksb = hx[:].rearrange("p (h n) -> p h n", h=8)
    keysT = P.sb([128, 8, 128], BF16, "keysT")
    for p_ in range(2):
        fw.dma(sp, kst.rearrange("k h (p d) -> k h p d", p=2)[:, :, p_, :],
               dr["peer_keys"][:, p_].rearrange("h k d -> k h d"), [], [kB], stage[2])
    P.cp(dve, ksb, kst, [kB], [cB, wk])
    for h in range(8):
        bt, btB = nextbank(k)
        btv = bt[:].bitcast(BF16)
        P.tr(btv[:, 0:128], ksb[:, h, :], [cB, wk], [btB])
        P.cp(dve, keysT[:, h, :], btv[:, 0:128], [btB], [cB])
    def W2(shape, dt, name):
        return [(P.sb(shape, dt, name), Buf(), P.slot()) for _ in range(2)]

    xts = W2([128, 1024], F32, "xt")
    mxs = [(P.sb([128, 1024], BF16, "mx"), Buf(), P.slot())] * 2
    outs = W2([128, 1024], F32, "outt")
    mT = P.sb([128, 8, 128], BF16, "mT")
    h1 = stage[0]
    hxT = mT
    qx = P.sb([128, 4, 128], BF16, "qx")
    qxT = P.sb([128, 4, 128], BF16, "qxT")
    PTx = [P.sb([128, 4, 128], BF16, "PTx") for _ in range(2)]
    ox = qx
    oxT = qxT
    h2 = P.sb([128, 1024], F32, "h2")
    h2S = P.slot()
    hnTgs = [P.sb([128, 8, 256], BF16, "hnTg") for _ in range(2)]
    grpBs = [Buf(), Buf()]
    hnb = hx
    hnT = mT
    qpT = P.sb([128, 8, 128], BF16, "qpT")
    s_sb = P.sb([128, 16, 128], F32, "s_sb")
    shr = P.sb([128, 2048], F32, "shr")
    sqx = shr[:, 0:512]
    junk = shr[:].bitcast(BF16)[:, 2048:3072]
    s2 = shr[:].rearrange("p (j n) -> p j n", j=16)
    sv = P.sb([128, 16, 16], F32, "sv")
    si = P.sb([128, 16, 16], U32, "si")
    sif = P.sb([128, 16, 16], F32, "sif")
    cand = s_sb[:].rearrange("p j n -> p (j n)").rearrange("p (h a b) -> p h a b", h=8, a=16)
    cand2 = shr[:].rearrange("p (h a b) -> p h a b", h=8, a=16)
    top = P.sb([128, 8, 16], F32, "top")
    pos = P.sb([128, 8, 16], U32, "pos")
    posf = P.sb([128, 8, 16], F32, "posf")
    af = P.sb([128, 8, 16], F32, "af")
    bf = P.sb([128, 8, 16], F32, "bf")
    eq = shr[:].rearrange("p (h a b) -> p h a b", h=8, a=16)
    i1s = P.sb([128, 8, 16], F32, "i1s")
    i2s = P.sb([128, 8, 16], F32, "i2s")
    gsm = P.sb([128, 8, 16], F32, "gsm")
    zs = P.sb([128, 8], F32, "zs")
    stt = P.sb([128, 16], F32, "stt")
    rcx = P.sb([128, 4], F32, "rcx")
    G_all = P.sb([128, 128, 256], BF16, "G_all")
    ITJ = P.sb([128, 3, 256], F32, "ITJ")
    iorow = P.sb([128, 128], F32, "iorow")
    fw.dma(sp, iorow[:], bcast_row(dr["iota128"], 128), [], [cB], cS)
    OHs = [(P.sb([128, 2, 128], BF16, "OH"), Buf()) for _ in range(4)]
    uTbs = [(P.sb([128, 8, 128], BF16, "uTb"), Buf(), P.slot()) for _ in range(2)]
    vbfs = [(P.sb([128, 1024], BF16, "vbf"), Buf(), P.slot()) for _ in range(2)]
    ges = [(P.sb([128, 256], BF16, "ge"), Buf()) for _ in range(2)]
    Wts = [(P.sb([128, 256], BF16, "Wt"), Buf()) for _ in range(2)]
    itjB = Buf()
    jBs = [Buf() for _ in range(16)]
    h2sB = Buf()
    GB = Buf()
    st4_ = {"cnt": 0}
    pk = Buf()
    actB = Buf()
    paccB = Buf()
    acc = k.banks[0:4]
    hi = {"n": 0}

    def hib():
        t, b_ = k.banks[4 + hi["n"] % 4]
        hi["n"] += 1
        return t, b_

    def front(b, i, tt, gp):
        hnTg = hnTgs[gp]
        grpB = grpBs[gp]
        s_ = st4_["cnt"] % 2
        st4_["cnt"] += 1
        rows = slice(i * 128, (i + 1) * 128)
        xt, xtB, xtS = xts[s_]
        mx, mxB, mxS = mxs[s_]
        outt, outB, outS = outs[s_]
        fw.dma(sp, xt[:], dr["x"][b, rows, :], [], [xtB], xtS)
        fw.dma(sp, mx[:], sc["mixed"][b, rows, :], [scB["mixed"]], [mxB], mxS)
        bt, btB = hib()
        btv = bt[:].bitcast(BF16)
        for c in range(8):
            P.tr(btv[:, c * 128:(c + 1) * 128], mx[:, c * 128:(c + 1) * 128], [mxB], [btB])
        P.cp(dve, mT[:].rearrange("p c t -> p (c t)"), btv[:, 0:1024], [btB], [wk])
        for half in range(2):
            bo, boB = hib()
            for c in range(8):
                P.mm(bo[:, 0:512], mT[:, c, :], w_out[:, c, half * 512:(half + 1) * 512], c == 0, c == 7, [wk, wB], [boB])
            P.tt(dve, h1[:, half * 512:(half + 1) * 512], bo[:, 0:512], xt[:, half * 512:(half + 1) * 512], ALU.add,
                 [boB, xtB], [wk])
        yield
        P.act(junk[:], h1[:], AF.Square, [wk], [junkB, wk, pk], accum=stt[:, 0:1])
        P.rstd(stt[:, 1:2], stt[:, 0:1], D, [wk], [wk])
        P.act(hx[:], h1[:], AF.Copy, [wk], [wk], scale=stt[:, 1:2])
        bt, btB = hib()
        btv = bt[:].bitcast(BF16)
        for c in range(8):
            P.tr(btv[:, c * 128:(c + 1) * 128], hx[:, c * 128:(c + 1) * 128], [wk], [btB])
        P.cp(dve, hxT[:].rearrange("p c t -> p (c t)"), btv[:, 0:1024], [btB], [wk])
        yield
        bq, bqB = hib()
        for c in range(8):
            P.mm(bq[:, 0:512], hxT[:, c, :], wq[:, c, :], c == 0, c == 7, [wk, wB], [bqB])
        P.act(sqx, bq[:, 0:512], AF.Square, [bqB], [wk, pk])
        P.red(stt[:, 4:8], sqx.rearrange("p (h d) -> p h d", h=4), [wk], [wk])
        P.rstd(stt[:, 8:12], stt[:, 4:8], 128, [wk], [wk])
        q3 = sqx.rearrange("p (h d) -> p h d", h=4)
        P.tt(dve, q3, bq[:, 0:512].rearrange("p (h d) -> p h d", h=4),
             stt[:, 8:12].unsqueeze(2).to_broadcast([128, 4, 128]), ALU.mult, [bqB, wk], [wk])
        P.tt(dve, qx[:], q3, gx[:, :].unsqueeze(1).to_broadcast([128, 4, 128]), ALU.mult, [wk, cB], [wk])
        bt, btB = hib()
        btv = bt[:].bitcast(BF16)
        for h in range(4):
            P.tr(btv[:, h * 128:(h + 1) * 128], qx[:, h, :], [wk], [btB])
        P.cp(dve, qxT[:].rearrange("p h t -> p (h t)"), btv[:, 0:512], [btB], [wk])
        yield
        for m in range(2):
            bs, bsB = hib()
            for h in range(4):
                P.mm(bs[:, h * 128:(h + 1) * 128], KmT[:, b, h, m * 128:(m + 1) * 128], qxT[:, h, :], True, True,
                     [memB, wk], [bsB])
            P.act(PTx[m][:].rearrange("p h q -> p (h q)"), bs[:, 0:512], AF.Exp, [bsB], [wk])
        yield
        xb = [hib(), hib()]
        for h in range(4):
            bo, boB = xb[h // 2]
            c0 = (h % 2) * 256
            for m in range(2):
                P.mm(bo[:, c0:c0 + 129], PTx[m][:, h, :], Vx[:, b, m, h, :], m == 0, m == 1, [wk, memB], [boB])
        for h in range(4):
            bo, boB = xb[h // 2]
            c0 = (h % 2) * 256
            fw.op(dve, lambda hh, bo=bo, h=h, c0=c0: hh.reciprocal(out=rcx[:, h:h + 1], in_=bo[:, c0 + 128:c0 + 129]), [boB], [wk])
            P.ts(dve, ox[:, h, :], bo[:, c0:c0 + 128], rcx[:, h:h + 1], ALU.mult, [boB, wk], [wk])
        yield
        bt, btB = hib()
        btv = bt[:].bitcast(BF16)
        for h in range(4):
            P.tr(btv[:, h * 128:(h + 1) * 128], ox[:, h, :], [wk], [btB])
        P.cp(dve, oxT[:].rearrange("p h t -> p (h t)"), btv[:, 0:512], [btB], [wk])
        for half in range(2):
            bo, boB = hib()
            for c in range(4):
                P.mm(bo[:, 0:512], oxT[:, c, :], wo[:, c, half * 512:(half + 1) * 512], c == 0, c == 3, [wk, wB], [boB])
            P.tt(dve, h2[:, half * 512:(half + 1) * 512], bo[:, 0:512], h1[:, half * 512:(half + 1) * 512], ALU.add,
                 [boB, wk], [wk])
        if P4_MODE == "xa":
            P.cp(dve, outt[:], h2[:], [wk], [outB])
            fw.dma(sp, k.out[b, rows, :], outt[:], [outB], [], outS)
            return
        fw.dma(sp, k.sc["h2s"][b, rows, :], h2[:], [wk], [h2sB], h2S)
        yield
        P.act(junk[:], h2[:], AF.Square, [wk], [junkB, wk, pk], accum=stt[:, 2:3])
        P.rstd(stt[:, 3:4], stt[:, 2:3], D, [wk], [wk])
        P.act(hnb[:], h2[:], AF.Copy, [wk], [wk], scale=stt[:, 3:4])
        bt, btB = hib()
        btv = bt[:].bitcast(BF16)
        for c in range(8):
            P.tr(btv[:, c * 128:(c + 1) * 128], hnb[:, c * 128:(c + 1) * 128], [wk], [btB])
        P.cp(dve, hnTg[:, :, tt * 128:(tt + 1) * 128], btv[:, 0:1024].rearrange("p (c t) -> p c t", c=8), [btB], [grpB])
        yield
        for hh4 in range(2):
            bq, bqB = hib()
            for h in range(4):
                hh_ = hh4 * 4 + h
                for c in range(8):
                    P.mm(bq[:, h * 128:(h + 1) * 128], pwq[:, c, hh_ * 128:(hh_ + 1) * 128],
                         hnTg[:, c, tt * 128:(tt + 1) * 128], c == 0, c == 7, [grpB, wB], [bqB])
            P.cp(act, qpT[:, hh4 * 4:(hh4 + 1) * 4, :].rearrange("p h t -> p (h t)"), bq[:, 0:512], [bqB], [pk])
        if P4_MODE == "r0c":
            return
        yield
        s_sb4 = s_sb[:].rearrange("p (h t) n -> p h t n", t=2)
        for hh4 in range(2):
            for p_ in range(2):
                bs, bsB = hib()
                for jj in range(4):
                    h = hh4 * 4 + jj
                    P.mm(bs[:, jj * 128:(jj + 1) * 128], qpT[p_ * 64:(p_ + 1) * 64, h, :],
                         keysT[p_ * 64:(p_ + 1) * 64, h, :], True, True, [pk, cB], [bsB])
                P.cp(act, s_sb4[:, hh4 * 4:(hh4 + 1) * 4, p_, :], bs[:, 0:512].rearrange("p (j n) -> p j n", j=4), [bsB], [pk])
        if P4_MODE == "r1":
            return
        for j in range(16):
            fw.op(dve, lambda hh, j=j: hh.max(out=sv[:, j, 0:8], in_=s_sb[:, j, :]), [pk], [jBs[j]])
        yield
        for j in range(16):
            fw.op(dve, lambda hh, j=j: hh.max_index(out=si[:, j, 0:8], in_max=sv[:, j, 0:8], in_values=s_sb[:, j, :]), [pk, jBs[j]], [jBs[j]])
        yield
        for j in range(16):
            fw.op(dve, lambda hh, j=j: hh.match_replace(out=s2[:, j, :], in_to_replace=sv[:, j, 0:8], in_values=s_sb[:, j, :],
                                                        imm_value=-3.0e38), [pk, jBs[j]], [jBs[j]])
        yield
        for j in range(16):
            fw.op(dve, lambda hh, j=j: hh.max(out=sv[:, j, 8:16], in_=s2[:, j, :]), [jBs[j]], [jBs[j]])
        yield
        for j in range(16):
            fw.op(dve, lambda hh, j=j: hh.max_index(out=si[:, j, 8:16], in_max=sv[:, j, 8:16], in_values=s2[:, j, :]), [jBs[j]], [jBs[j]])
        yield
        P.cp(dve, sif[:], si[:], [pk] + jBs, [pk] + jBs)
        sv4 = sv[:].rearrange("p (h t) n -> p h t n", t=2)
        sif4 = sif[:].rearrange("p (h t) n -> p h t n", t=2)
        P.tt(dve, cand[:], sv4[:, :, 0, :].unsqueeze(3).to_broadcast([128, 8, 16, 16]),
             sv4[:, :, 1, :].unsqueeze(2).to_broadcast([128, 8, 16, 16]), ALU.add, [pk], [pk])
        for h in range(8):
            if h % 2 == 0:
                yield
            ch = cand[:, h].rearrange("p a b -> p (a b)")
            c2h = cand2[:, h].rearrange("p a b -> p (a b)")
            fw.op(dve, lambda hh, h=h, ch=ch: hh.max(out=top[:, h, 0:8], in_=ch), [pk], [pk])
            fw.op(dve, lambda hh, h=h, ch=ch: hh.max_index(out=pos[:, h, 0:8], in_max=top[:, h, 0:8], in_values=ch), [pk], [pk])
            fw.op(dve, lambda hh, h=h, ch=ch, c2h=c2h: hh.match_replace(out=c2h, in_to_replace=top[:, h, 0:8], in_values=ch,
                                                                         imm_value=-3.0e38), [pk], [pk])
            fw.op(dve, lambda hh, h=h, c2h=c2h: hh.max(out=top[:, h, 8:16], in_=c2h), [pk], [pk])
            fw.op(dve, lambda hh, h=h, c2h=c2h: hh.max_index(out=pos[:, h, 8:16], in_max=top[:, h, 8:16], in_values=c2h), [pk], [pk])
        yield
        P.cp(dve, posf[:], pos[:], [pk], [pk])
        P.tt(dve, eq[:, :, :, 0:15], posf[:, :, :].unsqueeze(3).to_broadcast([128, 8, 16, 15]),
             iot[:, 16:31].unsqueeze(1).unsqueeze(1).to_broadcast([128, 8, 16, 15]), ALU.is_ge, [pk, cB], [pk])
        P.red(af[:], eq[:, :, :, 0:15], [pk], [pk])
        fw.op(dve, lambda hh: hh.scalar_tensor_tensor(out=bf[:], in0=af[:], scalar=-16.0, in1=posf[:], op0=ALU.mult, op1=ALU.add),
              [pk], [pk])
        for (src, t_, dst) in ((af, 0, i1s), (bf, 1, i2s)):
            P.tt(dve, eq[:], src[:, :, :].unsqueeze(3).to_broadcast([128, 8, 16, 16]),
                 iot[:, 0:16].unsqueeze(1).unsqueeze(1).to_broadcast([128, 8, 16, 16]), ALU.is_equal, [pk, cB], [pk])
            P.tt(dve, eq[:], eq[:], sif4[:, :, t_, :].unsqueeze(2).to_broadcast([128, 8, 16, 16]), ALU.mult, [pk], [pk])
            P.red(dst[:], eq[:], [pk], [pk])
        if P4_MODE == "route":
            P.cp(dve, outt[:, 0:128], eidf[:], [pk], [outB])
            P.cp(dve, outt[:, 128:256], top[:].rearrange("p h k -> p (h k)"), [pk], [outB])
            P.cp(dve, outt[:, 256:1024], h2[:, 256:1024], [wk], [outB])
            fw.dma(sp, k.out[b, rows, :], outt[:], [outB], [], outS)
            return
        P.tt(dve, gsm[:], top[:], top[:, :, 0:1].to_broadcast([128, 8, 16]), ALU.subtract, [pk], [pk])
        P.act(gsm[:], gsm[:], AF.Exp, [pk], [pk])
        P.red(zs[:], gsm[:], [pk], [pk])
        fw.op(dve, lambda hh: hh.reciprocal(out=zs[:], in_=zs[:]), [pk], [pk])
        P.tt(dve, gsm[:], gsm[:], zs[:, :].unsqueeze(2).to_broadcast([128, 8, 16]), ALU.mult, [pk], [pk])
        if P4_MODE == "r2":
            return
        yield
        btf, btfB = hib()
        for n_, src_ in enumerate((i1s, i2s, gsm)):
            fw.op(pe, lambda hh, n_=n_, src_=src_, btf=btf: hh.transpose(
                out=btf[:, n_ * 128:(n_ + 1) * 128], in_=src_[:].rearrange("p h k -> p (h k)"), identity=k.identf[:]),
                [pk, k.cB], [btfB])
        P.cp(dve, ITJ[:, :, tt * 128:(tt + 1) * 128], btf[:, 0:384].rearrange("p (n t) -> p n t", n=3), [btfB], [itjB])

    def group_peer(b, g2, gp, stepper):
        T = 256
        hnTg = hnTgs[gp]
        grpB = grpBs[gp]
        for t0 in range(0, T, 4):
            bg, bgB = hib()
            for t in range(t0, t0 + 4):
                OH, OHB = OHs[t % 4]
                P.ts(dve, OH[:, 0, :], iorow[:], ITJ[:, 1, t:t + 1], ALU.is_equal, [cB, itjB], [OHB])
                P.ts(dve, OH[:, 1, :], iorow[:], ITJ[:, 0, t:t + 1], ALU.is_equal, [cB, itjB], [OHB],
                     s2=ITJ[:, 2, t:t + 1], op1=ALU.mult)
                P.mm(bg[:, (t - t0) * 128:(t - t0 + 1) * 128], OH[:, 0, :], OH[:, 1, :], True, True, [OHB], [bgB])
            P.cp(act, G_all[:, :, t0:t0 + 4].rearrange("j i t -> j t i"),
                 bg[:, 0:512].rearrange("j (t i) -> j t i", t=4), [bgB], [GB])
        for i in range(128):
            uTb, uTbB, uTbS = uTbs[i % 2]
            vbf, vbfB, vbfS = vbfs[i % 2]
            ge, geB = ges[i % 2]
            Wt, WtB = Wts[i % 2]
            fw.dma(sp, uTb[:], k.sc["uT"][i], [], [uTbB], uTbS)
            fw.dma(sp, vbf[:], k.sc["vbf"][i * 128:(i + 1) * 128, :], [], [vbfB], vbfS)
            ba, baB = hib()
            for c in range(8):
                P.mm(ba[:, 0:T], uTb[:, c, :], hnTg[:, c, :], c == 0, c == 7, [uTbB, grpB], [baB])
            P.act(ge[:], ba[:, 0:T], AF.Gelu_apprx_tanh, [baB], [geB])
            P.tt(pool, Wt[:], ge[:], G_all[:, i, :], ALU.mult, [geB, GB], [WtB])
            for tt in range(2):
                for half in range(2):
                    bo, boB = acc[tt * 2 + half]
                    P.mm(bo[:, 0:512], Wt[:, tt * 128:(tt + 1) * 128], vbf[:, half * 512:(half + 1) * 512], i == 0, i == 127,
                         [WtB, vbfB], [boB])
            stepper(i)
        for tt in range(2):
            outt, outB, outS = outs[tt]
            rows = slice((g2 * 2 + tt) * 128, (g2 * 2 + tt + 1) * 128)
            fw.dma(sp, outt[:], k.sc["h2s"][b, rows, :], [h2sB], [outB], outS)
            for half in range(2):
                bo, boB = acc[tt * 2 + half]
                P.tt(dve, outt[:, half * 512:(half + 1) * 512], bo[:, 0:512], outt[:, half * 512:(half + 1) * 512], ALU.add,
                     [boB, outB], [outB])
            fw.dma(sp, k.out[b, rows, :], outt[:], [outB], [], outS)

    groups = [(b, g2) for b in range(NSEQ) for g2 in range(NT // 2)]

    def front_gen(gi):
        b, g2 = groups[gi]
        for tt in range(2):
            yield from front(b, g2 * 2 + tt, tt, gi % 2)

    def run_all(gen):
        for _ in gen:
            pass

    run_all(front_gen(0))
    for gi, (b, g2) in enumerate(groups):
        nxt = front_gen(gi + 1) if gi + 1 < len(groups) else None
        state = {"gen": nxt}

        def stepper(i, state=state):
            if state["gen"] is None or i % 2 == 1:
                return
            try:
                next(state["gen"])
            except StopIteration:
                state["gen"] = None

        if P4_MODE == "full":
            group_peer(b, g2, gi % 2, stepper)
        if state["gen"] is not None:
            run_all(state["gen"])
    P.close()
    P0.close()


_NC_CACHE = {}


def kernel(**inputs):
    S, NSEQ, NCORE = 4096, 2, 8
    if "nc" not in _NC_CACHE:
        _NC_CACHE["nc"] = build(S, NSEQ)
    nc = _NC_CACHE["nc"]
    consts = host_consts(S)
    x = np.ascontiguousarray(np.asarray(inputs["x"], dtype=np.float32))
    mem = np.ascontiguousarray(np.asarray(inputs["mem"], dtype=np.float32))
    params = {n: np.ascontiguousarray(np.asarray(inputs[n], dtype=np.float32)[0]) for n in PARAM_NAMES}
    in_maps = []
    for c in range(NCORE):
        m = {"x": x[c * NSEQ:(c + 1) * NSEQ], "mem": mem[c * NSEQ:(c + 1) * NSEQ]}
        m.update(params)
        m.update(consts)
        in_maps.append(m)
    res = run_bass_kernel_spmd(nc, in_maps, core_ids=list(range(NCORE)))
    return np.concatenate([np.asarray(r["out"]) for r in res.results], axis=0).astype(np.float32)
```
